# Optimizing a Trainium2 kernel written in Bass

```python
import math
import jax
import jax.numpy as jnp
from jax import lax
import numpy as np


D_MODEL = 1024
BATCH = 8
SEQ = 4096
DEPTH = 1

GRID_W = 64
CTX_LEN = 256
D_SSD = D_MODEL
SSD_HEADDIM = 64
SSD_HEADS = D_SSD // SSD_HEADDIM
SSD_GROUPS = 4
SSD_HEADS_PER_GROUP = SSD_HEADS // SSD_GROUPS
SSD_STATE = 128
CONV_W = 5
CHUNK = 128
CONV_DIM = D_SSD + 2 * SSD_GROUPS * SSD_STATE
D_FOURIER = D_MODEL // 2
FOURIER_GROUPS = 4
FOURIER_CH = D_FOURIER // FOURIER_GROUPS
D_MIX = D_SSD + D_FOURIER
D_PROJ = D_SSD + CONV_DIM + 2 * SSD_HEADS + D_FOURIER
N_EXPERT_GROUPS = 4
EXPERTS_PER_GROUP = 8
N_EXPERTS = N_EXPERT_GROUPS * EXPERTS_PER_GROUP
TOP_K_INNER = 2
D_EXPERT = D_MODEL // 2
EXPERT_BLOCK = 128
EPS = 1e-6

kernel_name = 'hybrid_ssd_fourier_hmoe_dit_block'


def rmsnorm(u, w):
    uf = u.astype(jnp.float32)
    uf = uf * lax.rsqrt(jnp.mean(uf * uf, axis=-1, keepdims=True) + EPS)
    return uf.astype(u.dtype) * w


def modulate(u, shift, scale):
    return u * (1 + scale) + shift


def depthwise_conv(u, w):
    return lax.conv_general_dilated(u, w[:, None, :], window_strides=(1,), padding='SAME',
                                    dimension_numbers=('NWC', 'WIO', 'NWC'),
                                    feature_group_count=u.shape[-1])


def ssd_scan(xd, da, bm, cm, h0):
    out_dtype = xd.dtype
    b, l = xd.shape[0], xd.shape[1]
    nc = l // CHUNK
    g, r, p, n = SSD_GROUPS, SSD_HEADS_PER_GROUP, SSD_HEADDIM, SSD_STATE
    f32 = jnp.float32
    xd = xd.astype(f32).reshape(b, nc, CHUNK, g, r, p)
    da = da.astype(f32).reshape(b, nc, CHUNK, g, r)
    bm = bm.astype(f32).reshape(b, nc, CHUNK, g, n)
    cm = cm.astype(f32).reshape(b, nc, CHUNK, g, n)
    a_cs = jnp.cumsum(da, axis=2)
    a_lr = jnp.moveaxis(a_cs, 2, -1)
    lower = jnp.tril(jnp.ones((CHUNK, CHUNK), bool))
    seg = jnp.where(lower, a_lr[..., :, None] - a_lr[..., None, :], -jnp.inf)
    cb = jnp.einsum('bclgn,bcsgn->bcgls', cm, bm)
    y_diag = jnp.einsum('bcgrls,bcsgrp->bclgrp', cb[:, :, :, None] * jnp.exp(seg), xd)
    decay_to_end = jnp.exp(a_cs[:, :, -1:] - a_cs)
    chunk_states = jnp.einsum('bcqgn,bcqgrp->bcgrpn', bm, xd * decay_to_end[..., None])
    chunk_decay = jnp.exp(a_cs[:, :, -1])

    def carry_state(h, inp):
        dec, st = inp
        return h * dec[..., None, None] + st, h

    h_final, h_in = lax.scan(carry_state, h0.astype(f32),
                             (jnp.moveaxis(chunk_decay, 1, 0), jnp.moveaxis(chunk_states, 1, 0)))
    h_in = jnp.moveaxis(h_in, 0, 1)
    y_off = jnp.einsum('bcqgn,bcgrpn->bcqgrp', cm, h_in) * jnp.exp(a_cs)[..., None]
    y = (y_diag + y_off).reshape(b, l, SSD_HEADS, p)
    return y.astype(out_dtype), h_final


def ssd_mixer(z, xbc, dt_raw, h0_f, h0_b, conv_w, conv_b, dt_bias, a_log, d_skip, norm_w):
    b, l = xbc.shape[0], xbc.shape[1]
    xbc = jax.nn.silu(depthwise_conv(xbc, conv_w) + conv_b)
    xs, bm, cm = jnp.split(xbc, [D_SSD, D_SSD + SSD_GROUPS * SSD_STATE], axis=-1)
    xs = xs.reshape(b, l, SSD_HEADS, SSD_HEADDIM)
    bm = bm.reshape(b, l, SSD_GROUPS, SSD_STATE)
    cm = cm.reshape(b, l, SSD_GROUPS, SSD_STATE)
    dt = jax.nn.softplus(dt_raw.reshape(b, l, 2, SSD_HEADS) + dt_bias)
    da = dt * -jnp.exp(a_log)
    flip = lambda u: jnp.flip(u, axis=1)
    y_f, h_f = ssd_scan(xs * dt[:, :, 0, :, None], da[:, :, 0], bm, cm, h0_f)
    y_b, h_b = ssd_scan(flip(xs * dt[:, :, 1, :, None]), flip(da[:, :, 1]), flip(bm), flip(cm), h0_b)
    y = y_f + flip(y_b) + d_skip[:, None] * xs
    y = y.reshape(b, l, D_SSD) * jax.nn.silu(z)
    return rmsnorm(y, norm_w), h_f, h_b


def fourier_mixer(f, w_four):
    b, l = f.shape[0], f.shape[1]
    fg = f.reshape(b, l, FOURIER_GROUPS, FOURIER_CH).astype(jnp.float32)
    spec = jnp.fft.fft2(fg, axes=(1, 3), norm='ortho').real.astype(f.dtype)
    return jnp.einsum('blgc,gcd->blgd', spec, w_four).reshape(b, l, D_FOURIER)


def split_proj(proj):
    return jnp.split(proj, [D_SSD, D_SSD + CONV_DIM, D_SSD + CONV_DIM + 2 * SSD_HEADS], axis=-1)


def hier_moe(h, w_rg, b_rg, w_re, b_re, w_eg, w_eu, w_ed):
    bsz, l, d = h.shape
    t = h.reshape(bsz * l, d)
    n_tok = t.shape[0]
    f32 = jnp.float32
    p_grp = jax.nn.softmax((t @ w_rg + b_rg).astype(f32), axis=-1)
    p_top_grp, grp = lax.top_k(p_grp, 1)
    logit_exp = jnp.einsum('td,gde->tge', t, w_re) + b_re
    sel = jnp.broadcast_to(grp[:, :, None], (n_tok, 1, EXPERTS_PER_GROUP))
    logit_sel = jnp.take_along_axis(logit_exp, sel, axis=1)[:, 0]
    p_exp = jax.nn.softmax(logit_sel.astype(f32), axis=-1)
    p_top, j_top = lax.top_k(p_exp, TOP_K_INNER)
    gate = p_top_grp * p_top / jnp.sum(p_top, axis=-1, keepdims=True)
    eid = grp * EXPERTS_PER_GROUP + j_top
    n_assign = n_tok * TOP_K_INNER
    e_flat = eid.reshape(-1)
    g_flat = gate.reshape(-1)
    tok_flat = jnp.repeat(jnp.arange(n_tok, dtype=jnp.int32), TOP_K_INNER)
    order = jnp.argsort(e_flat)
    e_s, tok_s, g_s = e_flat[order], tok_flat[order], g_flat[order]
    counts = jnp.bincount(e_flat, length=N_EXPERTS)
    padded = (counts + EXPERT_BLOCK - 1) // EXPERT_BLOCK * EXPERT_BLOCK
    start = jnp.cumsum(counts) - counts
    end_pad = jnp.cumsum(padded)
    start_pad = end_pad - padded
    dest = start_pad[e_s] + jnp.arange(n_assign, dtype=jnp.int32) - start[e_s]
    n_blocks = -(-(n_assign + N_EXPERTS * (EXPERT_BLOCK - 1)) // EXPERT_BLOCK)
    n_rows = n_blocks * EXPERT_BLOCK
    row_tok = jnp.full((n_rows,), n_tok, jnp.int32).at[dest].set(tok_s)
    row_gate = jnp.zeros((n_rows,), f32).at[dest].set(g_s)
    blk_exp = jnp.minimum(jnp.searchsorted(end_pad, jnp.arange(n_blocks) * EXPERT_BLOCK, side='right'),
                          N_EXPERTS - 1)
    t_pad = jnp.concatenate([t, jnp.zeros((1, d), t.dtype)], axis=0)
    rows = t_pad[row_tok].reshape(n_blocks, EXPERT_BLOCK, d)

    def expert_block(args):
        xb, e = args
        return (jax.nn.silu(xb @ w_eg[e]) * (xb @ w_eu[e])) @ w_ed[e]

    y_rows = lax.map(expert_block, (rows, blk_exp)).reshape(n_rows, d)
    y = jax.ops.segment_sum(y_rows.astype(f32) * row_gate[:, None], row_tok,
                            num_segments=n_tok + 1)[:n_tok]
    return y.astype(h.dtype).reshape(bsz, l, d)


def setup_inputs(seed: int = 0) -> dict:
    key = jax.random.key(seed)
    ks = jax.random.split(key, 26)
    f32 = jnp.float32

    def nrm(k, shape, scale):
        return jax.random.normal(k, shape, f32) * scale

    def gain(k, shape):
        return 1.0 + 0.05 * jax.random.normal(k, shape, f32)

    dt0 = jnp.exp(jax.random.uniform(ks[10], (DEPTH, 2, SSD_HEADS), f32,
                                     math.log(1e-3), math.log(1e-1)))
    dt_bias = dt0 + jnp.log(-jnp.expm1(-dt0))
    a_log = jnp.log(jax.random.uniform(ks[11], (DEPTH, 2, SSD_HEADS), f32, 1.0, 16.0))
    return {
        'x': nrm(ks[0], (BATCH, SEQ, D_MODEL), 1.0),
        'c': nrm(ks[1], (BATCH, D_MODEL), 1.0),
        'ctx': nrm(ks[2], (BATCH, CTX_LEN, D_MODEL), 1.0),
        'c_ctx': nrm(ks[3], (D_MODEL,), 1.0),
        'w_mod': nrm(ks[4], (DEPTH, D_MODEL, 6 * D_MODEL), D_MODEL ** -0.5),
        'b_mod': nrm(ks[5], (DEPTH, 6 * D_MODEL), 0.02),
        'norm1': gain(ks[6], (DEPTH, D_MODEL)),
        'w_in': nrm(ks[7], (DEPTH, D_MODEL, D_PROJ), D_MODEL ** -0.5),
        'conv_w': nrm(ks[8], (DEPTH, CONV_W, CONV_DIM), CONV_W ** -0.5),
        'conv_b': nrm(ks[9], (DEPTH, CONV_DIM), 0.02),
        'dt_bias': dt_bias,
        'a_log': a_log,
        'd_skip': gain(ks[12], (DEPTH, SSD_HEADS)),
        'ssd_norm': gain(ks[13], (DEPTH, D_SSD)),
        'w_four': nrm(ks[14], (DEPTH, FOURIER_GROUPS, FOURIER_CH, FOURIER_CH), FOURIER_CH ** -0.5),
        'w_out': nrm(ks[15], (DEPTH, D_MIX, D_MODEL), D_MIX ** -0.5),
        'norm2': gain(ks[16], (DEPTH, D_MODEL)),
        'w_rg': nrm(ks[17], (DEPTH, D_MODEL, N_EXPERT_GROUPS), D_MODEL ** -0.5),
        'b_rg': nrm(ks[18], (DEPTH, N_EXPERT_GROUPS), 0.01),
        'w_re': nrm(ks[19], (DEPTH, N_EXPERT_GROUPS, D_MODEL, EXPERTS_PER_GROUP), D_MODEL ** -0.5),
        'b_re': nrm(ks[20], (DEPTH, N_EXPERT_GROUPS, EXPERTS_PER_GROUP), 0.01),
        'w_eg': nrm(ks[21], (DEPTH, N_EXPERTS, D_MODEL, D_EXPERT), D_MODEL ** -0.5),
        'w_eu': nrm(ks[22], (DEPTH, N_EXPERTS, D_MODEL, D_EXPERT), D_MODEL ** -0.5),
        'w_ed': nrm(ks[23], (DEPTH, N_EXPERTS, D_EXPERT, D_MODEL), D_EXPERT ** -0.5),
        'final_norm': gain(ks[24], (D_MODEL,)),
    }


def reference(x, c, ctx, c_ctx, w_mod, b_mod, norm1, w_in, conv_w, conv_b, dt_bias, a_log,
              d_skip, ssd_norm, w_four, w_out, norm2, w_rg, b_rg, w_re, b_re, w_eg, w_eu, w_ed,
              final_norm):
    n_lat = x.shape[1]
    rows = n_lat // GRID_W
    assert rows * GRID_W == n_lat
    bsz = x.shape[0]
    h_zero = jnp.zeros((bsz, SSD_GROUPS, SSD_HEADS_PER_GROUP, SSD_HEADDIM, SSD_STATE), jnp.float32)
    for layer in range(DEPTH):
        last = layer == DEPTH - 1
        mod_lat = jax.nn.silu(c) @ w_mod[layer] + b_mod[layer]
        mod_ctx = jax.nn.silu(c_ctx) @ w_mod[layer] + b_mod[layer]
        sh1, sc1, g1, sh2, sc2, g2 = jnp.split(mod_lat[:, None, :], 6, axis=-1)
        sh1c, sc1c, g1c, sh2c, sc2c, g2c = jnp.split(mod_ctx, 6, axis=-1)
        prj_c = modulate(rmsnorm(ctx, norm1[layer]), sh1c, sc1c) @ w_in[layer]
        prj_l = modulate(rmsnorm(x, norm1[layer]), sh1, sc1) @ w_in[layer]
        z_c, xbc_c, dt_c, f_c = split_proj(prj_c)
        z_l, xbc_l, dt_l, f_l = split_proj(prj_l)
        y_c, hf_c, hb_c = ssd_mixer(z_c, xbc_c, dt_c, h_zero, h_zero, conv_w[layer], conv_b[layer],
                                    dt_bias[layer], a_log[layer], d_skip[layer], ssd_norm[layer])
        y_l, _, _ = ssd_mixer(z_l, xbc_l, dt_l, hf_c, hb_c, conv_w[layer], conv_b[layer],
                              dt_bias[layer], a_log[layer], d_skip[layer], ssd_norm[layer])
        mix_l = jnp.concatenate([y_l, fourier_mixer(f_l, w_four[layer])], axis=-1) @ w_out[layer]
        x = x + g1 * mix_l
        if not last:
            mix_c = jnp.concatenate([y_c, fourier_mixer(f_c, w_four[layer])], axis=-1) @ w_out[layer]
            ctx = ctx + g1c * mix_c
            ctx = ctx + g2c * hier_moe(modulate(rmsnorm(ctx, norm2[layer]), sh2c, sc2c), w_rg[layer],
                                       b_rg[layer], w_re[layer], b_re[layer], w_eg[layer],
                                       w_eu[layer], w_ed[layer])
        x = x + g2 * hier_moe(modulate(rmsnorm(x, norm2[layer]), sh2, sc2), w_rg[layer], b_rg[layer],
                              w_re[layer], b_re[layer], w_eg[layer], w_eu[layer], w_ed[layer])
    return rmsnorm(x, final_norm)
```

```python
import contextlib
import numpy as np
import ml_dtypes
import concourse.bass as bass
import concourse.mybir as mybir
from concourse.bass_utils import run_bass_kernel_spmd

F32 = mybir.dt.float32
BF16 = mybir.dt.bfloat16
ALU = mybir.AluOpType
AF = mybir.ActivationFunctionType
AX = mybir.AxisListType

D = 1024
KC = 8
T = 4096
TC = 256
TA = T + TC
NT = T // 128
NTA = TA // 128
DPROJ = 3616
EPS = 1e-6
ENGS = ("pe", "act", "dve", "pool", "sp")


class Tile:
    __slots__ = ("name", "w", "r")

    def __init__(self, name):
        self.name = name
        self.w = None
        self.r = {}


class DmaSem:
    def __init__(self, sem):
        self.sem = sem
        self.count = 0


class Ins:
    __slots__ = ("fn", "signal", "rank", "eng")

    def __init__(self, fn, eng):
        self.fn = fn
        self.eng = eng
        self.signal = False
        self.rank = None


class Prog:
    def __init__(self, nc):
        self.nc = nc
        self.ops = {e: [] for e in ENGS}
        self.known = {e: {} for e in ENGS}
        self.esem = {}
        self.stack = contextlib.ExitStack()
        for e in ENGS:
            self.esem[e] = self.stack.enter_context(nc.semaphore("s_" + e))
        self.dma_sems = []
        self.bg_sems = []
        self.last_ins = {e: None for e in ENGS}
        self.nins = {e: 0 for e in ENGS}

    def dsem(self, name, barrier=True):
        s = DmaSem(self.stack.enter_context(self.nc.semaphore(name)))
        if barrier:
            self.dma_sems.append(s)
        else:
            self.bg_sems.append(s)
        return s

    def _need(self, eng, tok):
        if tok is None:
            return
        if tok[0] == "e":
            ins = tok[1]
            if ins.eng == eng and eng == "pe":
                return
            key = ("e", ins.eng)
            idx = ins.rank
            if self.known[eng].get(key, -1) >= idx:
                return
            self.known[eng][key] = idx
            ins.signal = True
            self.ops[eng].append(("wait_e", ins))
        else:
            _, ds, val = tok
            val = max(val, ds.count)
            key = ("d", id(ds))
            if self.known[eng].get(key, -1) >= val:
                return
            self.known[eng][key] = val
            self.ops[eng].append(("wait_d", ds, val))

    def _deps(self, eng, reads, writes):
        for t in reads:
            self._need(eng, t.w)
        for t in writes:
            self._need(eng, t.w)
            for r in t.r.values():
                self._need(eng, r)

    def _commit(self, tok, reads, writes):
        key = ("e", tok[1].eng) if tok[0] == "e" else ("d", id(tok[1]))
        for t in reads:
            t.r[key] = tok
        for t in writes:
            t.w = tok
            t.r = {}

    def op(self, eng, fn, reads=(), writes=()):
        self._deps(eng, reads, writes)
        ins = Ins(fn, eng)
        ins.rank = self.nins[eng]
        self.nins[eng] += 1
        self.ops[eng].append(("ins", ins))
        self.last_ins[eng] = ins
        self._commit(("e", ins), reads, writes)
        return ins

    def dma(self, eng, ds, fn, reads=(), writes=()):
        self._deps(eng, reads, writes)
        ds.count += 16
        self.ops[eng].append(("dma", fn, ds))
        self._commit(("d", ds, ds.count), reads, writes)

    def barrier(self, final=False):
        toks = []
        if final:
            for ds in self.bg_sems:
                if ds.count:
                    toks.append(("d", ds, ds.count))
        for e in ENGS:
            if self.last_ins[e] is not None:
                toks.append(("e", self.last_ins[e]))
        for ds in self.dma_sems:
            if ds.count:
                toks.append(("d", ds, ds.count))
        for e in ENGS:
            for tk in toks:
                if tk[0] == "e" and tk[1].eng == e and e in ("pe", "sp"):
                    continue
                self._need(e, tk)

    def emit(self, block):
        sigcount = {}
        for e in ENGS:
            c = 0
            for o in self.ops[e]:
                if o[0] == "ins":
                    if o[1].signal:
                        c += 1
                        sigcount[id(o[1])] = c
        engs = {"pe": block.tensor, "act": block.scalar, "dve": block.vector,
                "pool": block.gpsimd, "sp": block.sync}
        esem = self.esem

        def make(e):
            ops = self.ops[e]

            def body(eng):
                for o in ops:
                    if o[0] == "ins":
                        r = o[1].fn(eng)
                        if o[1].signal:
                            r.then_inc(esem[e], 1)
                    elif o[0] == "dma":
                        o[1](eng).then_inc(o[2].sem, 16)
                    elif o[0] == "wait_e":
                        eng.wait_ge(esem[o[1].eng], sigcount[id(o[1])])
                    else:
                        eng.wait_ge(o[1].sem, o[2])
            return body

        for e in ENGS:
            engs[e](make(e))


class Buf:
    def __init__(self, t, name, nparts=1):
        self.t = t
        self.tiles = [Tile("%s.%d" % (name, i)) for i in range(nparts)]

    def __getitem__(self, k):
        return self.t[k]

    def p(self, i=0):
        return self.tiles[i]

    def all(self):
        return list(self.tiles)


def _consts():
    k = np.arange(128)
    c = {}
    c["ident_f"] = np.eye(128, dtype=np.float32)
    c["ident_b"] = np.eye(128, dtype=np.float32).astype(ml_dtypes.bfloat16)
    c["m_le"] = (k[:, None] <= k[None, :]).astype(np.float32)
    c["m_gt"] = (k[:, None] > k[None, :]).astype(np.float32)
    c["m_ge"] = (k[:, None] >= k[None, :]).astype(np.float32)
    c["m_lt"] = (k[:, None] < k[None, :]).astype(np.float32)
    c["ones"] = np.ones((128, 128), np.float32)
    cc = (k[:, None] * k[None, :]) % 128
    ang = 2.0 * np.pi * cc / 128.0
    c["cs"] = np.concatenate([np.cos(ang), -np.sin(ang)], axis=1).astype(np.float32).astype(ml_dtypes.bfloat16)
    n = (128 * np.arange(32)[None, :] + k[:, None]).astype(np.int64)
    kk = (512 * np.arange(8)[:, None] + np.arange(512)[None, :]).astype(np.int64)
    m = (n[None, :, :, None] * kk[:, None, None, :]) % 4096
    a = (2.0 * np.pi / 4096.0) * m.astype(np.float64)
    tab = np.stack([np.cos(a), np.sin(a)], axis=3)
    c["dft_tab"] = np.ascontiguousarray(tab.astype(np.float32).astype(ml_dtypes.bfloat16))
    return c


_CONSTS = None


def _get_consts():
    global _CONSTS
    if _CONSTS is None:
        _CONSTS = _consts()
    return _CONSTS


def build(dbg=()):
    nc = bass.Bass("TRN2", target_bir_lowering=False)
    dbg = set(dbg)

    def din(name, shape, dt=F32):
        return nc.dram_tensor(name, list(shape), dt, kind="ExternalInput").ap()

    def dout(name, shape, dt=F32):
        return nc.dram_tensor(name, list(shape), dt, kind="ExternalOutput").ap()

    x_d = din("x", [T, D])
    ctx_d = din("ctx", [TC, D])
    cvec_d = din("cvec", [128, KC, 2])
    w_mod_d = din("w_mod", [D, 6 * D])
    pp_d = din("pp", [128, 128])
    ident_f_d = din("ident_f", [128, 128])
    ident_b_d = din("ident_b", [128, 128], BF16)
    w_in_d = din("w_in", [D, DPROJ])
    cpar_d = din("cpar", [128, 16, 6])
    rbig_d = din("rbig", [128, 6, D])
    rp_d = din("rp", [128, 128])
    masks_d = din("masks", [128, 5, 128])
    w_four_d = din("w_four", [4, 128, 128])
    w_out_d = din("w_out", [1536, D])
    wr_d = din("wr", [128, KC, 36])
    w_eg_d = din("w_eg", [32, 128, KC, 512])
    w_eu_d = din("w_eu", [32, 128, KC, 512])
    w_ed_d = din("w_ed", [32, 128, 4, D])
    cs_d = din("cs", [128, 256], BF16)
    dft_d = din("dft_tab", [8, 128, 32, 2, 512], BF16)
    out_d = dout("out", [T, D])
    f_s = nc.dram_tensor("f_s", [4, 128, T], BF16).ap()
    four_s = nc.dram_tensor("four_s", [4, 128, T], BF16).ap()
    x1_s = nc.dram_tensor("x1_s", [T, D], F32).ap()
    h_s = nc.dram_tensor("h_s", [T + 128, D], BF16).ap()
    hs_sorted = nc.dram_tensor("hs_sorted", [48 * 512, D], BF16).ap()
    y_sorted = nc.dram_tensor("y_sorted", [48 * 512, D], F32).ap()
    hc_d = din("hc", [128, 128])
    zeros_d = din("zeros", [128, D], BF16)
    padidx_d = din("padidx", [48 * 512, 2], mybir.dt.int32)
    tokid_d = din("tokid", [128, NT, 2], mybir.dt.int32)
    rowtok = nc.dram_tensor("rowtok", [48 * 512, 2], mybir.dt.int32).ap()
    wgs = nc.dram_tensor("wgs", [32 * 128, 4096], BF16).ap()
    wus = nc.dram_tensor("wus", [32 * 128, 4096], BF16).ap()
    wds = nc.dram_tensor("wds", [32 * 128, 4096], BF16).ap()
    bc_s = nc.dram_tensor("bc_s", [8, 128, TA], BF16).ap()
    xs_s = nc.dram_tensor("xs_s", [TA, 1024], BF16).ap()
    bt_s = nc.dram_tensor("bt_s", [TA, 512], BF16).ap()
    y_s = nc.dram_tensor("y_s", [T, 1024], F32).ap()
    yb_s = nc.dram_tensor("yb_s", [T, 1024], F32).ap()
    dbg_d = {}
    if "uT" in dbg:
        dbg_d["uT"] = dout("dbg_uT", [128, KC, TA], BF16)
    if "ssd" in dbg:
        dbg_d["y"] = dout("dbg_y", [T, 1024])
        dbg_d["yb"] = dout("dbg_yb", [T, 1024])
    if "four" in dbg:
        dbg_d["four"] = dout("dbg_four", [4, 128, T], BF16)
    if "x1" in dbg:
        dbg_d["x1"] = dout("dbg_x1", [T, D])
        dbg_d["lg"] = dout("dbg_lg", [128, NT, 36])
        dbg_d["slot"] = dout("dbg_slot", [128, 2, NT], mybir.dt.int32)
        dbg_d["widx"] = dout("dbg_widx", [128, 2, 48], mybir.dt.int32)
        dbg_d["gates"] = dout("dbg_gates", [128, 2, NT])
        dbg_d["slotf"] = dout("dbg_slotf", [128, 2, NT])
    if "conv" in dbg:
        dbg_d["bc"] = dout("dbg_bc", [8, 128, TA], BF16)
        dbg_d["xs"] = dout("dbg_xs", [TA, 1024], BF16)
        dbg_d["bt"] = dout("dbg_bt", [TA, 512], BF16)
        dbg_d["dts"] = dout("dbg_dts", [128, 7, NTA * 32])

    P = Prog(nc)
    st = contextlib.ExitStack()

    cur = [st]

    def sb(name, shape, dt=F32, nparts=1):
        return Buf(cur[0].enter_context(nc.sbuf_tensor("sb_" + name, list(shape), dt)), name, nparts)

    def ps(name, shape, dt=F32, nparts=1):
        return Buf(st.enter_context(nc.psum_tensor("ps_" + name, list(shape), dt)), name, nparts)

    class Phase:
        def __enter__(self):
            self.prev = cur[0]
            self.stk = contextlib.ExitStack()
            cur[0] = self.stk
            return self

        def __exit__(self, *a):
            P.barrier()
            cur[0] = self.prev
            self.stk.close()
            return False

    stp = contextlib.ExitStack()

    def sbp(name, shape, dt=F32, nparts=1):
        return Buf(stp.enter_context(nc.sbuf_tensor("sb_" + name, list(shape), dt)), name, nparts)

    with P.stack, stp, st, nc.Block() as block:
        ident_f = sb("ident_f", [128, 128])
        ident_b = sb("ident_b", [128, 128], BF16)
        pp = sb("pp", [128, 128])
        cvec = sb("cvec", [128, KC, 2])
        svec = sb("svec", [128, KC, 2])
        ds_c = P.dsem("ds_c")
        P.dma("sp", ds_c, lambda e: e.dma_start(out=ident_f[:, :], in_=ident_f_d[:, :]), writes=[ident_f.p()])
        P.dma("sp", ds_c, lambda e: e.dma_start(out=ident_b[:, :], in_=ident_b_d[:, :]), writes=[ident_b.p()])
        P.dma("sp", ds_c, lambda e: e.dma_start(out=pp[:, :], in_=pp_d[:, :]), writes=[pp.p()])
        P.dma("sp", ds_c, lambda e: e.dma_start(out=cvec[:, :, :], in_=cvec_d[:, :, :]), writes=[cvec.p()])
        P.op("act", lambda e: e.activation(out=svec[:, :, :], in_=cvec[:, :, :], func=AF.Silu),
             reads=[cvec.p()], writes=[svec.p()])

        PP_N1, PP_BMOD = 0, 8

        banks = [ps("bank%d" % i, [128, 512]) for i in range(8)]
        psm = banks[0]
        pst = banks[1:3]
        psA = banks[3:7]
        modT = sb("modT", [128, 48, 2])
        scA = sb("scA", [128, KC, 2])
        epsb = sb("epsb", [128, 1])
        oneb = sb("oneb", [128, 1])
        P.op("pool", lambda e: e.memset(oneb[:, :], 1.0), writes=[oneb.p()])
        P.op("pool", lambda e: e.memset(epsb[:, :], EPS), writes=[epsb.p()])
        rp = sb("rp", [128, 128])
        g12 = sb("g12", [128, 4, D])
        rs = sb("rs", [128, NTA])
        scB = sb("scB", [128, KC])
        phA0 = Phase().__enter__()
        rbig = sb("rbig", [128, 5, D])
        ds_c1 = P.dsem("ds_c1")
        P.dma("sp", ds_c1, lambda e: e.dma_start(out=rbig[:, 0:2, :], in_=rbig_d[:, 0:2, :]), writes=[rbig.p()])
        P.dma("sp", ds_c1, lambda e: e.dma_start(out=rbig[:, 2:5, :], in_=rbig_d[:, 3:6, :]), writes=[rbig.p()])
        svb = sb("svb", [128, KC, 128])
        P.op("dve", lambda e: e.tensor_copy(out=svb[:, :, :], in_=svec[:, :, 0:1].to_broadcast([128, KC, 128])),
             reads=[svec.p()], writes=[svb.p()])
        wm = [sb("wm%d" % i, [128, KC, 512]) for i in range(2)]
        ds_wm = [P.dsem("ds_wm%d" % i) for i in range(2)]
        w_mod_v = w_mod_d.rearrange("(kc p) n -> p kc n", p=128)
        for blk in range(12):
            b = wm[blk % 2]
            P.dma("sp", ds_wm[blk % 2],
                  lambda e, b=b, blk=blk: e.dma_start(out=b[:, :, :], in_=w_mod_v[:, :, blk * 512:(blk + 1) * 512]),
                  writes=[b.p()])
            if blk in (4, 5, 6, 7, 8, 9, 10, 11):
                gi = {4: 0, 5: 0, 10: 1, 11: 1, 6: 2, 7: 2, 8: 3, 9: 3}[blk]
                hb = blk % 2
                pgb = banks[1 + (blk % 2)]
                for kc in range(KC):
                    P.op("pe", lambda e, b=b, kc=kc, pgb=pgb: e.matmul(
                        pgb[:, :], lhsT=svb[:, kc, :], rhs=b[:, kc, :], start=(kc == 0), stop=(kc == KC - 1)),
                        reads=[b.p(), svb.p()], writes=[pgb.p()])
                P.op("dve", lambda e, pgb=pgb, gi=gi, hb=hb: e.tensor_tensor(
                    out=g12[:, gi, hb * 512:(hb + 1) * 512], in0=pgb[:, :], in1=rbig[:, gi, hb * 512:(hb + 1) * 512],
                    op=ALU.add), reads=[pgb.p(), rbig.p()], writes=[g12.p()])
            for j in range(4):
                cj = blk * 4 + j
                for kc in range(KC):
                    P.op("pe", lambda e, b=b, j=j, kc=kc, cj=cj: e.matmul(
                        psm[:, cj * 2:cj * 2 + 2], lhsT=b[:, kc, j * 128:(j + 1) * 128], rhs=svec[:, kc, :],
                        start=(kc == 0), stop=(kc == KC - 1)),
                        reads=[b.p(), svec.p()], writes=[psm.p()])
        P.op("dve", lambda e: e.tensor_tensor(
            out=modT[:, :, :], in0=psm[:, 0:96].rearrange("p (c two) -> p c two", two=2),
            in1=pp[:, PP_BMOD:PP_BMOD + 48].unsqueeze(2).to_broadcast([128, 48, 2]), op=ALU.add),
            reads=[psm.p(), pp.p()], writes=[modT.p()])
        P.op("dve", lambda e: e.scalar_tensor_tensor(
            out=scA[:, :, :], in0=modT[:, 8:16, :], scalar=1.0,
            in1=pp[:, PP_N1:PP_N1 + 8].unsqueeze(2).to_broadcast([128, KC, 2]),
            op0=ALU.add, op1=ALU.mult),
            reads=[modT.p(), pp.p()], writes=[scA.p()])
        P.op("dve", lambda e: e.scalar_tensor_tensor(
            out=g12[:, 3, :], in0=g12[:, 3, :], scalar=1.0, in1=rbig[:, 4, :], op0=ALU.add, op1=ALU.mult),
            reads=[g12.p(), rbig.p()], writes=[g12.p()])
        P.op("dve", lambda e: e.scalar_tensor_tensor(
            out=scB[:, :], in0=modT[:, 32:40, 0], scalar=1.0, in1=pp[:, 56:64], op0=ALU.add, op1=ALU.mult),
            reads=[modT.p(), pp.p()], writes=[scB.p()])
        phA0.__exit__()
        phX = Phase().__enter__()
        NDT = NTA * 32
        dts = sb("dts", [128, 7, NDT])
        masks = sb("masks", [128, 5, 128])
        phU = Phase().__enter__()
        uT = sb("uT", [128, KC, TA], BF16, nparts=NTA)
        phA1 = Phase().__enter__()
        ds_z = P.dsem("ds_z", barrier=False)
        t_hs0 = Tile("rowtok_init")
        P.dma("pool", ds_z, lambda e: e.dma_start(out=rowtok[:, :], in_=padidx_d[:, :]), writes=[t_hs0])
        P.dma("pool", ds_z, lambda e: e.dma_start(out=h_s[T:T + 128, :], in_=zeros_d[:, :]), writes=[t_hs0])
        xt = [sb("xt%d" % i, [128, D]) for i in range(3)]
        ds_x = [P.dsem("ds_x%d" % i) for i in range(3)]
        xn = [sb("xn%d" % i, [128, D], BF16) for i in range(2)]
        junk = sb("junk", [128, D])
        ss = sb("ss", [128, NTA])
        utmp = [sb("utmp%d" % i, [128, KC, 128]) for i in range(2)]
        def xsrc(i):
            return ctx_d[i * 128:(i + 1) * 128, :] if i < 2 else x_d[(i - 2) * 128:(i - 1) * 128, :]
        for i in range(NTA):
            xb = xt[i % 3]
            P.dma("sp", ds_x[i % 3], lambda e, xb=xb, src=xsrc(i): e.dma_start(out=xb[:, :], in_=src), writes=[xb.p()])
            P.op("act", lambda e, xb=xb, i=i: e.activation(out=junk[:, :], in_=xb[:, :], func=AF.Square,
                                                           accum_out=ss[:, i:i + 1]),
                 reads=[xb.p()], writes=[junk.p(), ss.p()])
        P.op("act", lambda e: e.activation(out=rs[:, :], in_=ss[:, :], func=AF.Sqrt, scale=1.0 / D, bias=epsb[:, 0:1]),
             reads=[ss.p(), epsb.p()], writes=[rs.p()])
        P.op("dve", lambda e: e.reciprocal(out=rs[:, :], in_=rs[:, :]), reads=[rs.p()], writes=[rs.p()])
        for i in range(NTA):
            xb = xt[i % 3]
            which = 1 if i < 2 else 0
            P.dma("sp", ds_x[i % 3], lambda e, xb=xb, src=xsrc(i): e.dma_start(out=xb[:, :], in_=src), writes=[xb.p()])
            xnb = xn[i % 2]
            P.op("act", lambda e, xb=xb, xnb=xnb, i=i: e.activation(out=xnb[:, :], in_=xb[:, :], func=AF.Copy,
                                                                    scale=rs[:, i:i + 1]),
                 reads=[xb.p(), rs.p()], writes=[xnb.p()])
            pt = pst[i % 2]
            ptb = pt.t[:, :].bitcast(BF16)
            for kc in range(KC):
                P.op("pe", lambda e, ptb=ptb, xnb=xnb, kc=kc: e.transpose(
                    ptb[:, kc * 128:(kc + 1) * 128], xnb[:, kc * 128:(kc + 1) * 128], ident_b[:, :]),
                    reads=[xnb.p(), ident_b.p()], writes=[pt.p()])
            ut = utmp[i % 2]
            P.op("dve", lambda e, ptb=ptb, ut=ut, which=which: e.tensor_tensor(
                out=ut[:, :, :], in0=ptb.rearrange("p (k t) -> p k t", k=KC),
                in1=scA[:, :, which:which + 1].to_broadcast([128, KC, 128]), op=ALU.mult),
                reads=[pt.p(), scA.p()], writes=[ut.p()])
            P.op("pool", lambda e, ut=ut, i=i, which=which: e.tensor_tensor(
                out=uT[:, :, i * 128:(i + 1) * 128], in0=ut[:, :, :],
                in1=modT[:, 0:8, which:which + 1].to_broadcast([128, KC, 128]), op=ALU.add),
                reads=[ut.p(), modT.p()], writes=[uT.p(i)])

        if "uT" in dbg:
            ds_dbg = P.dsem("ds_dbg")
            P.dma("sp", ds_dbg, lambda e: e.dma_start(out=dbg_d["uT"][:, :, :], in_=uT[:, :, :]),
                  reads=uT.all())

        phA1.__exit__()
        phB = Phase().__enter__()
        RP_DTB, RP_ALOG, RP_DSKIP = 0, 32, 64
        cpar = sb("cpar", [128, 16, 6])
        ds_c3 = P.dsem("ds_c3")
        P.dma("sp", ds_c3, lambda e: e.dma_start(out=cpar[:, :, :], in_=cpar_d[:, :, :]), writes=[cpar.p()])
        P.dma("sp", ds_c3, lambda e: e.dma_start(out=rp[:, :], in_=rp_d[:, :]), writes=[rp.p()])
        P.dma("sp", ds_c3, lambda e: e.dma_start(out=masks[:, :, :], in_=masks_d[:, :, :]), writes=[masks.p()])
        w_in_v = w_in_d.rearrange("(kc p) n -> p kc n", p=128)
        wcb = [sb("wcb%d" % i, [128, KC, 512], BF16) for i in range(2)]
        ds_pc = [P.dsem("ds_pc%d" % i, barrier=False) for i in range(4)]
        t_pc = [Tile("pc%d" % i) for i in range(4)]
        t_wcast = Tile("wcast")
        pc_list = []
        for (src_, dst_) in ((w_eg_d, wgs), (w_eu_d, wus), (w_ed_d, wds)):
            sv_ = src_.rearrange("e p k f -> e p (k f)")
            for ex in range(32):
                pc_list.append((sv_, dst_, ex))
        pc_pos = [0]

        def precast_issue(n):
            for _ in range(n):
                if pc_pos[0] >= len(pc_list):
                    return
                sv_, dst_, ex = pc_list[pc_pos[0]]
                k = pc_pos[0] % 4
                pc_pos[0] += 1
                P.dma("pool", ds_pc[k], lambda e, sv_=sv_, dst_=dst_, ex=ex: e.dma_start(
                    out=dst_[ex * 128:(ex + 1) * 128, :], in_=sv_[ex]), writes=[t_pc[k], t_wcast])
        ds_wc = [P.dsem("ds_wc%d" % i) for i in range(2)]
        pre = [sb("pre%d" % i, [128, TA + 8], BF16) for i in range(2)]
        acc = [sb("acc%d" % i, [128, TA + 4]) for i in range(1)] * 2
        post = [sb("post%d" % i, [128, TA], BF16) for i in range(2)]
        tokb = [sb("tokb%d" % i, [128, NTA, 128], BF16) for i in range(1)] * 2
        ds_post = [P.dsem("ds_post%d" % i) for i in range(2)]
        ds_tokb = [P.dsem("ds_tokb%d" % i) for i in range(2)]
        for pb in pre:
            P.op("pool", lambda e, pb=pb: e.memset(pb[:, :], 0.0), writes=[pb.p()])
        xs_v = xs_s.rearrange("(i p) c -> p i c", p=128)
        bt_v = bt_s.rearrange("(i p) c -> p i c", p=128)
        CL = TA + 4
        for cc in range(16):
            if cc % 4 == 0:
                wb = wcb[(cc // 4) % 2]
                c0 = 1024 + 128 * cc
                P.dma("pool", ds_wc[(cc // 4) % 2],
                      lambda e, wb=wb, c0=c0: e.dma_start(out=wb[:, :, :], in_=w_in_v[:, :, c0:c0 + 512]),
                      writes=[wb.p()])
            j = cc % 4
            pb, ab, qb = pre[cc % 2], acc[cc % 2], post[cc % 2]
            for tb in range(9):
                n = 256 if tb == 0 else 512
                tok0 = 0 if tb == 0 else 256 + 512 * (tb - 1)
                off = 2 if tb == 0 else 262 + 512 * (tb - 1)
                pa = psA[tb % 4]
                for kc in range(KC):
                    P.op("pe", lambda e, pa=pa, wb=wb, j=j, kc=kc, tok0=tok0, n=n: e.matmul(
                        pa[:, 0:n], lhsT=wb[:, kc, j * 128:(j + 1) * 128], rhs=uT[:, kc, tok0:tok0 + n],
                        start=(kc == 0), stop=(kc == KC - 1)),
                        reads=[wb.p()] + uT.all()[tok0 // 128:(tok0 + n) // 128], writes=[pa.p()])
                P.op("act", lambda e, pa=pa, pb=pb, off=off, n=n: e.copy(out=pb[:, off:off + n], in_=pa[:, 0:n]),
                     reads=[pa.p()], writes=[pb.p()])
            ceng = "dve"
            precast_issue(4)
            P.op(ceng, lambda e, ab=ab, pb=pb, cc=cc: e.tensor_scalar(
                out=ab[:, :], in0=pb[:, 0:CL], scalar1=cpar[:, cc, 0:1], scalar2=None, op0=ALU.mult),
                reads=[pb.p(), cpar.p()], writes=[ab.p()])
            for tap in range(1, 5):
                P.op(ceng, lambda e, ab=ab, pb=pb, cc=cc, tap=tap: e.scalar_tensor_tensor(
                    out=ab[:, :], in0=pb[:, tap:tap + CL], scalar=cpar[:, cc, tap:tap + 1], in1=ab[:, :],
                    op0=ALU.mult, op1=ALU.add),
                    reads=[pb.p(), cpar.p(), ab.p()], writes=[ab.p()])
            P.op("act", lambda e, ab=ab, qb=qb, cc=cc: e.activation(
                out=qb[:, 0:256], in_=ab[:, 0:256], func=AF.Silu, bias=cpar[:, cc, 5:6]),
                reads=[ab.p(), cpar.p()], writes=[qb.p()])
            P.op("act", lambda e, ab=ab, qb=qb, cc=cc: e.activation(
                out=qb[:, 256:TA], in_=ab[:, 260:260 + T], func=AF.Silu, bias=cpar[:, cc, 5:6]),
                reads=[ab.p(), cpar.p()], writes=[qb.p()])
            if cc >= 8:
                P.dma("sp", ds_post[cc % 2], lambda e, qb=qb, cc=cc: e.dma_start(out=bc_s[cc - 8, :, :], in_=qb[:, :]),
                      reads=[qb.p()])
            if cc < 12:
                tk = tokb[cc % 2]
                for i0 in range(0, NTA, 8):
                    ni = min(8, NTA - i0)
                    pt = pst[(i0 // 8) % 2]
                    ptb = pt.t[:, :].bitcast(BF16)
                    for ii in range(ni):
                        i = i0 + ii
                        P.op("pe", lambda e, ptb=ptb, qb=qb, i=i, ii=ii: e.transpose(
                            ptb[:, ii * 128:(ii + 1) * 128], qb[:, i * 128:(i + 1) * 128], ident_b[:, :]),
                            reads=[qb.p(), ident_b.p()], writes=[pt.p()])
                    eng2 = "pool" if cc % 2 == 0 else "dve"
                    eng2 = "dve"
                    P.op(eng2, lambda e, ptb=ptb, tk=tk, i0=i0, ni=ni: e.tensor_copy(
                        out=tk[:, i0:i0 + ni, :], in_=ptb[:, 0:ni * 128].rearrange("p (i c) -> p i c", c=128)),
                        reads=[pt.p()], writes=[tk.p()])
                if cc < 8:
                    dst = xs_v[:, :, cc * 128:(cc + 1) * 128]
                else:
                    dst = bt_v[:, :, (cc - 8) * 128:(cc - 7) * 128]
                P.dma("sp", ds_tokb[cc % 2], lambda e, tk=tk, dst=dst: e.dma_start(out=dst, in_=tk[:, :, :]),
                      reads=[tk.p()])

        wdt = sb("wdt", [128, KC, 32], BF16)
        ds_wdt = P.dsem("ds_wdt")
        P.dma("pool", ds_wdt, lambda e: e.dma_start(out=wdt[:, :, :], in_=w_in_v[:, :, 3072:3104]), writes=[wdt.p()])
        abc = sb("abc", [128, 32])
        P.op("act", lambda e: e.activation(out=abc[:, :], in_=rp[:, RP_ALOG:RP_ALOG + 32], func=AF.Exp),
             reads=[rp.p()], writes=[abc.p()])
        P.op("dve", lambda e: e.tensor_scalar(out=abc[:, :], in0=abc[:, :], scalar1=-1.0, scalar2=None, op0=ALU.mult),
             reads=[abc.p()], writes=[abc.p()])
        for c3 in range(3):
            i0 = c3 * 16
            ni = min(16, NTA - i0)
            pa = psA[c3]
            for ii in range(ni):
                i = i0 + ii
                for kc in range(KC):
                    P.op("pe", lambda e, pa=pa, ii=ii, i=i, kc=kc: e.matmul(
                        pa[:, ii * 32:(ii + 1) * 32], lhsT=uT[:, kc, i * 128:(i + 1) * 128], rhs=wdt[:, kc, :],
                        start=(kc == 0), stop=(kc == KC - 1)),
                        reads=[uT.p(i), wdt.p()], writes=[pa.p()])
            P.op("dve", lambda e, pa=pa, i0=i0, ni=ni: e.tensor_tensor(
                out=dts[:, 0, i0 * 32:(i0 + ni) * 32].rearrange("p (i c) -> p i c", c=32),
                in0=pa[:, 0:ni * 32].rearrange("p (i c) -> p i c", c=32),
                in1=rp[:, RP_DTB:RP_DTB + 32].unsqueeze(1).to_broadcast([128, ni, 32]), op=ALU.add),
                reads=[pa.p(), rp.p()], writes=[dts.p()])
        P.op("act", lambda e: e.activation(out=dts[:, 0, :], in_=dts[:, 0, :], func=AF.Exp),
             reads=[dts.p()], writes=[dts.p()])
        P.op("act", lambda e: e.activation(out=dts[:, 0, :], in_=dts[:, 0, :], func=AF.Ln, bias=oneb[:, 0:1]),
             reads=[dts.p(), oneb.p()], writes=[dts.p()])
        P.op("dve", lambda e: e.tensor_tensor(
            out=dts[:, 1, :].rearrange("p (i c) -> p i c", c=32),
            in0=dts[:, 0, :].rearrange("p (i c) -> p i c", c=32),
            in1=abc[:, :].unsqueeze(1).to_broadcast([128, NTA, 32]), op=ALU.mult),
            reads=[dts.p(), abc.p()], writes=[dts.p()])
        for q, mi in enumerate([0, 1, 2, 3, 4]):
            for c3 in range(3):
                c0 = c3 * 512
                n = min(512, NDT - c0)
                pa = psA[(q * 3 + c3) % 4]
                P.op("pe", lambda e, pa=pa, mi=mi, c0=c0, n=n: e.matmul(
                    pa[:, 0:n], lhsT=masks[:, mi, :], rhs=dts[:, 1, c0:c0 + n], start=True, stop=True),
                    reads=[masks.p(), dts.p()], writes=[pa.p()])
                P.op("act", lambda e, pa=pa, q=q, c0=c0, n=n: e.activation(
                    out=dts[:, 2 + q, c0:c0 + n], in_=pa[:, 0:n], func=AF.Exp),
                    reads=[pa.p()], writes=[dts.p()])
        if "conv" in dbg:
            P.barrier()
            ds_dbg2 = P.dsem("ds_dbg2")
            P.dma("sp", ds_dbg2, lambda e: e.dma_start(out=dbg_d["bc"][:, :, :], in_=bc_s[:, :, :]))
            P.dma("sp", ds_dbg2, lambda e: e.dma_start(out=dbg_d["xs"][:, :], in_=xs_s[:, :]))
            P.dma("sp", ds_dbg2, lambda e: e.dma_start(out=dbg_d["bt"][:, :], in_=bt_s[:, :]))
            P.dma("sp", ds_dbg2, lambda e: e.dma_start(out=dbg_d["dts"][:, :, :], in_=dts[:, :, :]), reads=[dts.p()])
        phB.__exit__()

        phC1 = Phase().__enter__()
        wf = sb("wf", [128, KC, 512], BF16)
        ds_wf = P.dsem("ds_wf")
        P.dma("pool", ds_wf, lambda e: e.dma_start(out=wf[:, :, :], in_=w_in_v[:, :, 3104:3616]), writes=[wf.p()])
        fblk = [sb("fblk%d" % i, [128, T], BF16) for i in range(2)]
        ds_fb = [P.dsem("ds_fb%d" % i) for i in range(2)]
        for g in range(4):
            fb = fblk[g % 2]
            for tb in range(8):
                pa = psA[tb % 4]
                tok0 = 256 + 512 * tb
                for kc in range(KC):
                    P.op("pe", lambda e, pa=pa, g=g, kc=kc, tok0=tok0: e.matmul(
                        pa[:, :], lhsT=wf[:, kc, g * 128:(g + 1) * 128], rhs=uT[:, kc, tok0:tok0 + 512],
                        start=(kc == 0), stop=(kc == KC - 1)),
                        reads=[wf.p()] + uT.all()[tok0 // 128:tok0 // 128 + 4], writes=[pa.p()])
                ev = "act" if tb % 2 == 0 else "dve"
                if ev == "act":
                    P.op("act", lambda e, pa=pa, fb=fb, tb=tb: e.copy(out=fb[:, tb * 512:(tb + 1) * 512], in_=pa[:, :]),
                         reads=[pa.p()], writes=[fb.p()])
                else:
                    P.op("dve", lambda e, pa=pa, fb=fb, tb=tb: e.tensor_copy(out=fb[:, tb * 512:(tb + 1) * 512], in_=pa[:, :]),
                         reads=[pa.p()], writes=[fb.p()])
            P.dma("sp", ds_fb[g % 2], lambda e, fb=fb, g=g: e.dma_start(out=f_s[g, :, :], in_=fb[:, :]), reads=[fb.p()])
        phC1.__exit__()

        phU.__exit__()

        phC = Phase().__enter__()
        fT = sb("fT", [128, 4, T], BF16)
        Yb = sb("Yb", [128, NT, 4, 256], BF16, nparts=NT)
        cs = sb("cs", [128, 256], BF16)
        wfour = sb("wfour", [128, 4, 128], BF16)
        tabs = [sb("tabs%d" % i, [128, 8, 2, 512], BF16) for i in range(2)]
        ds_tab = [P.dsem("ds_tab%d" % i) for i in range(2)]
        specT = [sb("specT%d" % i, [128, 4, 512], BF16) for i in range(2)]
        fourb = [sb("fourb%d" % i, [128, 4, 512], BF16) for i in range(2)]
        ds_four = [P.dsem("ds_four%d" % i) for i in range(2)]
        ds_cc = P.dsem("ds_cc")
        P.dma("sp", ds_cc, lambda e: e.dma_start(out=fT[:, :, :], in_=f_s.rearrange("g p t -> p g t")), writes=[fT.p()])
        P.dma("sp", ds_cc, lambda e: e.dma_start(out=cs[:, :], in_=cs_d[:, :]), writes=[cs.p()])
        ds_cc2 = P.dsem("ds_cc2")
        P.dma("pool", ds_cc2, lambda e: e.dma_start(out=wfour[:, :, :], in_=w_four_d.rearrange("g c d -> c g d")),
              writes=[wfour.p()])
        for i in range(NT):
            for h2 in range(2):
                pa = psA[(2 * i + h2) % 4]
                for gg in range(2):
                    g = 2 * h2 + gg
                    P.op("pe", lambda e, pa=pa, gg=gg, g=g, i=i: e.matmul(
                        pa[:, gg * 256:(gg + 1) * 256], lhsT=fT[:, g, i * 128:(i + 1) * 128], rhs=cs[:, :],
                        start=True, stop=True), reads=[fT.p(), cs.p()], writes=[pa.p()])
                if h2 == 0:
                    P.op("act", lambda e, pa=pa, i=i, h2=h2: e.copy(
                        out=Yb[:, i, 2 * h2:2 * h2 + 2, :], in_=pa[:, :].rearrange("p (g c) -> p g c", c=256)),
                        reads=[pa.p()], writes=[Yb.p(i)])
                else:
                    P.op("dve", lambda e, pa=pa, i=i, h2=h2: e.tensor_copy(
                        out=Yb[:, i, 2 * h2:2 * h2 + 2, :], in_=pa[:, :].rearrange("p (g c) -> p g c", c=256)),
                        reads=[pa.p()], writes=[Yb.p(i)])
        ORTHO = 1.0 / float(np.sqrt(4096.0 * 128.0))
        piece = 0
        for kb in range(8):
            for q in range(4):
                tb_ = tabs[piece % 2]
                P.dma("sp", ds_tab[piece % 2], lambda e, tb_=tb_, kb=kb, q=q: e.dma_start(
                    out=tb_[:, :, :, :], in_=dft_d[kb, :, q * 8:(q + 1) * 8, :, :]), writes=[tb_.p()])
                piece += 1
                for ii in range(8):
                    i = q * 8 + ii
                    for g in range(4):
                        P.op("pe", lambda e, g=g, i=i, ii=ii, tb_=tb_: e.matmul(
                            banks[g][:, :], lhsT=Yb[:, i, g, 0:128], rhs=tb_[:, ii, 0, :], start=(i == 0), stop=False),
                            reads=[Yb.p(i), tb_.p()], writes=[banks[g].p()])
                        P.op("pe", lambda e, g=g, i=i, ii=ii, tb_=tb_: e.matmul(
                            banks[g][:, :], lhsT=Yb[:, i, g, 128:256], rhs=tb_[:, ii, 1, :], start=False, stop=(i == NT - 1)),
                            reads=[Yb.p(i), tb_.p()], writes=[banks[g].p()])
            precast_issue(4)
            sp_ = specT[kb % 2]
            fo_ = fourb[kb % 2]
            for g in range(4):
                if g % 2 == 0:
                    P.op("act", lambda e, g=g, sp_=sp_: e.activation(out=sp_[:, g, :], in_=banks[g][:, :], func=AF.Copy,
                                                                    scale=ORTHO), reads=[banks[g].p()], writes=[sp_.p()])
                else:
                    P.op("dve", lambda e, g=g, sp_=sp_: e.tensor_scalar(out=sp_[:, g, :], in0=banks[g][:, :], scalar1=ORTHO,
                                                                       scalar2=None, op0=ALU.mult),
                         reads=[banks[g].p()], writes=[sp_.p()])
            for g in range(4):
                pb_ = banks[4 + g]
                P.op("pe", lambda e, g=g, sp_=sp_, pb_=pb_: e.matmul(
                    pb_[:, :], lhsT=wfour[:, g, :], rhs=sp_[:, g, :], start=True, stop=True),
                    reads=[wfour.p(), sp_.p()], writes=[pb_.p()])
                if g % 2 == 0:
                    P.op("act", lambda e, g=g, fo_=fo_, pb_=pb_: e.copy(out=fo_[:, g, :], in_=pb_[:, :]),
                         reads=[pb_.p()], writes=[fo_.p()])
                else:
                    P.op("dve", lambda e, g=g, fo_=fo_, pb_=pb_: e.tensor_copy(out=fo_[:, g, :], in_=pb_[:, :]),
                         reads=[pb_.p()], writes=[fo_.p()])
            P.dma("sp", ds_four[kb % 2], lambda e, fo_=fo_, kb=kb: e.dma_start(
                out=four_s.rearrange("g p t -> p g t")[:, :, kb * 512:(kb + 1) * 512], in_=fo_[:, :, :]), reads=[fo_.p()])
        precast_issue(1000)
        phC.__exit__()
        if "four" in dbg:
            ds_dbg4 = P.dsem("ds_dbg4")
            P.dma("sp", ds_dbg4, lambda e: e.dma_start(out=dbg_d["four"][:, :, :], in_=four_s[:, :, :]))
            P.barrier()
        phD = Phase().__enter__()
        y_v = y_s.rearrange("(i p) c -> p i c", p=128)
        dtsv = lambda q: dts[:, q, :].rearrange("p (i d h) -> p i d h", d=2, h=16)
        ys_tiles = [[Tile("ys%d_%d" % (g, i)) for i in range(NT)] for g in range(4)]

        def interleave(gens, level=0):
            gens = list(gens)
            while gens:
                for gn in list(gens):
                    try:
                        while next(gn) < level:
                            pass
                    except StopIteration:
                        gens.remove(gn)

        yb_v = yb_s.rearrange("(i p) c -> p i c", p=128)

        class GBuf:
            def __init__(self, k):
                self.BT = sb("BT%d" % k, [128, TA], BF16)
                self.CT = sb("CT%d" % k, [128, TA], BF16)
                self.xs_tok = sb("xs_tok%d" % k, [128, NTA, 256], BF16)
                self.B_tok = sb("B_tok%d" % k, [128, NTA, 128], BF16)
                self.ds = P.dsem("ds_g%d" % k)

        class CBuf:
            def __init__(self, c):
                self.kb = [banks[2 * c], banks[2 * c + 1]]
                self.cbm = [sb("cbm%d_%d" % (c, i), [128, 128], BF16) for i in range(2)]
                self.Rb = [sb("Rb%d_%d" % (c, i), [128, 512]) for i in range(2)]
                self.Eb = [sb("Eb%d_%d" % (c, i), [128, 512]) for i in range(1)] * 2
                self.MTb = [sb("MTb%d_%d" % (c, i), [128, 512], BF16) for i in range(2)]
                self.tmpb = [sb("tmpb%d_%d" % (c, i), [128, 256]) for i in range(2)]
                self.youtb = [sb("yout%d_%d" % (c, i), [128, 256]) for i in range(2)]
                self.xdb = [sb("xdb%d_%d" % (c, i), [128, 256], BF16) for i in range(2)]
                self.xddb = [sb("xddb%d_%d" % (c, i), [128, 256], BF16) for i in range(2)]
                self.ds_yout = [P.dsem("ds_yout%d_%d" % (c, i)) for i in range(2)]
                self.h32 = sb("h32_%d" % c, [128, 256])
                self.h16 = sb("h16_%d" % c, [128, 256], BF16)

        gbufs = [GBuf(0), GBuf(1)]
        cbufs = [CBuf(c) for c in range(4)]

        def ssd_dir_chain(gb, cbf, g, d):
            BT, CT, xs_tok, B_tok = gb.BT, gb.CT, gb.xs_tok, gb.B_tok
            kA, kB = cbf.kb
            h32, h16 = cbf.h32, cbf.h16
            hs = slice(4 * g, 4 * g + 4)
            qd = 3 if d == 0 else 5
            P.op("pool", lambda e: e.memset(h32[:, :], 0.0), writes=[h32.p()])
            P.op("pool", lambda e: e.memset(h16[:, :], 0.0), writes=[h16.p()])
            order = list(range(NTA)) if d == 0 else [1, 0] + list(range(NTA - 1, 1, -1))
            m_cb = 0 if d == 0 else 2
            m_R = 0 if d == 0 else 2
            m_L = 1 if d == 0 else 3
            q_incl = 2 if d == 0 else 4
            ydst = y_v if d == 0 else yb_v
            yield 1
            for idx, i in enumerate(order):
                last = idx == len(order) - 1
                tsl = slice(i * 128, (i + 1) * 128)
                par = idx % 2
                xd, xdd = cbf.xdb[par], cbf.xddb[par]
                P.op("pool", lambda e, xd=xd, i=i: e.tensor_tensor(
                    out=xd[:, :].rearrange("p (r c) -> p r c", c=64),
                    in0=xs_tok[:, i, :].rearrange("p (r c) -> p r c", c=64),
                    in1=dtsv(0)[:, i, d, hs].unsqueeze(2).to_broadcast([128, 4, 64]), op=ALU.mult),
                    reads=[xs_tok.p(), dts.p()], writes=[xd.p()])
                if not last:
                    P.op("pool", lambda e, xd=xd, xdd=xdd, i=i: e.tensor_tensor(
                        out=xdd[:, :].rearrange("p (r c) -> p r c", c=64),
                        in0=xd[:, :].rearrange("p (r c) -> p r c", c=64),
                        in1=dtsv(qd)[:, i, d, hs].unsqueeze(2).to_broadcast([128, 4, 64]), op=ALU.mult),
                        reads=[xd.p(), dts.p()], writes=[xdd.p()])
                yield 0
                if i >= 2:
                    cb, R, E, MT, tmp = cbf.cbm[par], cbf.Rb[par], cbf.Eb[par], cbf.MTb[par], cbf.tmpb[par]
                    P.op("pe", lambda e, tsl=tsl: e.matmul(kA[:, 0:128], lhsT=BT[:, tsl], rhs=CT[:, tsl], start=True, stop=True),
                         reads=[BT.p(), CT.p()], writes=[kA.p()])
                    for r in range(4):
                        P.op("act", lambda e, R=R, r=r, i=i: e.activation(
                            out=R[:, r * 128:(r + 1) * 128], in_=masks[:, m_R, :], func=AF.Copy,
                            scale=dtsv(1)[:, i, d, 4 * g + r:4 * g + r + 1]),
                            reads=[masks.p(), dts.p()], writes=[R.p()])
                    yield 0
                    P.op("dve", lambda e, cb=cb: e.tensor_tensor(
                        out=cb[:, :], in0=kA[:, 0:128], in1=masks[:, m_cb, :], op=ALU.mult),
                        reads=[kA.p(), masks.p()], writes=[cb.p()])
                    P.op("pe", lambda e, R=R: e.matmul(kB[:, :], lhsT=masks[:, m_L, :], rhs=R[:, :], start=True, stop=True),
                         reads=[masks.p(), R.p()], writes=[kB.p()])
                    yield 0
                    P.op("act", lambda e, E=E: e.activation(out=E[:, :], in_=kB[:, :], func=AF.Exp),
                         reads=[kB.p()], writes=[E.p()])
                    P.op("pe", lambda e, tsl=tsl: e.matmul(kA[:, 128:384], lhsT=CT[:, tsl], rhs=h16[:, :], start=True, stop=True),
                         reads=[CT.p(), h16.p()], writes=[kA.p()])
                    yield 0
                    P.op("dve", lambda e, E=E, MT=MT, cb=cb: e.tensor_tensor(
                        out=MT[:, :].rearrange("p (r l) -> p r l", l=128),
                        in0=E[:, :].rearrange("p (r l) -> p r l", l=128),
                        in1=cb[:, :].unsqueeze(1).to_broadcast([128, 4, 128]), op=ALU.mult),
                        reads=[E.p(), cb.p()], writes=[MT.p()])
                    P.op("dve", lambda e, tmp=tmp, i=i: e.tensor_tensor(
                        out=tmp[:, :].rearrange("p (r c) -> p r c", c=64),
                        in0=kA[:, 128:384].rearrange("p (r c) -> p r c", c=64),
                        in1=dtsv(q_incl)[:, i, d, hs].unsqueeze(2).to_broadcast([128, 4, 64]), op=ALU.mult),
                        reads=[kA.p(), dts.p()], writes=[tmp.p()])
                    yield 0
                    for r in range(4):
                        P.op("pe", lambda e, MT=MT, r=r, xd=xd: e.matmul(
                            kA[:, r * 64:(r + 1) * 64], lhsT=MT[:, r * 128:(r + 1) * 128],
                            rhs=xd[:, r * 64:(r + 1) * 64], start=True, stop=True),
                            reads=[MT.p(), xd.p()], writes=[kA.p()])
                    yield 0
                    yo = cbf.youtb[par]
                    P.op("dve", lambda e, tmp=tmp, yo=yo: e.tensor_tensor(
                        out=yo[:, :], in0=kA[:, 0:256], in1=tmp[:, :], op=ALU.add),
                        reads=[kA.p(), tmp.p()], writes=[yo.p()])
                    if d == 1:
                        yield 0
                        P.op("pool", lambda e, tmp=tmp, i=i: e.tensor_tensor(
                            out=tmp[:, :].rearrange("p (r c) -> p r c", c=64),
                            in0=xs_tok[:, i, :].rearrange("p (r c) -> p r c", c=64),
                            in1=rp[:, RP_DSKIP + 4 * g:RP_DSKIP + 4 * g + 4].unsqueeze(2).to_broadcast([128, 4, 64]),
                            op=ALU.mult),
                            reads=[xs_tok.p(), rp.p()], writes=[tmp.p()])
                        P.op("pool", lambda e, yo=yo, tmp=tmp: e.tensor_tensor(
                            out=yo[:, :], in0=yo[:, :], in1=tmp[:, :], op=ALU.add),
                            reads=[tmp.p(), yo.p()], writes=[yo.p()])
                    P.dma("sp", cbf.ds_yout[par], lambda e, yo=yo, i=i: e.dma_start(
                        out=ydst[:, i - 2, 256 * g:256 * g + 256], in_=yo[:, :]), reads=[yo.p()])
                    yield 0
                if not last:
                    P.op("pe", lambda e, i=i, xdd=xdd: e.matmul(
                        kB[:, 0:256], lhsT=B_tok[:, i, :], rhs=xdd[:, :], start=True, stop=True),
                        reads=[B_tok.p(), xdd.p()], writes=[kB.p()])
                    P.op("dve", lambda e, i=i: e.tensor_tensor(
                        out=h32[:, :].rearrange("p (r c) -> p r c", c=64),
                        in0=h32[:, :].rearrange("p (r c) -> p r c", c=64),
                        in1=dtsv(6)[:, i, d, hs].unsqueeze(2).to_broadcast([128, 4, 64]), op=ALU.mult),
                        reads=[h32.p(), dts.p()], writes=[h32.p()])
                    yield 0
                    P.op("dve", lambda e: e.tensor_tensor(out=h32[:, :], in0=h32[:, :], in1=kB[:, 0:256], op=ALU.add),
                         reads=[h32.p(), kB.p()], writes=[h32.p()])
                    P.op("act", lambda e: e.copy(out=h16[:, :], in_=h32[:, :]), reads=[h32.p()], writes=[h16.p()])
                yield 1

        for rnd in range(2):
            chains = []
            for k in range(2):
                g = 2 * rnd + k
                gb = gbufs[k]
                P.dma("sp", gb.ds, lambda e, gb=gb, g=g: e.dma_start(out=gb.BT[:, :], in_=bc_s[g, :, :]), writes=[gb.BT.p()])
                P.dma("sp", gb.ds, lambda e, gb=gb, g=g: e.dma_start(out=gb.CT[:, :], in_=bc_s[4 + g, :, :]), writes=[gb.CT.p()])
                P.dma("sp", gb.ds, lambda e, gb=gb, g=g: e.dma_start(out=gb.xs_tok[:, :, :], in_=xs_v[:, :, 256 * g:256 * g + 256]),
                      writes=[gb.xs_tok.p()])
                P.dma("sp", gb.ds, lambda e, gb=gb, g=g: e.dma_start(out=gb.B_tok[:, :, :], in_=bt_v[:, :, 128 * g:128 * g + 128]),
                      writes=[gb.B_tok.p()])
                for d in range(2):
                    chains.append(ssd_dir_chain(gb, cbufs[2 * k + d], g, d))
            interleave(chains)
        phD.__exit__()
        phX.__exit__()
        if "ssd" in dbg:
            ds_dbg3 = P.dsem("ds_dbg3")
            P.dma("sp", ds_dbg3, lambda e: e.dma_start(out=dbg_d["y"][:, :], in_=y_s[:, :]))
            P.dma("sp", ds_dbg3, lambda e: e.dma_start(out=dbg_d["yb"][:, :], in_=yb_s[:, :]))
            P.barrier()

        BS = 512
        NB = 48
        I32 = mybir.dt.int32
        gates = sb("gates", [128, 2, NT])
        sloti = sb("sloti", [128, 2, NT], I32)
        widx = sb("widx", [128, 2, NB], I32)
        hc = sb("hc", [128, 128])
        masksE = sb("masksE", [128, 2, 128])
        ds_c2 = P.dsem("ds_c2")
        P.dma("sp", ds_c2, lambda e: e.dma_start(out=hc[:, :], in_=hc_d[:, :]), writes=[hc.p()])
        P.dma("sp", ds_c2, lambda e: e.dma_start(out=masksE[:, :, :], in_=masks_d[:, 3:5, :]), writes=[masksE.p()])
        phE = Phase().__enter__()
        lgall = sb("lgall", [128, NT, 36])
        phE1 = Phase().__enter__()
        wz = sb("wz", [128, KC, 1024], BF16)
        wout = sb("wout", [128, 12, 1024], BF16)
        four_sb = sb("four_sb", [128, 4, T], BF16)
        wr = sb("wr", [128, KC, 36])
        junk2 = sb("junk2", [128, D])
        ds_e = P.dsem("ds_e")
        ds_e2 = P.dsem("ds_e2")
        P.dma("pool", ds_e2, lambda e: e.dma_start(out=wz[:, :, :], in_=w_in_v[:, :, 0:1024]), writes=[wz.p()])
        P.dma("pool", ds_e2, lambda e: e.dma_start(out=wout[:, :, :], in_=w_out_d.rearrange("(k p) d -> p k d", p=128)),
              writes=[wout.p()])
        P.dma("sp", ds_e, lambda e: e.dma_start(out=four_sb[:, :, :], in_=four_s.rearrange("g p t -> p g t")),
              writes=[four_sb.p()])
        P.dma("sp", ds_e, lambda e: e.dma_start(out=wr[:, :, :], in_=wr_d[:, :, :]), writes=[wr.p()])

        def e_chain(ch):
            kb = banks[4 * ch:4 * ch + 4]
            xb = sb("xt2_%d" % ch, [128, D])
            yb = sb("yt2_%d" % ch, [128, D])
            yb3 = sb("yt3_%d" % ch, [128, D])
            ds_y3 = P.dsem("ds_y3_%d" % ch)
            ds_x2 = P.dsem("ds_x2_%d" % ch)
            ds_y2 = P.dsem("ds_y2_%d" % ch)
            xn2 = sb("xn2_%d" % ch, [128, D], BF16)
            utmp2 = sb("utmp2_%d" % ch, [128, KC, 128])
            uTt = sb("uTt%d" % ch, [128, KC, 128], BF16)
            sz = sb("sz%d" % ch, [128, D])
            yz = sb("yz%d" % ch, [128, D])
            yzb = sb("yzb%d" % ch, [128, D], BF16)
            catT = sb("catT%d" % ch, [128, KC, 128], BF16)
            x1b = sb("x1t%d" % ch, [128, D])
            ds_x1 = P.dsem("ds_x1_%d" % ch)
            hn = sb("hn%d" % ch, [128, D])
            hT32 = sb("hT32_%d" % ch, [128, KC, 128])
            hb2 = sb("hTb%d" % ch, [128, D], BF16)
            ds_hT = P.dsem("ds_hT%d" % ch)
            sse = sb("sse%d" % ch, [128, 4])
            yield 1
            for i in range(ch, NT, 2):
                rows = slice(i * 128, (i + 1) * 128)
                P.dma("sp", ds_x2, lambda e, rows=rows: e.dma_start(out=xb[:, :], in_=x_d[rows, :]), writes=[xb.p()])
                P.dma("sp", ds_y2, lambda e, rows=rows: e.dma_start(out=yb[:, :], in_=y_s[rows, :]), writes=[yb.p()])
                P.dma("sp", ds_y3, lambda e, rows=rows: e.dma_start(out=yb3[:, :], in_=yb_s[rows, :]), writes=[yb3.p()])
                P.op("pool", lambda e: e.tensor_tensor(out=yb[:, :], in0=yb[:, :], in1=yb3[:, :], op=ALU.add),
                     reads=[yb.p(), yb3.p()], writes=[yb.p()])
                P.op("act", lambda e, i=i: e.activation(out=xn2[:, :], in_=xb[:, :], func=AF.Copy, scale=rs[:, i + 2:i + 3]),
                     reads=[xb.p(), rs.p()], writes=[xn2.p()])
                yield 0
                pt = kb[0]
                ptb = pt.t[:, :].bitcast(BF16)
                for kc in range(KC):
                    P.op("pe", lambda e, ptb=ptb, kc=kc: e.transpose(
                        ptb[:, kc * 128:(kc + 1) * 128], xn2[:, kc * 128:(kc + 1) * 128], ident_b[:, :]),
                        reads=[xn2.p(), ident_b.p()], writes=[pt.p()])
                yield 0
                P.op("dve", lambda e, ptb=ptb: e.tensor_tensor(
                    out=utmp2[:, :, :], in0=ptb.rearrange("p (k t) -> p k t", k=KC),
                    in1=scA[:, :, 0:1].to_broadcast([128, KC, 128]), op=ALU.mult),
                    reads=[pt.p(), scA.p()], writes=[utmp2.p()])
                yield 0
                P.op("pool", lambda e: e.tensor_tensor(
                    out=uTt[:, :, :], in0=utmp2[:, :, :], in1=modT[:, 0:8, 0:1].to_broadcast([128, KC, 128]), op=ALU.add),
                    reads=[utmp2.p(), modT.p()], writes=[uTt.p()])
                yield 0
                for nb in range(2):
                    zb = kb[1 + nb]
                    for kc in range(KC):
                        P.op("pe", lambda e, zb=zb, kc=kc, nb=nb: e.matmul(
                            zb[:, :], lhsT=uTt[:, kc, :], rhs=wz[:, kc, nb * 512:(nb + 1) * 512],
                            start=(kc == 0), stop=(kc == KC - 1)), reads=[uTt.p(), wz.p()], writes=[zb.p()])
                    yield 0
                    P.op("act", lambda e, zb=zb, nb=nb: e.activation(out=sz[:, nb * 512:(nb + 1) * 512], in_=zb[:, :], func=AF.Silu),
                         reads=[zb.p()], writes=[sz.p()])
                yield 0
                P.op("dve", lambda e: e.tensor_tensor(out=yz[:, :], in0=yb[:, :], in1=sz[:, :], op=ALU.mult),
                     reads=[yb.p(), sz.p()], writes=[yz.p()])
                yield 0
                P.op("act", lambda e: e.activation(out=junk2[:, :], in_=yz[:, :], func=AF.Square, accum_out=sse[:, 0:1]),
                     reads=[yz.p()], writes=[junk2.p(), sse.p()])
                P.op("act", lambda e: e.activation(out=sse[:, 1:2], in_=sse[:, 0:1], func=AF.Sqrt, scale=1.0 / D, bias=epsb[:, 0:1]),
                     reads=[sse.p(), epsb.p()], writes=[sse.p()])
                yield 0
                P.op("dve", lambda e: e.reciprocal(out=sse[:, 1:2], in_=sse[:, 1:2]), reads=[sse.p()], writes=[sse.p()])
                yield 0
                P.op("act", lambda e: e.activation(out=yzb[:, :], in_=yz[:, :], func=AF.Copy, scale=sse[:, 1:2]),
                     reads=[yz.p(), sse.p()], writes=[yzb.p()])
                yield 0
                for kc in range(KC):
                    P.op("pe", lambda e, ptb=ptb, kc=kc: e.transpose(
                        ptb[:, kc * 128:(kc + 1) * 128], yzb[:, kc * 128:(kc + 1) * 128], ident_b[:, :]),
                        reads=[yzb.p(), ident_b.p()], writes=[pt.p()])
                yield 0
                P.op("dve", lambda e, ptb=ptb: e.tensor_tensor(
                    out=catT[:, :, :], in0=ptb.rearrange("p (k t) -> p k t", k=KC),
                    in1=pp[:, 64:72].unsqueeze(2).to_broadcast([128, KC, 128]), op=ALU.mult),
                    reads=[pt.p(), pp.p()], writes=[catT.p()])
                yield 0
                for nb in range(2):
                    mb = kb[1 + nb]
                    for k in range(12):
                        lh = (lambda k=k: catT[:, k, :]) if k < 8 else (lambda k=k, i=i: four_sb[:, k - 8, i * 128:(i + 1) * 128])
                        P.op("pe", lambda e, mb=mb, k=k, nb=nb, lh=lh: e.matmul(
                            mb[:, :], lhsT=lh(), rhs=wout[:, k, nb * 512:(nb + 1) * 512], start=(k == 0), stop=(k == 11)),
                            reads=[catT.p(), four_sb.p(), wout.p()], writes=[mb.p()])
                    yield 0
                    P.op("dve", lambda e, mb=mb, nb=nb: e.tensor_tensor(
                        out=x1b[:, nb * 512:(nb + 1) * 512], in0=mb[:, :], in1=g12[:, 0, nb * 512:(nb + 1) * 512], op=ALU.mult),
                        reads=[mb.p(), g12.p()], writes=[x1b.p()])
                yield 0
                P.op("pool", lambda e: e.tensor_tensor(out=x1b[:, :], in0=x1b[:, :], in1=xb[:, :], op=ALU.add),
                     reads=[x1b.p(), xb.p()], writes=[x1b.p()])
                yield 0
                P.dma("sp", ds_x1, lambda e, rows=rows: e.dma_start(out=x1_s[rows, :], in_=x1b[:, :]), reads=[x1b.p()])
                P.op("act", lambda e: e.activation(out=junk2[:, :], in_=x1b[:, :], func=AF.Square, accum_out=sse[:, 2:3]),
                     reads=[x1b.p()], writes=[junk2.p(), sse.p()])
                P.op("act", lambda e: e.activation(out=sse[:, 3:4], in_=sse[:, 2:3], func=AF.Sqrt, scale=1.0 / D, bias=epsb[:, 0:1]),
                     reads=[sse.p(), epsb.p()], writes=[sse.p()])
                yield 0
                P.op("dve", lambda e: e.reciprocal(out=sse[:, 3:4], in_=sse[:, 3:4]), reads=[sse.p()], writes=[sse.p()])
                yield 0
                P.op("act", lambda e: e.activation(out=hn[:, :], in_=x1b[:, :], func=AF.Copy, scale=sse[:, 3:4]),
                     reads=[x1b.p(), sse.p()], writes=[hn.p()])
                yield 0
                P.op("dve", lambda e: e.tensor_tensor(out=hn[:, :], in0=hn[:, :], in1=g12[:, 3, :], op=ALU.mult),
                     reads=[hn.p(), g12.p()], writes=[hn.p()])
                yield 0
                P.op("pool", lambda e: e.tensor_tensor(out=hn[:, :], in0=hn[:, :], in1=g12[:, 2, :], op=ALU.add),
                     reads=[hn.p(), g12.p()], writes=[hn.p()])
                yield 0
                P.op("act", lambda e: e.copy(out=hb2[:, :], in_=hn[:, :]), reads=[hn.p()], writes=[hb2.p()])
                P.dma("sp", ds_hT, lambda e, rows=rows: e.dma_start(out=h_s[rows, :], in_=hb2[:, :]), reads=[hb2.p()])
                for h2 in range(2):
                    hb_ = kb[1 + h2]
                    for k4 in range(4):
                        kc = 4 * h2 + k4
                        P.op("pe", lambda e, hb_=hb_, k4=k4, kc=kc: e.transpose(
                            hb_[:, k4 * 128:(k4 + 1) * 128], hn[:, kc * 128:(kc + 1) * 128], ident_f[:, :]),
                            reads=[hn.p(), ident_f.p()], writes=[hb_.p()])
                    yield 0
                    if h2 == 0:
                        P.op("act", lambda e, hb_=hb_, h2=h2: e.copy(
                            out=hT32[:, 4 * h2:4 * h2 + 4, :], in_=hb_[:, :].rearrange("p (k t) -> p k t", k=4)),
                            reads=[hb_.p()], writes=[hT32.p()])
                    else:
                        P.op("dve", lambda e, hb_=hb_, h2=h2: e.tensor_copy(
                            out=hT32[:, 4 * h2:4 * h2 + 4, :], in_=hb_[:, :].rearrange("p (k t) -> p k t", k=4)),
                            reads=[hb_.p()], writes=[hT32.p()])
                yield 0
                lb = kb[3]
                for kc in range(KC):
                    P.op("pe", lambda e, kc=kc, lb=lb: e.matmul(lb[:, 0:36], lhsT=hT32[:, kc, :], rhs=wr[:, kc, :],
                                                              start=(kc == 0), stop=(kc == KC - 1)),
                         reads=[hT32.p(), wr.p()], writes=[lb.p()])
                yield 0
                P.op("dve", lambda e, lb=lb, i=i: e.tensor_copy(out=lgall[:, i, :], in_=lb[:, 0:36]), reads=[lb.p()], writes=[lgall.p()])
                yield 1

        interleave([e_chain(0), e_chain(1)])
        phE1.__exit__()
        if "noroute" not in dbg:
            r_lg = sb("r_lg", [128, NT, 4]); r_mx = sb("r_mx", [128, NT]); r_eg = sb("r_eg", [128, NT, 4])
            r_sg = sb("r_sg", [128, NT]); r_oh = sb("r_oh", [128, NT, 4]); r_le = sb("r_le", [128, NT, 4, 8])
            r_sel = sb("r_sel", [128, NT, 8]); r_m1 = sb("r_m1", [128, NT]); r_o1 = sb("r_o1", [128, NT, 8])
            r_s2 = sb("r_s2", [128, NT, 8]); r_m2 = sb("r_m2", [128, NT]); r_o2 = sb("r_o2", [128, NT, 8])
            r_e2 = sb("r_e2", [128, NT])
            OH1 = sb("OH1", [128, NT, 32]); OH2 = sb("OH2", [128, NT, 32]); Asum = sb("Asum", [128, NT * 32])
            rank = sb("rank", [128, NT, 32]); TTb = sb("TTb", [128, NT, 32]); PTb = sb("PTb", [128, NT, 32])
            cnt = sb("cnt", [128, 32]); cmpj = sb("cmpj", [128, 32, 16]); nblk = sb("nblk", [128, 32])
            sblk = sb("sblk", [128, 32]); eblk = sb("eblk", [128, 32]); cmpb = sb("cmpb", [128, NB, 32])
            ebf = sb("ebf", [128, NB]); tmpr = sb("tmpr", [128, NT, 32]); slotf = sb("slotf", [128, 2, NT])
            RT = [lgall, r_lg, r_mx, r_eg, r_sg, r_oh, r_le, r_sel, r_m1, r_o1, r_s2, r_m2, r_o2, r_e2, rp,
                  OH1, OH2, Asum, rank, TTb, PTb, cnt, cmpj, nblk, sblk, eblk, cmpb, ebf, tmpr, slotf, gates, sloti, widx, hc]
            rt = [b.p() for b in RT]

            rmax = [int(x[5:]) for x in dbg if x.startswith("rmax:")]
            rmax = rmax[0] if rmax else 10 ** 9
            rcnt = [0]

            def V(fn):
                rcnt[0] += 1
                if rcnt[0] <= rmax:
                    P.op("dve", fn, reads=rt, writes=rt)

            def A(fn):
                rcnt[0] += 1
                if rcnt[0] <= rmax:
                    P.op("act", fn, reads=rt, writes=rt)
            bc3 = lambda ap, n: ap.unsqueeze(2).to_broadcast([128, NT, n])
            V(lambda e: e.tensor_tensor(out=r_lg[:, :, :], in0=lgall[:, :, 0:4],
                                        in1=rp[:, 80:84].unsqueeze(1).to_broadcast([128, NT, 4]), op=ALU.add))
            V(lambda e: e.tensor_reduce(out=r_mx[:, :], in_=r_lg[:, :, :], axis=AX.X, op=ALU.max))
            V(lambda e: e.tensor_tensor(out=r_eg[:, :, :], in0=r_lg[:, :, :], in1=bc3(r_mx[:, :], 4), op=ALU.subtract))
            V(lambda e: e.tensor_tensor(out=r_oh[:, :, :], in0=r_lg[:, :, :], in1=bc3(r_mx[:, :], 4), op=ALU.is_equal))
            A(lambda e: e.activation(out=r_eg[:, :, :], in_=r_eg[:, :, :], func=AF.Exp))
            V(lambda e: e.tensor_reduce(out=r_sg[:, :], in_=r_eg[:, :, :], axis=AX.X, op=ALU.add))
            V(lambda e: e.reciprocal(out=r_sg[:, :], in_=r_sg[:, :]))
            V(lambda e: e.tensor_tensor(out=r_le[:, :, :, :], in0=lgall[:, :, 4:36].rearrange("p t (g x) -> p t g x", x=8),
                                        in1=rp[:, 84:116].rearrange("p (g x) -> p g x", x=8).unsqueeze(1).to_broadcast([128, NT, 4, 8]),
                                        op=ALU.add))
            V(lambda e: e.tensor_tensor(out=r_le[:, :, :, :], in0=r_le[:, :, :, :],
                                        in1=r_oh[:, :, :].unsqueeze(3).to_broadcast([128, NT, 4, 8]), op=ALU.mult))
            V(lambda e: e.tensor_reduce(out=r_sel[:, :, :], in_=r_le[:, :, :, :].rearrange("p t g x -> p t x g"), axis=AX.X, op=ALU.add))
            V(lambda e: e.tensor_reduce(out=r_m1[:, :], in_=r_sel[:, :, :], axis=AX.X, op=ALU.max))
            V(lambda e: e.tensor_tensor(out=r_o1[:, :, :], in0=r_sel[:, :, :], in1=bc3(r_m1[:, :], 8), op=ALU.is_equal))
            V(lambda e: e.scalar_tensor_tensor(out=r_s2[:, :, :], in0=r_o1[:, :, :], scalar=-1.0e30, in1=r_sel[:, :, :],
                                               op0=ALU.mult, op1=ALU.add))
            V(lambda e: e.tensor_reduce(out=r_m2[:, :], in_=r_s2[:, :, :], axis=AX.X, op=ALU.max))
            V(lambda e: e.tensor_tensor(out=r_o2[:, :, :], in0=r_s2[:, :, :], in1=bc3(r_m2[:, :], 8), op=ALU.is_equal))
            V(lambda e: e.tensor_tensor(out=r_e2[:, :], in0=r_m2[:, :], in1=r_m1[:, :], op=ALU.subtract))
            A(lambda e: e.activation(out=r_e2[:, :], in_=r_e2[:, :], func=AF.Exp))
            V(lambda e: e.tensor_scalar(out=gates[:, 0, :], in0=r_e2[:, :], scalar1=1.0, scalar2=None, op0=ALU.add))
            V(lambda e: e.reciprocal(out=gates[:, 0, :], in_=gates[:, 0, :]))
            V(lambda e: e.tensor_tensor(out=gates[:, 0, :], in0=gates[:, 0, :], in1=r_sg[:, :], op=ALU.mult))
            V(lambda e: e.tensor_tensor(out=gates[:, 1, :], in0=gates[:, 0, :], in1=r_e2[:, :], op=ALU.mult))
            V(lambda e: e.tensor_tensor(out=OH1[:, :, :].rearrange("p t (g x) -> p t g x", x=8),
                                        in0=r_o1[:, :, :].unsqueeze(2).to_broadcast([128, NT, 4, 8]),
                                        in1=r_oh[:, :, :].unsqueeze(3).to_broadcast([128, NT, 4, 8]), op=ALU.mult))
            V(lambda e: e.tensor_tensor(out=OH2[:, :, :].rearrange("p t (g x) -> p t g x", x=8),
                                        in0=r_o2[:, :, :].unsqueeze(2).to_broadcast([128, NT, 4, 8]),
                                        in1=r_oh[:, :, :].unsqueeze(3).to_broadcast([128, NT, 4, 8]), op=ALU.mult))
            V(lambda e: e.tensor_tensor(out=Asum[:, :], in0=OH1[:, :, :].rearrange("p t e -> p (t e)"),
                                        in1=OH2[:, :, :].rearrange("p t e -> p (t e)"), op=ALU.add))
            for hf in range(2):
                P.op("pe", lambda e, hf=hf: e.matmul(banks[hf][:, :], lhsT=masksE[:, 0, :], rhs=Asum[:, hf * 512:(hf + 1) * 512],
                                                     start=True, stop=True), reads=rt + [masksE.p()], writes=[banks[hf].p()])
                P.op("pe", lambda e, hf=hf: e.matmul(banks[2 + hf][:, :], lhsT=masksE[:, 1, :], rhs=Asum[:, hf * 512:(hf + 1) * 512],
                                                     start=True, stop=True), reads=rt + [masksE.p()], writes=[banks[2 + hf].p()])
                P.op("dve", lambda e, hf=hf: e.tensor_copy(out=rank[:, hf * 16:(hf + 1) * 16, :],
                                                           in_=banks[hf][:, :].rearrange("p (t e) -> p t e", e=32)),
                     reads=[banks[hf].p()] + rt, writes=rt)
                P.op("dve", lambda e, hf=hf: e.tensor_copy(out=TTb[:, hf * 16:(hf + 1) * 16, :],
                                                           in_=banks[2 + hf][:, :].rearrange("p (t e) -> p t e", e=32)),
                     reads=[banks[2 + hf].p()] + rt, writes=rt)
            V(lambda e: e.memset(PTb[:, 0, :], 0.0))
            for ti in range(1, NT):
                V(lambda e, ti=ti: e.tensor_tensor(out=PTb[:, ti, :], in0=PTb[:, ti - 1, :], in1=TTb[:, ti - 1, :], op=ALU.add))
            V(lambda e: e.tensor_tensor(out=cnt[:, :], in0=PTb[:, NT - 1, :], in1=TTb[:, NT - 1, :], op=ALU.add))
            V(lambda e: e.tensor_tensor(out=rank[:, :, :], in0=rank[:, :, :], in1=PTb[:, :, :], op=ALU.add))
            V(lambda e: e.tensor_tensor(out=cmpj[:, :, :], in0=cnt[:, :].unsqueeze(2).to_broadcast([128, 32, 16]),
                                        in1=hc[:, 0:16].unsqueeze(1).to_broadcast([128, 32, 16]), op=ALU.is_gt))
            V(lambda e: e.tensor_reduce(out=nblk[:, :], in_=cmpj[:, :, :], axis=AX.X, op=ALU.add))
            V(lambda e: e.memset(sblk[:, 0:1], 0.0))
            for ex in range(1, 32):
                V(lambda e, ex=ex: e.tensor_tensor(out=sblk[:, ex:ex + 1], in0=sblk[:, ex - 1:ex], in1=nblk[:, ex - 1:ex], op=ALU.add))
            V(lambda e: e.tensor_tensor(out=eblk[:, :], in0=sblk[:, :], in1=nblk[:, :], op=ALU.add))
            V(lambda e: e.tensor_tensor(out=cmpb[:, :, :], in0=eblk[:, :].unsqueeze(1).to_broadcast([128, NB, 32]),
                                        in1=hc[:, 16:16 + NB].unsqueeze(2).to_broadcast([128, NB, 32]), op=ALU.is_le))
            V(lambda e: e.tensor_reduce(out=ebf[:, :], in_=cmpb[:, :, :], axis=AX.X, op=ALU.add))
            V(lambda e: e.tensor_scalar(out=ebf[:, :], in0=ebf[:, :], scalar1=31.0, scalar2=128.0, op0=ALU.min, op1=ALU.mult))
            V(lambda e: e.tensor_tensor(out=ebf[:, :], in0=ebf[:, :], in1=hc[:, 80:81].to_broadcast([128, NB]), op=ALU.add))
            V(lambda e: e.tensor_copy(out=widx[:, 0, :], in_=ebf[:, :]))
            V(lambda e: e.tensor_scalar(out=ebf[:, :], in0=ebf[:, :], scalar1=1.0, scalar2=None, op0=ALU.add))
            V(lambda e: e.tensor_copy(out=widx[:, 1, :], in_=ebf[:, :]))
            V(lambda e: e.tensor_scalar(out=sblk[:, :], in0=sblk[:, :], scalar1=float(BS), scalar2=None, op0=ALU.mult))
            V(lambda e: e.tensor_tensor(out=rank[:, :, :], in0=rank[:, :, :],
                                        in1=sblk[:, :].unsqueeze(1).to_broadcast([128, NT, 32]), op=ALU.add))
            for j, OH in enumerate((OH1, OH2)):
                V(lambda e, OH=OH: e.tensor_tensor(out=tmpr[:, :, :], in0=rank[:, :, :], in1=OH[:, :, :], op=ALU.mult))
                V(lambda e, j=j: e.tensor_reduce(out=slotf[:, j, :], in_=tmpr[:, :, :], axis=AX.X, op=ALU.add))
            V(lambda e: e.tensor_copy(out=sloti[:, 0, :], in_=slotf[:, 0, :]))
            V(lambda e: e.tensor_copy(out=sloti[:, 1, :], in_=slotf[:, 1, :]))
        if "x1" in dbg:
            P.barrier()
            ds_dbg5 = P.dsem("ds_dbg5")
            P.dma("sp", ds_dbg5, lambda e: e.dma_start(out=dbg_d["x1"][:, :], in_=x1_s[:, :]))
            P.dma("sp", ds_dbg5, lambda e: e.dma_start(out=dbg_d["lg"][:, :, :], in_=lgall[:, :, :]), reads=[lgall.p()])
            P.dma("sp", ds_dbg5, lambda e: e.dma_start(out=dbg_d["slot"][:, :, :], in_=sloti[:, :, :]), reads=[sloti.p()])
            P.dma("sp", ds_dbg5, lambda e: e.dma_start(out=dbg_d["widx"][:, :, :], in_=widx[:, :, :]), reads=[widx.p()])
            P.dma("sp", ds_dbg5, lambda e: e.dma_start(out=dbg_d["gates"][:, :, :], in_=gates[:, :, :]), reads=[gates.p()])
            if "noroute" not in dbg:
                P.dma("sp", ds_dbg5, lambda e: e.dma_start(out=dbg_d["slotf"][:, :, :], in_=slotf[:, :, :]), reads=[slotf.p()])
        phE.__exit__()

        if "nomoe" not in dbg:
            phF1 = Phase().__enter__()
            tokid = sb("tokid", [128, NT, 2], mybir.dt.int32)
            ds_tk = P.dsem("ds_tk")
            ds_sc = P.dsem("ds_sc")
            t_rowtok = Tile("rowtok")
            P.dma("sp", ds_tk, lambda e: e.dma_start(out=tokid[:, :, :], in_=tokid_d[:, :, :]), writes=[tokid.p()])
            for i in range(NT):
                for j in range(2):
                    P.dma("pool", ds_sc, lambda e, i=i, j=j: e.indirect_dma_start(
                        out=rowtok[:, :], out_offset=bass.IndirectOffsetOnAxis(ap=sloti[:, j, i:i + 1], axis=0),
                        in_=tokid[:, i, :], in_offset=None), reads=[tokid.p(), sloti.p(), t_hs0], writes=[t_rowtok])
            phF1.__exit__()
            phF2 = Phase().__enter__()
            weg_v, weu_v, wed_v = wgs, wus, wds
            rt_v = rowtok.rearrange("(b s p) two -> b p s two", s=4, p=128)
            ys_v = y_sorted.rearrange("(b s p) d -> b p s d", s=4, p=128)

            def f2_chain(ch):
                kb = banks[4 * ch:4 * ch + 4]
                Wg = [sb("Wg%d_%d" % (ch, i), [128, KC * 512], BF16) for i in range(2)]
                Wu = [sb("Wu%d_%d" % (ch, i), [128, KC * 512], BF16) for i in range(2)]
                Wd = [sb("Wd%d_%d" % (ch, i), [128, 4 * D], BF16) for i in range(2)]
                ds_w = [P.dsem("ds_w%d_%d" % (ch, i)) for i in range(2)]
                hsblk = [sb("hsblk%d_%d" % (ch, i), [128, 4, D], BF16) for i in range(2)]
                ds_hb = [P.dsem("ds_hblk%d_%d" % (ch, i)) for i in range(2)]
                idxb = [sb("idxb%d_%d" % (ch, i), [128, 4, 2], mybir.dt.int32) for i in range(2)]
                ds_ib = [P.dsem("ds_ib%d_%d" % (ch, i)) for i in range(2)]
                hTblk = sb("hTblk%d" % ch, [128, KC, BS], BF16)
                actT = sb("actT%d" % ch, [128, 4, BS], BF16, nparts=4)
                sgb = [sb("sgb%d_%d" % (ch, i), [128, 512]) for i in range(2)]
                yblk = [sb("yblk%d_%d" % (ch, i), [128, D]) for i in range(2)]
                ds_yb = [P.dsem("ds_yb%d_%d" % (ch, i)) for i in range(2)]
                blocks = list(range(ch, NB, 2))

                def fetch(n):
                    b = blocks[n]
                    par = n % 2
                    for (wt, src) in ((Wg[par], weg_v), (Wu[par], weu_v), (Wd[par], wed_v)):
                        P.dma("pool", ds_w[par], lambda e, wt=wt, src=src, b=b: e.indirect_dma_start(
                            out=wt[:, :], out_offset=None, in_=src[:, :],
                            in_offset=bass.IndirectOffsetOnAxis(ap=widx[:, 0, b:b + 1], axis=0)),
                            reads=[widx.p(), t_wcast], writes=[wt.p()])
                    hb_ = hsblk[par]
                    ib_ = idxb[par]
                    P.dma("sp", ds_ib[par], lambda e, ib_=ib_, b=b: e.dma_start(out=ib_[:, :, :], in_=rt_v[b]),
                          reads=[t_rowtok], writes=[ib_.p()])
                    for s_ in range(4):
                        P.dma("pool", ds_hb[par], lambda e, hb_=hb_, ib_=ib_, s_=s_: e.indirect_dma_start(
                            out=hb_[:, s_, :], out_offset=None, in_=h_s[:, :],
                            in_offset=bass.IndirectOffsetOnAxis(ap=ib_[:, s_, 0:1], axis=0)),
                            reads=[ib_.p()], writes=[hb_.p()])
                fetch(0)
                yield 1
                for n, b in enumerate(blocks):
                    par = n % 2
                    if n + 1 < len(blocks):
                        fetch(n + 1)
                    wg, wu, wd, hb_ = Wg[par], Wu[par], Wd[par], hsblk[par]
                    for j2 in range(4):
                        pt = kb[j2]
                        ptb = pt.t[:, :].bitcast(BF16)
                        for kk in range(2):
                            kc = 2 * j2 + kk
                            for s_ in range(4):
                                P.op("pe", lambda e, ptb=ptb, hb_=hb_, kk=kk, kc=kc, s_=s_: e.transpose(
                                    ptb[:, kk * 512 + s_ * 128:kk * 512 + (s_ + 1) * 128], hb_[:, s_, kc * 128:(kc + 1) * 128],
                                    ident_b[:, :]), reads=[hb_.p(), ident_b.p()], writes=[pt.p()])
                        if j2 % 2 == 0:
                            P.op("act", lambda e, ptb=ptb, j2=j2: e.copy(
                                out=hTblk[:, 2 * j2:2 * j2 + 2, :], in_=ptb.rearrange("p (k t) -> p k t", k=2)),
                                reads=[pt.p()], writes=[hTblk.p()])
                        else:
                            P.op("dve", lambda e, ptb=ptb, j2=j2: e.tensor_copy(
                                out=hTblk[:, 2 * j2:2 * j2 + 2, :], in_=ptb.rearrange("p (k t) -> p k t", k=2)),
                                reads=[pt.p()], writes=[hTblk.p()])
                        yield 0
                    for fc in range(4):
                        pg, pu = kb[fc % 2], kb[2 + fc % 2]
                        sg_ = sgb[fc % 2]
                        for kc in range(KC):
                            P.op("pe", lambda e, pg=pg, wg=wg, kc=kc, fc=fc: e.matmul(
                                pg[:, :], lhsT=wg[:, kc * 512 + fc * 128:kc * 512 + (fc + 1) * 128], rhs=hTblk[:, kc, :],
                                start=(kc == 0), stop=(kc == KC - 1)), reads=[wg.p(), hTblk.p()], writes=[pg.p()])
                        yield 0
                        for kc in range(KC):
                            P.op("pe", lambda e, pu=pu, wu=wu, kc=kc, fc=fc: e.matmul(
                                pu[:, :], lhsT=wu[:, kc * 512 + fc * 128:kc * 512 + (fc + 1) * 128], rhs=hTblk[:, kc, :],
                                start=(kc == 0), stop=(kc == KC - 1)), reads=[wu.p(), hTblk.p()], writes=[pu.p()])
                        P.op("act", lambda e, pg=pg, sg_=sg_: e.activation(out=sg_[:, :], in_=pg[:, :], func=AF.Silu),
                             reads=[pg.p()], writes=[sg_.p()])
                        yield 0
                        P.op("dve", lambda e, pu=pu, sg_=sg_, fc=fc: e.tensor_tensor(
                            out=actT[:, fc, :], in0=pu[:, :], in1=sg_[:, :], op=ALU.mult),
                            reads=[pu.p(), sg_.p()], writes=[actT.p(fc)])
                    for s_ in range(4):
                        for nb in range(2):
                            pd = kb[(2 * s_ + nb) % 4]
                            for fc in range(4):
                                P.op("pe", lambda e, pd=pd, fc=fc, s_=s_, nb=nb, wd=wd: e.matmul(
                                    pd[:, :], lhsT=actT[:, fc, s_ * 128:(s_ + 1) * 128],
                                    rhs=wd[:, fc * D + nb * 512:fc * D + (nb + 1) * 512],
                                    start=(fc == 0), stop=(fc == 3)), reads=[actT.p(fc), wd.p()], writes=[pd.p()])
                            yb_ = yblk[s_ % 2]
                            if nb == 0:
                                P.op("act", lambda e, pd=pd, yb_=yb_, nb=nb: e.copy(
                                    out=yb_[:, nb * 512:(nb + 1) * 512], in_=pd[:, :]), reads=[pd.p()], writes=[yb_.p()])
                            else:
                                P.op("dve", lambda e, pd=pd, yb_=yb_, nb=nb: e.tensor_copy(
                                    out=yb_[:, nb * 512:(nb + 1) * 512], in_=pd[:, :]), reads=[pd.p()], writes=[yb_.p()])
                            yield 0
                        P.dma("sp", ds_yb[s_ % 2], lambda e, b=b, s_=s_, yb_=yb_: e.dma_start(
                            out=ys_v[b][:, s_, :], in_=yb_[:, :]), reads=[yb_.p()])
                    yield 1

            interleave([f2_chain(0), f2_chain(1)])
            phF2.__exit__()
            phF = Phase().__enter__()
            NF = 3
            ya = [sb("ya%d" % i, [128, D]) for i in range(NF)]
            yb2 = [sb("yb2_%d" % i, [128, D]) for i in range(NF)]
            x1f = [sb("x1f%d" % i, [128, D]) for i in range(NF)]
            ds_ya = [P.dsem("ds_ya%d" % i) for i in range(NF)]
            ds_x1f = [P.dsem("ds_x1f%d" % i) for i in range(NF)]
            of = [sb("of%d" % i, [128, D]) for i in range(2)]
            of2 = [sb("of2_%d" % i, [128, D]) for i in range(2)]
            ds_out = [P.dsem("ds_out%d" % i) for i in range(2)]
            fnb = sb("fnb", [128, D])
            ssf = sb("ssf", [128, 4])
            ds_f = P.dsem("ds_f")
            P.dma("sp", ds_f, lambda e: e.dma_start(out=fnb[:, :], in_=rbig_d[:, 2, :]), writes=[fnb.p()])

            def f3_load(i):
                q = i % NF
                rows = slice(i * 128, (i + 1) * 128)
                P.dma("sp", ds_x1f[q], lambda e, rows=rows, q=q: e.dma_start(out=x1f[q][:, :], in_=x1_s[rows, :]),
                      writes=[x1f[q].p()])
                for j, yt_ in enumerate((ya[q], yb2[q])):
                    P.dma("pool", ds_ya[q], lambda e, yt_=yt_, i=i, j=j: e.indirect_dma_start(
                        out=yt_[:, :], out_offset=None, in_=y_sorted[:, :],
                        in_offset=bass.IndirectOffsetOnAxis(ap=sloti[:, j, i:i + 1], axis=0)),
                        reads=[sloti.p()], writes=[yt_.p()])
            for i in range(NF - 1):
                f3_load(i)
            for i in range(NT):
                if i + NF - 1 < NT:
                    f3_load(i + NF - 1)
                q = i % NF
                par = i % 2
                rows = slice(i * 128, (i + 1) * 128)
                o1, o2 = of[par], of2[par]
                P.op("dve", lambda e, q=q, i=i, o1=o1: e.tensor_scalar(
                    out=o1[:, :], in0=ya[q][:, :], scalar1=gates[:, 0, i:i + 1], scalar2=None, op0=ALU.mult),
                    reads=[ya[q].p(), gates.p()], writes=[o1.p()])
                P.op("dve", lambda e, q=q, i=i, o1=o1: e.scalar_tensor_tensor(
                    out=o1[:, :], in0=yb2[q][:, :], scalar=gates[:, 1, i:i + 1], in1=o1[:, :], op0=ALU.mult, op1=ALU.add),
                    reads=[yb2[q].p(), gates.p(), o1.p()], writes=[o1.p()])
                P.op("pool", lambda e, o1=o1: e.tensor_tensor(out=o1[:, :], in0=o1[:, :], in1=g12[:, 1, :], op=ALU.mult),
                     reads=[o1.p(), g12.p()], writes=[o1.p()])
                P.op("pool", lambda e, q=q, o1=o1: e.tensor_tensor(out=o1[:, :], in0=o1[:, :], in1=x1f[q][:, :], op=ALU.add),
                     reads=[o1.p(), x1f[q].p()], writes=[o1.p()])
                P.op("act", lambda e, o1=o1, o2=o2, par=par: e.activation(out=o2[:, :], in_=o1[:, :], func=AF.Square,
                                                                          accum_out=ssf[:, 2 * par:2 * par + 1]),
                     reads=[o1.p()], writes=[o2.p(), ssf.p()])
                P.op("act", lambda e, par=par: e.activation(out=ssf[:, 2 * par + 1:2 * par + 2], in_=ssf[:, 2 * par:2 * par + 1],
                                                            func=AF.Sqrt, scale=1.0 / D, bias=epsb[:, 0:1]),
                     reads=[ssf.p(), epsb.p()], writes=[ssf.p()])
                P.op("dve", lambda e, par=par: e.reciprocal(out=ssf[:, 2 * par + 1:2 * par + 2], in_=ssf[:, 2 * par + 1:2 * par + 2]),
                     reads=[ssf.p()], writes=[ssf.p()])
                P.op("dve", lambda e, o1=o1, o2=o2, par=par: e.scalar_tensor_tensor(
                    out=o2[:, :], in0=o1[:, :], scalar=ssf[:, 2 * par + 1:2 * par + 2], in1=fnb[:, :],
                    op0=ALU.mult, op1=ALU.mult), reads=[o1.p(), ssf.p(), fnb.p()], writes=[o2.p()])
                P.dma("sp", ds_out[par], lambda e, rows=rows, o2=o2: e.dma_start(out=out_d[rows, :], in_=o2[:, :]), reads=[o2.p()])
            phF.__exit__()
        P.barrier(final=True)
        P.emit(block)
    return nc


def _prep_inputs(inputs, b):
    g = lambda n: np.asarray(inputs[n], dtype=np.float32)
    c = _get_consts()
    m = {}
    m["x"] = np.ascontiguousarray(g("x")[b])
    m["ctx"] = np.ascontiguousarray(g("ctx")[b])
    cv = np.stack([g("c")[b].reshape(KC, 128).T, g("c_ctx").reshape(KC, 128).T], axis=-1)
    m["cvec"] = np.ascontiguousarray(cv)
    m["w_mod"] = np.ascontiguousarray(g("w_mod")[0])
    pp = np.zeros((128, 128), np.float32)
    pp[:, 0:8] = g("norm1")[0].reshape(KC, 128).T
    pp[:, 8:56] = g("b_mod")[0].reshape(48, 128).T
    pp[:, 56:64] = g("norm2")[0].reshape(KC, 128).T
    pp[:, 64:72] = g("ssd_norm")[0].reshape(KC, 128).T
    m["pp"] = pp
    m["w_in"] = np.ascontiguousarray(g("w_in")[0])
    cw = g("conv_w")[0]
    cb = g("conv_b")[0]
    cpar = np.zeros((128, 16, 6), np.float32)
    cpar[:, :, 0:5] = cw.reshape(5, 16, 128).transpose(2, 1, 0)
    cpar[:, :, 5] = cb.reshape(16, 128).T
    m["cpar"] = cpar
    rp = np.zeros((128, 128), np.float32)
    rp[:, 0:32] = g("dt_bias")[0].reshape(1, 32)
    rp[:, 32:64] = g("a_log")[0].reshape(1, 32)
    rp[:, 64:80] = g("d_skip")[0].reshape(1, 16)
    rp[:, 80:84] = g("b_rg")[0].reshape(1, 4)
    rp[:, 84:116] = g("b_re")[0].reshape(1, 32)
    rbig = np.zeros((128, 6, D), np.float32)
    rbig[:, 0, :] = g("b_mod")[0][2 * D:3 * D][None, :]
    rbig[:, 1, :] = g("b_mod")[0][5 * D:6 * D][None, :]
    rbig[:, 2, :] = g("final_norm")[None, :]
    rbig[:, 3, :] = g("b_mod")[0][3 * D:4 * D][None, :]
    rbig[:, 4, :] = g("b_mod")[0][4 * D:5 * D][None, :]
    rbig[:, 5, :] = g("norm2")[0][None, :]
    m["rbig"] = rbig
    m["w_four"] = np.ascontiguousarray(g("w_four")[0])
    m["w_out"] = np.ascontiguousarray(g("w_out")[0])
    wr = np.concatenate([g("w_rg")[0], g("w_re")[0].transpose(1, 0, 2).reshape(D, 32)], axis=1)
    m["wr"] = np.ascontiguousarray(wr.reshape(KC, 128, 36).transpose(1, 0, 2))
    m["w_eg"] = np.ascontiguousarray(g("w_eg")[0].reshape(32, KC, 128, 512).transpose(0, 2, 1, 3))
    m["w_eu"] = np.ascontiguousarray(g("w_eu")[0].reshape(32, KC, 128, 512).transpose(0, 2, 1, 3))
    m["w_ed"] = np.ascontiguousarray(g("w_ed")[0].reshape(32, 4, 128, D).transpose(0, 2, 1, 3))
    hc = np.zeros((128, 128), np.float32)
    hc[:, 0:16] = (512.0 * np.arange(16))[None, :]
    hc[:, 16:64] = np.arange(48, dtype=np.float32)[None, :]
    hc[:, 80] = np.arange(128, dtype=np.float32)
    m["hc"] = hc
    m["zeros"] = np.zeros((128, D), dtype=ml_dtypes.bfloat16)
    m["padidx"] = np.full((48 * 512, 2), T, dtype=np.int32)
    tk = np.zeros((128, NT, 2), np.int32)
    tk[:, :, 0] = np.arange(NT)[None, :] * 128 + np.arange(128)[:, None]
    m["tokid"] = tk
    m["cs"] = c["cs"]
    m["dft_tab"] = c["dft_tab"]
    m["rp"] = rp
    m["masks"] = np.ascontiguousarray(np.stack([c["m_le"], c["m_gt"], c["m_ge"], c["m_lt"], c["ones"]], axis=1))
    m["ident_f"] = c["ident_f"]
    m["ident_b"] = c["ident_b"]
    return m


def kernel(**inputs):
    nc = build()
    in_maps = [_prep_inputs(inputs, b) for b in range(8)]
    res = run_bass_kernel_spmd(nc, in_maps, core_ids=list(range(8)))
    return np.stack([r["out"] for r in res.results], axis=0)
```

```python
import contextlib
import numpy as np
import ml_dtypes
import concourse.bass as bass
import concourse.mybir as mybir
from concourse.bass_utils import run_bass_kernel_spmd

F32 = mybir.dt.float32
BF16 = mybir.dt.bfloat16
ALU = mybir.AluOpType
AF = mybir.ActivationFunctionType
AX = mybir.AxisListType

D = 1024
KC = 8
T = 4096
TC = 256
TA = T + TC
NT = T // 128
NTA = TA // 128
DPROJ = 3616
EPS = 1e-6
ENGS = ("pe", "act", "dve", "pool", "sp")


class Tile:
    __slots__ = ("name", "w", "r")

    def __init__(self, name):
        self.name = name
        self.w = None
        self.r = {}


class DmaSem:
    def __init__(self, sem):
        self.sem = sem
        self.count = 0


class Ins:
    __slots__ = ("fn", "signal", "rank", "eng")

    def __init__(self, fn, eng):
        self.fn = fn
        self.eng = eng
        self.signal = False
        self.rank = None


class Prog:
    def __init__(self, nc):
        self.nc = nc
        self.ops = {e: [] for e in ENGS}
        self.known = {e: {} for e in ENGS}
        self.esem = {}
        self.stack = contextlib.ExitStack()
        for e in ENGS:
            self.esem[e] = self.stack.enter_context(nc.semaphore("s_" + e))
        self.dma_sems = []
        self.bg_sems = []
        self.last_ins = {e: None for e in ENGS}
        self.nins = {e: 0 for e in ENGS}

    def dsem(self, name, barrier=True):
        s = DmaSem(self.stack.enter_context(self.nc.semaphore(name)))
        if barrier:
            self.dma_sems.append(s)
        else:
            self.bg_sems.append(s)
        return s

    def _need(self, eng, tok):
        if tok is None:
            return
        if tok[0] == "e":
            ins = tok[1]
            if ins.eng == eng and eng == "pe":
                return
            key = ("e", ins.eng)
            idx = ins.rank
            if self.known[eng].get(key, -1) >= idx:
                return
            self.known[eng][key] = idx
            ins.signal = True
            self.ops[eng].append(("wait_e", ins))
        else:
            _, ds, val = tok
            val = max(val, ds.count)
            key = ("d", id(ds))
            if self.known[eng].get(key, -1) >= val:
                return
            self.known[eng][key] = val
            self.ops[eng].append(("wait_d", ds, val))

    def _deps(self, eng, reads, writes):
        for t in reads:
            self._need(eng, t.w)
        for t in writes:
            self._need(eng, t.w)
            for r in t.r.values():
                self._need(eng, r)

    def _commit(self, tok, reads, writes):
        key = ("e", tok[1].eng) if tok[0] == "e" else ("d", id(tok[1]))
        for t in reads:
            t.r[key] = tok
        for t in writes:
            t.w = tok
            t.r = {}

    def op(self, eng, fn, reads=(), writes=()):
        self._deps(eng, reads, writes)
        ins = Ins(fn, eng)
        ins.rank = self.nins[eng]
        self.nins[eng] += 1
        self.ops[eng].append(("ins", ins))
        self.last_ins[eng] = ins
        self._commit(("e", ins), reads, writes)
        return ins

    def dma(self, eng, ds, fn, reads=(), writes=()):
        self._deps(eng, reads, writes)
        ds.count += 16
        self.ops[eng].append(("dma", fn, ds))
        self._commit(("d", ds, ds.count), reads, writes)

    def barrier(self, final=False):
        toks = []
        if final:
            for ds in self.bg_sems:
                if ds.count:
                    toks.append(("d", ds, ds.count))
        for e in ENGS:
            if self.last_ins[e] is not None:
                toks.append(("e", self.last_ins[e]))
        for ds in self.dma_sems:
            if ds.count:
                toks.append(("d", ds, ds.count))
        for e in ENGS:
            for tk in toks:
                if tk[0] == "e" and tk[1].eng == e and e in ("pe", "sp"):
                    continue
                self._need(e, tk)

    def emit(self, block):
        sigcount = {}
        for e in ENGS:
            c = 0
            for o in self.ops[e]:
                if o[0] == "ins":
                    if o[1].signal:
                        c += 1
                        sigcount[id(o[1])] = c
        engs = {"pe": block.tensor, "act": block.scalar, "dve": block.vector,
                "pool": block.gpsimd, "sp": block.sync}
        esem = self.esem

        def make(e):
            ops = self.ops[e]

            def body(eng):
                for o in ops:
                    if o[0] == "ins":
                        r = o[1].fn(eng)
                        if o[1].signal:
                            r.then_inc(esem[e], 1)
                    elif o[0] == "dma":
                        o[1](eng).then_inc(o[2].sem, 16)
                    elif o[0] == "wait_e":
                        eng.wait_ge(esem[o[1].eng], sigcount[id(o[1])])
                    else:
                        eng.wait_ge(o[1].sem, o[2])
            return body

        for e in ENGS:
            engs[e](make(e))


class Buf:
    def __init__(self, t, name, nparts=1):
        self.t = t
        self.tiles = [Tile("%s.%d" % (name, i)) for i in range(nparts)]

    def __getitem__(self, k):
        return self.t[k]

    def p(self, i=0):
        return self.tiles[i]

    def all(self):
        return list(self.tiles)


def _consts():
    k = np.arange(128)
    c = {}
    c["ident_f"] = np.eye(128, dtype=np.float32)
    c["ident_b"] = np.eye(128, dtype=np.float32).astype(ml_dtypes.bfloat16)
    c["m_le"] = (k[:, None] <= k[None, :]).astype(np.float32)
    c["m_gt"] = (k[:, None] > k[None, :]).astype(np.float32)
    c["m_ge"] = (k[:, None] >= k[None, :]).astype(np.float32)
    c["m_lt"] = (k[:, None] < k[None, :]).astype(np.float32)
    c["ones"] = np.ones((128, 128), np.float32)
    cc = (k[:, None] * k[None, :]) % 128
    ang = 2.0 * np.pi * cc / 128.0
    c["cs"] = np.concatenate([np.cos(ang), -np.sin(ang)], axis=1).astype(np.float32).astype(ml_dtypes.bfloat16)
    n = (128 * np.arange(32)[None, :] + k[:, None]).astype(np.int64)
    kk = (512 * np.arange(8)[:, None] + np.arange(512)[None, :]).astype(np.int64)
    m = (n[None, :, :, None] * kk[:, None, None, :]) % 4096
    a = (2.0 * np.pi / 4096.0) * m.astype(np.float64)
    tab = np.stack([np.cos(a), np.sin(a)], axis=3)
    c["dft_tab"] = np.ascontiguousarray(tab.astype(np.float32).astype(ml_dtypes.bfloat16))
    return c


_CONSTS = None


def _get_consts():
    global _CONSTS
    if _CONSTS is None:
        _CONSTS = _consts()
    return _CONSTS


def build(dbg=()):
    nc = bass.Bass("TRN2", target_bir_lowering=False)
    dbg = set(dbg)

    def din(name, shape, dt=F32):
        return nc.dram_tensor(name, list(shape), dt, kind="ExternalInput").ap()

    def dout(name, shape, dt=F32):
        return nc.dram_tensor(name, list(shape), dt, kind="ExternalOutput").ap()

    x_d = din("x", [T, D])
    ctx_d = din("ctx", [TC, D])
    cvec_d = din("cvec", [128, KC, 2])
    w_mod_d = din("w_mod", [D, 6 * D])
    pp_d = din("pp", [128, 128])
    ident_f_d = din("ident_f", [128, 128])
    ident_b_d = din("ident_b", [128, 128], BF16)
    w_in_d = din("w_in", [D, DPROJ])
    cpar_d = din("cpar", [128, 16, 6])
    rbig_d = din("rbig", [128, 6, D])
    rp_d = din("rp", [128, 128])
    masks_d = din("masks", [128, 5, 128])
    w_four_d = din("w_four", [4, 128, 128])
    w_out_d = din("w_out", [1536, D])
    wr_d = din("wr", [128, KC, 36])
    w_eg_d = din("w_eg", [32, 128, KC, 512])
    w_eu_d = din("w_eu", [32, 128, KC, 512])
    w_ed_d = din("w_ed", [32, 128, 4, D])
    cs_d = din("cs", [128, 256], BF16)
    dft_d = din("dft_tab", [8, 128, 32, 2, 512], BF16)
    out_d = dout("out", [T, D])
    f_s = nc.dram_tensor("f_s", [4, 128, T], BF16).ap()
    four_s = nc.dram_tensor("four_s", [4, 128, T], BF16).ap()
    x1_s = nc.dram_tensor("x1_s", [T, D], F32).ap()
    h_s = nc.dram_tensor("h_s", [T, D], BF16).ap()
    hs_sorted = nc.dram_tensor("hs_sorted", [48 * 512, D], BF16).ap()
    y_sorted = nc.dram_tensor("y_sorted", [48 * 512, D], F32).ap()
    hc_d = din("hc", [128, 128])
    zeros_d = din("zeros", [2048, D], BF16)
    wgs = nc.dram_tensor("wgs", [32 * 128, 4096], BF16).ap()
    wus = nc.dram_tensor("wus", [32 * 128, 4096], BF16).ap()
    wds = nc.dram_tensor("wds", [32 * 128, 4096], BF16).ap()
    bc_s = nc.dram_tensor("bc_s", [8, 128, TA], BF16).ap()
    xs_s = nc.dram_tensor("xs_s", [TA, 1024], BF16).ap()
    bt_s = nc.dram_tensor("bt_s", [TA, 512], BF16).ap()
    y_s = nc.dram_tensor("y_s", [T, 1024], F32).ap()
    yb_s = nc.dram_tensor("yb_s", [T, 1024], F32).ap()
    dbg_d = {}
    if "uT" in dbg:
        dbg_d["uT"] = dout("dbg_uT", [128, KC, TA], BF16)
    if "ssd" in dbg:
        dbg_d["y"] = dout("dbg_y", [T, 1024])
        dbg_d["yb"] = dout("dbg_yb", [T, 1024])
    if "four" in dbg:
        dbg_d["four"] = dout("dbg_four", [4, 128, T], BF16)
    if "x1" in dbg:
        dbg_d["x1"] = dout("dbg_x1", [T, D])
        dbg_d["lg"] = dout("dbg_lg", [128, NT, 36])
        dbg_d["slot"] = dout("dbg_slot", [128, 2, NT], mybir.dt.int32)
        dbg_d["widx"] = dout("dbg_widx", [128, 2, 48], mybir.dt.int32)
        dbg_d["gates"] = dout("dbg_gates", [128, 2, NT])
        dbg_d["slotf"] = dout("dbg_slotf", [128, 2, NT])
    if "conv" in dbg:
        dbg_d["bc"] = dout("dbg_bc", [8, 128, TA], BF16)
        dbg_d["xs"] = dout("dbg_xs", [TA, 1024], BF16)
        dbg_d["bt"] = dout("dbg_bt", [TA, 512], BF16)
        dbg_d["dts"] = dout("dbg_dts", [128, 7, NTA * 32])

    P = Prog(nc)
    st = contextlib.ExitStack()

    cur = [st]

    def sb(name, shape, dt=F32, nparts=1):
        return Buf(cur[0].enter_context(nc.sbuf_tensor("sb_" + name, list(shape), dt)), name, nparts)

    def ps(name, shape, dt=F32, nparts=1):
        return Buf(st.enter_context(nc.psum_tensor("ps_" + name, list(shape), dt)), name, nparts)

    class Phase:
        def __enter__(self):
            self.prev = cur[0]
            self.stk = contextlib.ExitStack()
            cur[0] = self.stk
            return self

        def __exit__(self, *a):
            P.barrier()
            cur[0] = self.prev
            self.stk.close()
            return False

    stp = contextlib.ExitStack()

    def sbp(name, shape, dt=F32, nparts=1):
        return Buf(stp.enter_context(nc.sbuf_tensor("sb_" + name, list(shape), dt)), name, nparts)

    with P.stack, stp, st, nc.Block() as block:
        ident_f = sb("ident_f", [128, 128])
        ident_b = sb("ident_b", [128, 128], BF16)
        pp = sb("pp", [128, 128])
        cvec = sb("cvec", [128, KC, 2])
        svec = sb("svec", [128, KC, 2])
        ds_c = P.dsem("ds_c")
        P.dma("sp", ds_c, lambda e: e.dma_start(out=ident_f[:, :], in_=ident_f_d[:, :]), writes=[ident_f.p()])
        P.dma("sp", ds_c, lambda e: e.dma_start(out=ident_b[:, :], in_=ident_b_d[:, :]), writes=[ident_b.p()])
        P.dma("sp", ds_c, lambda e: e.dma_start(out=pp[:, :], in_=pp_d[:, :]), writes=[pp.p()])
        P.dma("sp", ds_c, lambda e: e.dma_start(out=cvec[:, :, :], in_=cvec_d[:, :, :]), writes=[cvec.p()])
        P.op("act", lambda e: e.activation(out=svec[:, :, :], in_=cvec[:, :, :], func=AF.Silu),
             reads=[cvec.p()], writes=[svec.p()])

        PP_N1, PP_BMOD = 0, 8

        banks = [ps("bank%d" % i, [128, 512]) for i in range(8)]
        psm = banks[0]
        pst = banks[1:3]
        psA = banks[3:7]
        modT = sb("modT", [128, 48, 2])
        scA = sb("scA", [128, KC, 2])
        epsb = sb("epsb", [128, 1])
        oneb = sb("oneb", [128, 1])
        P.op("pool", lambda e: e.memset(oneb[:, :], 1.0), writes=[oneb.p()])
        P.op("pool", lambda e: e.memset(epsb[:, :], EPS), writes=[epsb.p()])
        rp = sb("rp", [128, 128])
        g12 = sb("g12", [128, 4, D])
        rs = sb("rs", [128, NTA])
        scB = sb("scB", [128, KC])
        phA0 = Phase().__enter__()
        rbig = sb("rbig", [128, 5, D])
        ds_c1 = P.dsem("ds_c1")
        P.dma("sp", ds_c1, lambda e: e.dma_start(out=rbig[:, 0:2, :], in_=rbig_d[:, 0:2, :]), writes=[rbig.p()])
        P.dma("sp", ds_c1, lambda e: e.dma_start(out=rbig[:, 2:5, :], in_=rbig_d[:, 3:6, :]), writes=[rbig.p()])
        svb = sb("svb", [128, KC, 128])
        P.op("dve", lambda e: e.tensor_copy(out=svb[:, :, :], in_=svec[:, :, 0:1].to_broadcast([128, KC, 128])),
             reads=[svec.p()], writes=[svb.p()])
        wm = [sb("wm%d" % i, [128, KC, 512]) for i in range(2)]
        ds_wm = [P.dsem("ds_wm%d" % i) for i in range(2)]
        w_mod_v = w_mod_d.rearrange("(kc p) n -> p kc n", p=128)
        for blk in range(12):
            b = wm[blk % 2]
            P.dma("sp", ds_wm[blk % 2],
                  lambda e, b=b, blk=blk: e.dma_start(out=b[:, :, :], in_=w_mod_v[:, :, blk * 512:(blk + 1) * 512]),
                  writes=[b.p()])
            if blk in (4, 5, 6, 7, 8, 9, 10, 11):
                gi = {4: 0, 5: 0, 10: 1, 11: 1, 6: 2, 7: 2, 8: 3, 9: 3}[blk]
                hb = blk % 2
                pgb = banks[1 + (blk % 2)]
                for kc in range(KC):
                    P.op("pe", lambda e, b=b, kc=kc, pgb=pgb: e.matmul(
                        pgb[:, :], lhsT=svb[:, kc, :], rhs=b[:, kc, :], start=(kc == 0), stop=(kc == KC - 1)),
                        reads=[b.p(), svb.p()], writes=[pgb.p()])
                P.op("dve", lambda e, pgb=pgb, gi=gi, hb=hb: e.tensor_tensor(
                    out=g12[:, gi, hb * 512:(hb + 1) * 512], in0=pgb[:, :], in1=rbig[:, gi, hb * 512:(hb + 1) * 512],
                    op=ALU.add), reads=[pgb.p(), rbig.p()], writes=[g12.p()])
            for j in range(4):
                cj = blk * 4 + j
                for kc in range(KC):
                    P.op("pe", lambda e, b=b, j=j, kc=kc, cj=cj: e.matmul(
                        psm[:, cj * 2:cj * 2 + 2], lhsT=b[:, kc, j * 128:(j + 1) * 128], rhs=svec[:, kc, :],
                        start=(kc == 0), stop=(kc == KC - 1)),
                        reads=[b.p(), svec.p()], writes=[psm.p()])
        P.op("dve", lambda e: e.tensor_tensor(
            out=modT[:, :, :], in0=psm[:, 0:96].rearrange("p (c two) -> p c two", two=2),
            in1=pp[:, PP_BMOD:PP_BMOD + 48].unsqueeze(2).to_broadcast([128, 48, 2]), op=ALU.add),
            reads=[psm.p(), pp.p()], writes=[modT.p()])
        P.op("dve", lambda e: e.scalar_tensor_tensor(
            out=scA[:, :, :], in0=modT[:, 8:16, :], scalar=1.0,
            in1=pp[:, PP_N1:PP_N1 + 8].unsqueeze(2).to_broadcast([128, KC, 2]),
            op0=ALU.add, op1=ALU.mult),
            reads=[modT.p(), pp.p()], writes=[scA.p()])
        P.op("dve", lambda e: e.scalar_tensor_tensor(
            out=g12[:, 3, :], in0=g12[:, 3, :], scalar=1.0, in1=rbig[:, 4, :], op0=ALU.add, op1=ALU.mult),
            reads=[g12.p(), rbig.p()], writes=[g12.p()])
        P.op("dve", lambda e: e.scalar_tensor_tensor(
            out=scB[:, :], in0=modT[:, 32:40, 0], scalar=1.0, in1=pp[:, 56:64], op0=ALU.add, op1=ALU.mult),
            reads=[modT.p(), pp.p()], writes=[scB.p()])
        phA0.__exit__()
        phX = Phase().__enter__()
        NDT = NTA * 32
        dts = sb("dts", [128, 7, NDT])
        masks = sb("masks", [128, 5, 128])
        phU = Phase().__enter__()
        uT = sb("uT", [128, KC, TA], BF16, nparts=NTA)
        phA1 = Phase().__enter__()
        xt = [sb("xt%d" % i, [128, D]) for i in range(3)]
        ds_x = [P.dsem("ds_x%d" % i) for i in range(3)]
        xn = [sb("xn%d" % i, [128, D], BF16) for i in range(2)]
        junk = sb("junk", [128, D])
        ss = sb("ss", [128, NTA])
        utmp = [sb("utmp%d" % i, [128, KC, 128]) for i in range(2)]
        def xsrc(i):
            return ctx_d[i * 128:(i + 1) * 128, :] if i < 2 else x_d[(i - 2) * 128:(i - 1) * 128, :]
        for i in range(NTA):
            xb = xt[i % 3]
            P.dma("sp", ds_x[i % 3], lambda e, xb=xb, src=xsrc(i): e.dma_start(out=xb[:, :], in_=src), writes=[xb.p()])
            P.op("act", lambda e, xb=xb, i=i: e.activation(out=junk[:, :], in_=xb[:, :], func=AF.Square,
                                                           accum_out=ss[:, i:i + 1]),
                 reads=[xb.p()], writes=[junk.p(), ss.p()])
        P.op("act", lambda e: e.activation(out=rs[:, :], in_=ss[:, :], func=AF.Sqrt, scale=1.0 / D, bias=epsb[:, 0:1]),
             reads=[ss.p(), epsb.p()], writes=[rs.p()])
        P.op("dve", lambda e: e.reciprocal(out=rs[:, :], in_=rs[:, :]), reads=[rs.p()], writes=[rs.p()])
        for i in range(NTA):
            xb = xt[i % 3]
            which = 1 if i < 2 else 0
            P.dma("sp", ds_x[i % 3], lambda e, xb=xb, src=xsrc(i): e.dma_start(out=xb[:, :], in_=src), writes=[xb.p()])
            xnb = xn[i % 2]
            P.op("act", lambda e, xb=xb, xnb=xnb, i=i: e.activation(out=xnb[:, :], in_=xb[:, :], func=AF.Copy,
                                                                    scale=rs[:, i:i + 1]),
                 reads=[xb.p(), rs.p()], writes=[xnb.p()])
            pt = pst[i % 2]
            ptb = pt.t[:, :].bitcast(BF16)
            for kc in range(KC):
                P.op("pe", lambda e, ptb=ptb, xnb=xnb, kc=kc: e.transpose(
                    ptb[:, kc * 128:(kc + 1) * 128], xnb[:, kc * 128:(kc + 1) * 128], ident_b[:, :]),
                    reads=[xnb.p(), ident_b.p()], writes=[pt.p()])
            ut = utmp[i % 2]
            P.op("dve", lambda e, ptb=ptb, ut=ut, which=which: e.tensor_tensor(
                out=ut[:, :, :], in0=ptb.rearrange("p (k t) -> p k t", k=KC),
                in1=scA[:, :, which:which + 1].to_broadcast([128, KC, 128]), op=ALU.mult),
                reads=[pt.p(), scA.p()], writes=[ut.p()])
            P.op("pool", lambda e, ut=ut, i=i, which=which: e.tensor_tensor(
                out=uT[:, :, i * 128:(i + 1) * 128], in0=ut[:, :, :],
                in1=modT[:, 0:8, which:which + 1].to_broadcast([128, KC, 128]), op=ALU.add),
                reads=[ut.p(), modT.p()], writes=[uT.p(i)])

        if "uT" in dbg:
            ds_dbg = P.dsem("ds_dbg")
            P.dma("sp", ds_dbg, lambda e: e.dma_start(out=dbg_d["uT"][:, :, :], in_=uT[:, :, :]),
                  reads=uT.all())

        phA1.__exit__()
        phB = Phase().__enter__()
        RP_DTB, RP_ALOG, RP_DSKIP = 0, 32, 64
        cpar = sb("cpar", [128, 16, 6])
        ds_c3 = P.dsem("ds_c3")
        P.dma("sp", ds_c3, lambda e: e.dma_start(out=cpar[:, :, :], in_=cpar_d[:, :, :]), writes=[cpar.p()])
        P.dma("sp", ds_c3, lambda e: e.dma_start(out=rp[:, :], in_=rp_d[:, :]), writes=[rp.p()])
        P.dma("sp", ds_c3, lambda e: e.dma_start(out=masks[:, :, :], in_=masks_d[:, :, :]), writes=[masks.p()])
        w_in_v = w_in_d.rearrange("(kc p) n -> p kc n", p=128)
        wcb = [sb("wcb%d" % i, [128, KC, 512], BF16) for i in range(2)]
        ds_pc = [P.dsem("ds_pc%d" % i, barrier=False) for i in range(4)]
        t_pc = [Tile("pc%d" % i) for i in range(4)]
        t_wcast = Tile("wcast")
        pc_list = []
        for (src_, dst_) in ((w_eg_d, wgs), (w_eu_d, wus), (w_ed_d, wds)):
            sv_ = src_.rearrange("e p k f -> e p (k f)")
            for ex in range(32):
                pc_list.append((sv_, dst_, ex))
        pc_pos = [0]

        def precast_issue(n):
            for _ in range(n):
                if pc_pos[0] >= len(pc_list):
                    return
                sv_, dst_, ex = pc_list[pc_pos[0]]
                k = pc_pos[0] % 4
                pc_pos[0] += 1
                P.dma("pool", ds_pc[k], lambda e, sv_=sv_, dst_=dst_, ex=ex: e.dma_start(
                    out=dst_[ex * 128:(ex + 1) * 128, :], in_=sv_[ex]), writes=[t_pc[k], t_wcast])
        ds_wc = [P.dsem("ds_wc%d" % i) for i in range(2)]
        pre = [sb("pre%d" % i, [128, TA + 8], BF16) for i in range(2)]
        acc = [sb("acc%d" % i, [128, TA + 4]) for i in range(1)] * 2
        post = [sb("post%d" % i, [128, TA], BF16) for i in range(2)]
        tokb = [sb("tokb%d" % i, [128, NTA, 128], BF16) for i in range(1)] * 2
        ds_post = [P.dsem("ds_post%d" % i) for i in range(2)]
        ds_tokb = [P.dsem("ds_tokb%d" % i) for i in range(2)]
        for pb in pre:
            P.op("pool", lambda e, pb=pb: e.memset(pb[:, :], 0.0), writes=[pb.p()])
        xs_v = xs_s.rearrange("(i p) c -> p i c", p=128)
        bt_v = bt_s.rearrange("(i p) c -> p i c", p=128)
        CL = TA + 4
        for cc in range(16):
            if cc % 4 == 0:
                wb = wcb[(cc // 4) % 2]
                c0 = 1024 + 128 * cc
                P.dma("pool", ds_wc[(cc // 4) % 2],
                      lambda e, wb=wb, c0=c0: e.dma_start(out=wb[:, :, :], in_=w_in_v[:, :, c0:c0 + 512]),
                      writes=[wb.p()])
            j = cc % 4
            pb, ab, qb = pre[cc % 2], acc[cc % 2], post[cc % 2]
            for tb in range(9):
                n = 256 if tb == 0 else 512
                tok0 = 0 if tb == 0 else 256 + 512 * (tb - 1)
                off = 2 if tb == 0 else 262 + 512 * (tb - 1)
                pa = psA[tb % 4]
                for kc in range(KC):
                    P.op("pe", lambda e, pa=pa, wb=wb, j=j, kc=kc, tok0=tok0, n=n: e.matmul(
                        pa[:, 0:n], lhsT=wb[:, kc, j * 128:(j + 1) * 128], rhs=uT[:, kc, tok0:tok0 + n],
                        start=(kc == 0), stop=(kc == KC - 1)),
                        reads=[wb.p()] + uT.all()[tok0 // 128:(tok0 + n) // 128], writes=[pa.p()])
                P.op("act", lambda e, pa=pa, pb=pb, off=off, n=n: e.copy(out=pb[:, off:off + n], in_=pa[:, 0:n]),
                     reads=[pa.p()], writes=[pb.p()])
            ceng = "dve"
            precast_issue(4)
            P.op(ceng, lambda e, ab=ab, pb=pb, cc=cc: e.tensor_scalar(
                out=ab[:, :], in0=pb[:, 0:CL], scalar1=cpar[:, cc, 0:1], scalar2=None, op0=ALU.mult),
                reads=[pb.p(), cpar.p()], writes=[ab.p()])
            for tap in range(1, 5):
                P.op(ceng, lambda e, ab=ab, pb=pb, cc=cc, tap=tap: e.scalar_tensor_tensor(
                    out=ab[:, :], in0=pb[:, tap:tap + CL], scalar=cpar[:, cc, tap:tap + 1], in1=ab[:, :],
                    op0=ALU.mult, op1=ALU.add),
                    reads=[pb.p(), cpar.p(), ab.p()], writes=[ab.p()])
            P.op("act", lambda e, ab=ab, qb=qb, cc=cc: e.activation(
                out=qb[:, 0:256], in_=ab[:, 0:256], func=AF.Silu, bias=cpar[:, cc, 5:6]),
                reads=[ab.p(), cpar.p()], writes=[qb.p()])
            P.op("act", lambda e, ab=ab, qb=qb, cc=cc: e.activation(
                out=qb[:, 256:TA], in_=ab[:, 260:260 + T], func=AF.Silu, bias=cpar[:, cc, 5:6]),
                reads=[ab.p(), cpar.p()], writes=[qb.p()])
            if cc >= 8:
                P.dma("sp", ds_post[cc % 2], lambda e, qb=qb, cc=cc: e.dma_start(out=bc_s[cc - 8, :, :], in_=qb[:, :]),
                      reads=[qb.p()])
            if cc < 12:
                tk = tokb[cc % 2]
                for i0 in range(0, NTA, 8):
                    ni = min(8, NTA - i0)
                    pt = pst[(i0 // 8) % 2]
                    ptb = pt.t[:, :].bitcast(BF16)
                    for ii in range(ni):
                        i = i0 + ii
                        P.op("pe", lambda e, ptb=ptb, qb=qb, i=i, ii=ii: e.transpose(
                            ptb[:, ii * 128:(ii + 1) * 128], qb[:, i * 128:(i + 1) * 128], ident_b[:, :]),
                            reads=[qb.p(), ident_b.p()], writes=[pt.p()])
                    eng2 = "pool" if cc % 2 == 0 else "dve"
                    eng2 = "dve"
                    P.op(eng2, lambda e, ptb=ptb, tk=tk, i0=i0, ni=ni: e.tensor_copy(
                        out=tk[:, i0:i0 + ni, :], in_=ptb[:, 0:ni * 128].rearrange("p (i c) -> p i c", c=128)),
                        reads=[pt.p()], writes=[tk.p()])
                if cc < 8:
                    dst = xs_v[:, :, cc * 128:(cc + 1) * 128]
                else:
                    dst = bt_v[:, :, (cc - 8) * 128:(cc - 7) * 128]
                P.dma("sp", ds_tokb[cc % 2], lambda e, tk=tk, dst=dst: e.dma_start(out=dst, in_=tk[:, :, :]),
                      reads=[tk.p()])

        wdt = sb("wdt", [128, KC, 32], BF16)
        ds_wdt = P.dsem("ds_wdt")
        P.dma("pool", ds_wdt, lambda e: e.dma_start(out=wdt[:, :, :], in_=w_in_v[:, :, 3072:3104]), writes=[wdt.p()])
        abc = sb("abc", [128, 32])
        P.op("act", lambda e: e.activation(out=abc[:, :], in_=rp[:, RP_ALOG:RP_ALOG + 32], func=AF.Exp),
             reads=[rp.p()], writes=[abc.p()])
        P.op("dve", lambda e: e.tensor_scalar(out=abc[:, :], in0=abc[:, :], scalar1=-1.0, scalar2=None, op0=ALU.mult),
             reads=[abc.p()], writes=[abc.p()])
        for c3 in range(3):
            i0 = c3 * 16
            ni = min(16, NTA - i0)
            pa = psA[c3]
            for ii in range(ni):
                i = i0 + ii
                for kc in range(KC):
                    P.op("pe", lambda e, pa=pa, ii=ii, i=i, kc=kc: e.matmul(
                        pa[:, ii * 32:(ii + 1) * 32], lhsT=uT[:, kc, i * 128:(i + 1) * 128], rhs=wdt[:, kc, :],
                        start=(kc == 0), stop=(kc == KC - 1)),
                        reads=[uT.p(i), wdt.p()], writes=[pa.p()])
            P.op("dve", lambda e, pa=pa, i0=i0, ni=ni: e.tensor_tensor(
                out=dts[:, 0, i0 * 32:(i0 + ni) * 32].rearrange("p (i c) -> p i c", c=32),
                in0=pa[:, 0:ni * 32].rearrange("p (i c) -> p i c", c=32),
                in1=rp[:, RP_DTB:RP_DTB + 32].unsqueeze(1).to_broadcast([128, ni, 32]), op=ALU.add),
                reads=[pa.p(), rp.p()], writes=[dts.p()])
        P.op("act", lambda e: e.activation(out=dts[:, 0, :], in_=dts[:, 0, :], func=AF.Exp),
             reads=[dts.p()], writes=[dts.p()])
        P.op("act", lambda e: e.activation(out=dts[:, 0, :], in_=dts[:, 0, :], func=AF.Ln, bias=oneb[:, 0:1]),
             reads=[dts.p(), oneb.p()], writes=[dts.p()])
        P.op("dve", lambda e: e.tensor_tensor(
            out=dts[:, 1, :].rearrange("p (i c) -> p i c", c=32),
            in0=dts[:, 0, :].rearrange("p (i c) -> p i c", c=32),
            in1=abc[:, :].unsqueeze(1).to_broadcast([128, NTA, 32]), op=ALU.mult),
            reads=[dts.p(), abc.p()], writes=[dts.p()])
        for q, mi in enumerate([0, 1, 2, 3, 4]):
            for c3 in range(3):
                c0 = c3 * 512
                n = min(512, NDT - c0)
                pa = psA[(q * 3 + c3) % 4]
                P.op("pe", lambda e, pa=pa, mi=mi, c0=c0, n=n: e.matmul(
                    pa[:, 0:n], lhsT=masks[:, mi, :], rhs=dts[:, 1, c0:c0 + n], start=True, stop=True),
                    reads=[masks.p(), dts.p()], writes=[pa.p()])
                P.op("act", lambda e, pa=pa, q=q, c0=c0, n=n: e.activation(
                    out=dts[:, 2 + q, c0:c0 + n], in_=pa[:, 0:n], func=AF.Exp),
                    reads=[pa.p()], writes=[dts.p()])
        if "conv" in dbg:
            P.barrier()
            ds_dbg2 = P.dsem("ds_dbg2")
            P.dma("sp", ds_dbg2, lambda e: e.dma_start(out=dbg_d["bc"][:, :, :], in_=bc_s[:, :, :]))
            P.dma("sp", ds_dbg2, lambda e: e.dma_start(out=dbg_d["xs"][:, :], in_=xs_s[:, :]))
            P.dma("sp", ds_dbg2, lambda e: e.dma_start(out=dbg_d["bt"][:, :], in_=bt_s[:, :]))
            P.dma("sp", ds_dbg2, lambda e: e.dma_start(out=dbg_d["dts"][:, :, :], in_=dts[:, :, :]), reads=[dts.p()])
        phB.__exit__()

        phC1 = Phase().__enter__()
        wf = sb("wf", [128, KC, 512], BF16)
        ds_wf = P.dsem("ds_wf")
        P.dma("pool", ds_wf, lambda e: e.dma_start(out=wf[:, :, :], in_=w_in_v[:, :, 3104:3616]), writes=[wf.p()])
        fblk = [sb("fblk%d" % i, [128, T], BF16) for i in range(2)]
        ds_fb = [P.dsem("ds_fb%d" % i) for i in range(2)]
        for g in range(4):
            fb = fblk[g % 2]
            for tb in range(8):
                pa = psA[tb % 4]
                tok0 = 256 + 512 * tb
                for kc in range(KC):
                    P.op("pe", lambda e, pa=pa, g=g, kc=kc, tok0=tok0: e.matmul(
                        pa[:, :], lhsT=wf[:, kc, g * 128:(g + 1) * 128], rhs=uT[:, kc, tok0:tok0 + 512],
                        start=(kc == 0), stop=(kc == KC - 1)),
                        reads=[wf.p()] + uT.all()[tok0 // 128:tok0 // 128 + 4], writes=[pa.p()])
                ev = "act" if tb % 2 == 0 else "dve"
                if ev == "act":
                    P.op("act", lambda e, pa=pa, fb=fb, tb=tb: e.copy(out=fb[:, tb * 512:(tb + 1) * 512], in_=pa[:, :]),
                         reads=[pa.p()], writes=[fb.p()])
                else:
                    P.op("dve", lambda e, pa=pa, fb=fb, tb=tb: e.tensor_copy(out=fb[:, tb * 512:(tb + 1) * 512], in_=pa[:, :]),
                         reads=[pa.p()], writes=[fb.p()])
            P.dma("sp", ds_fb[g % 2], lambda e, fb=fb, g=g: e.dma_start(out=f_s[g, :, :], in_=fb[:, :]), reads=[fb.p()])
        phC1.__exit__()

        phU.__exit__()

        phC = Phase().__enter__()
        fT = sb("fT", [128, 4, T], BF16)
        Yb = sb("Yb", [128, NT, 4, 256], BF16, nparts=NT)
        cs = sb("cs", [128, 256], BF16)
        wfour = sb("wfour", [128, 4, 128], BF16)
        tabs = [sb("tabs%d" % i, [128, 8, 2, 512], BF16) for i in range(2)]
        ds_tab = [P.dsem("ds_tab%d" % i) for i in range(2)]
        specT = [sb("specT%d" % i, [128, 4, 512], BF16) for i in range(2)]
        fourb = [sb("fourb%d" % i, [128, 4, 512], BF16) for i in range(2)]
        ds_four = [P.dsem("ds_four%d" % i) for i in range(2)]
        ds_cc = P.dsem("ds_cc")
        P.dma("sp", ds_cc, lambda e: e.dma_start(out=fT[:, :, :], in_=f_s.rearrange("g p t -> p g t")), writes=[fT.p()])
        P.dma("sp", ds_cc, lambda e: e.dma_start(out=cs[:, :], in_=cs_d[:, :]), writes=[cs.p()])
        ds_cc2 = P.dsem("ds_cc2")
        P.dma("pool", ds_cc2, lambda e: e.dma_start(out=wfour[:, :, :], in_=w_four_d.rearrange("g c d -> c g d")),
              writes=[wfour.p()])
        for i in range(NT):
            for h2 in range(2):
                pa = psA[(2 * i + h2) % 4]
                for gg in range(2):
                    g = 2 * h2 + gg
                    P.op("pe", lambda e, pa=pa, gg=gg, g=g, i=i: e.matmul(
                        pa[:, gg * 256:(gg + 1) * 256], lhsT=fT[:, g, i * 128:(i + 1) * 128], rhs=cs[:, :],
                        start=True, stop=True), reads=[fT.p(), cs.p()], writes=[pa.p()])
                if h2 == 0:
                    P.op("act", lambda e, pa=pa, i=i, h2=h2: e.copy(
                        out=Yb[:, i, 2 * h2:2 * h2 + 2, :], in_=pa[:, :].rearrange("p (g c) -> p g c", c=256)),
                        reads=[pa.p()], writes=[Yb.p(i)])
                else:
                    P.op("dve", lambda e, pa=pa, i=i, h2=h2: e.tensor_copy(
                        out=Yb[:, i, 2 * h2:2 * h2 + 2, :], in_=pa[:, :].rearrange("p (g c) -> p g c", c=256)),
                        reads=[pa.p()], writes=[Yb.p(i)])
        ORTHO = 1.0 / float(np.sqrt(4096.0 * 128.0))
        piece = 0
        for kb in range(8):
            for q in range(4):
                tb_ = tabs[piece % 2]
                P.dma("sp", ds_tab[piece % 2], lambda e, tb_=tb_, kb=kb, q=q: e.dma_start(
                    out=tb_[:, :, :, :], in_=dft_d[kb, :, q * 8:(q + 1) * 8, :, :]), writes=[tb_.p()])
                piece += 1
                for ii in range(8):
                    i = q * 8 + ii
                    for g in range(4):
                        P.op("pe", lambda e, g=g, i=i, ii=ii, tb_=tb_: e.matmul(
                            banks[g][:, :], lhsT=Yb[:, i, g, 0:128], rhs=tb_[:, ii, 0, :], start=(i == 0), stop=False),
                            reads=[Yb.p(i), tb_.p()], writes=[banks[g].p()])
                        P.op("pe", lambda e, g=g, i=i, ii=ii, tb_=tb_: e.matmul(
                            banks[g][:, :], lhsT=Yb[:, i, g, 128:256], rhs=tb_[:, ii, 1, :], start=False, stop=(i == NT - 1)),
                            reads=[Yb.p(i), tb_.p()], writes=[banks[g].p()])
            precast_issue(4)
            sp_ = specT[kb % 2]
            fo_ = fourb[kb % 2]
            for g in range(4):
                if g % 2 == 0:
                    P.op("act", lambda e, g=g, sp_=sp_: e.activation(out=sp_[:, g, :], in_=banks[g][:, :], func=AF.Copy,
                                                                    scale=ORTHO), reads=[banks[g].p()], writes=[sp_.p()])
                else:
                    P.op("dve", lambda e, g=g, sp_=sp_: e.tensor_scalar(out=sp_[:, g, :], in0=banks[g][:, :], scalar1=ORTHO,
                                                                       scalar2=None, op0=ALU.mult),
                         reads=[banks[g].p()], writes=[sp_.p()])
            for g in range(4):
                pb_ = banks[4 + g]
                P.op("pe", lambda e, g=g, sp_=sp_, pb_=pb_: e.matmul(
                    pb_[:, :], lhsT=wfour[:, g, :], rhs=sp_[:, g, :], start=True, stop=True),
                    reads=[wfour.p(), sp_.p()], writes=[pb_.p()])
                if g % 2 == 0:
                    P.op("act", lambda e, g=g, fo_=fo_, pb_=pb_: e.copy(out=fo_[:, g, :], in_=pb_[:, :]),
                         reads=[pb_.p()], writes=[fo_.p()])
                else:
                    P.op("dve", lambda e, g=g, fo_=fo_, pb_=pb_: e.tensor_copy(out=fo_[:, g, :], in_=pb_[:, :]),
                         reads=[pb_.p()], writes=[fo_.p()])
            P.dma("sp", ds_four[kb % 2], lambda e, fo_=fo_, kb=kb: e.dma_start(
                out=four_s.rearrange("g p t -> p g t")[:, :, kb * 512:(kb + 1) * 512], in_=fo_[:, :, :]), reads=[fo_.p()])
        precast_issue(1000)
        phC.__exit__()
        if "four" in dbg:
            ds_dbg4 = P.dsem("ds_dbg4")
            P.dma("sp", ds_dbg4, lambda e: e.dma_start(out=dbg_d["four"][:, :, :], in_=four_s[:, :, :]))
            P.barrier()
        phD = Phase().__enter__()
        y_v = y_s.rearrange("(i p) c -> p i c", p=128)
        dtsv = lambda q: dts[:, q, :].rearrange("p (i d h) -> p i d h", d=2, h=16)
        ys_tiles = [[Tile("ys%d_%d" % (g, i)) for i in range(NT)] for g in range(4)]

        def interleave(gens, level=0):
            gens = list(gens)
            while gens:
                for gn in list(gens):
                    try:
                        while next(gn) < level:
                            pass
                    except StopIteration:
                        gens.remove(gn)

        yb_v = yb_s.rearrange("(i p) c -> p i c", p=128)

        class GBuf:
            def __init__(self, k):
                self.BT = sb("BT%d" % k, [128, TA], BF16)
                self.CT = sb("CT%d" % k, [128, TA], BF16)
                self.xs_tok = sb("xs_tok%d" % k, [128, NTA, 256], BF16)
                self.B_tok = sb("B_tok%d" % k, [128, NTA, 128], BF16)
                self.ds = P.dsem("ds_g%d" % k)

        class CBuf:
            def __init__(self, c):
                self.kb = [banks[2 * c], banks[2 * c + 1]]
                self.cbm = [sb("cbm%d_%d" % (c, i), [128, 128], BF16) for i in range(2)]
                self.Rb = [sb("Rb%d_%d" % (c, i), [128, 512]) for i in range(2)]
                self.Eb = [sb("Eb%d_%d" % (c, i), [128, 512]) for i in range(1)] * 2
                self.MTb = [sb("MTb%d_%d" % (c, i), [128, 512], BF16) for i in range(2)]
                self.tmpb = [sb("tmpb%d_%d" % (c, i), [128, 256]) for i in range(2)]
                self.youtb = [sb("yout%d_%d" % (c, i), [128, 256]) for i in range(2)]
                self.xdb = [sb("xdb%d_%d" % (c, i), [128, 256], BF16) for i in range(2)]
                self.xddb = [sb("xddb%d_%d" % (c, i), [128, 256], BF16) for i in range(2)]
                self.ds_yout = [P.dsem("ds_yout%d_%d" % (c, i)) for i in range(2)]
                self.h32 = sb("h32_%d" % c, [128, 256])
                self.h16 = sb("h16_%d" % c, [128, 256], BF16)

        gbufs = [GBuf(0), GBuf(1)]
        cbufs = [CBuf(c) for c in range(4)]

        def ssd_dir_chain(gb, cbf, g, d):
            BT, CT, xs_tok, B_tok = gb.BT, gb.CT, gb.xs_tok, gb.B_tok
            kA, kB = cbf.kb
            h32, h16 = cbf.h32, cbf.h16
            hs = slice(4 * g, 4 * g + 4)
            qd = 3 if d == 0 else 5
            P.op("pool", lambda e: e.memset(h32[:, :], 0.0), writes=[h32.p()])
            P.op("pool", lambda e: e.memset(h16[:, :], 0.0), writes=[h16.p()])
            order = list(range(NTA)) if d == 0 else [1, 0] + list(range(NTA - 1, 1, -1))
            m_cb = 0 if d == 0 else 2
            m_R = 0 if d == 0 else 2
            m_L = 1 if d == 0 else 3
            q_incl = 2 if d == 0 else 4
            ydst = y_v if d == 0 else yb_v
            yield 1
            for idx, i in enumerate(order):
                last = idx == len(order) - 1
                tsl = slice(i * 128, (i + 1) * 128)
                par = idx % 2
                xd, xdd = cbf.xdb[par], cbf.xddb[par]
                P.op("pool", lambda e, xd=xd, i=i: e.tensor_tensor(
                    out=xd[:, :].rearrange("p (r c) -> p r c", c=64),
                    in0=xs_tok[:, i, :].rearrange("p (r c) -> p r c", c=64),
                    in1=dtsv(0)[:, i, d, hs].unsqueeze(2).to_broadcast([128, 4, 64]), op=ALU.mult),
                    reads=[xs_tok.p(), dts.p()], writes=[xd.p()])
                if not last:
                    P.op("pool", lambda e, xd=xd, xdd=xdd, i=i: e.tensor_tensor(
                        out=xdd[:, :].rearrange("p (r c) -> p r c", c=64),
                        in0=xd[:, :].rearrange("p (r c) -> p r c", c=64),
                        in1=dtsv(qd)[:, i, d, hs].unsqueeze(2).to_broadcast([128, 4, 64]), op=ALU.mult),
                        reads=[xd.p(), dts.p()], writes=[xdd.p()])
                yield 0
                if i >= 2:
                    cb, R, E, MT, tmp = cbf.cbm[par], cbf.Rb[par], cbf.Eb[par], cbf.MTb[par], cbf.tmpb[par]
                    P.op("pe", lambda e, tsl=tsl: e.matmul(kA[:, 0:128], lhsT=BT[:, tsl], rhs=CT[:, tsl], start=True, stop=True),
                         reads=[BT.p(), CT.p()], writes=[kA.p()])
                    for r in range(4):
                        P.op("act", lambda e, R=R, r=r, i=i: e.activation(
                            out=R[:, r * 128:(r + 1) * 128], in_=masks[:, m_R, :], func=AF.Copy,
                            scale=dtsv(1)[:, i, d, 4 * g + r:4 * g + r + 1]),
                            reads=[masks.p(), dts.p()], writes=[R.p()])
                    yield 0
                    P.op("dve", lambda e, cb=cb: e.tensor_tensor(
                        out=cb[:, :], in0=kA[:, 0:128], in1=masks[:, m_cb, :], op=ALU.mult),
                        reads=[kA.p(), masks.p()], writes=[cb.p()])
                    P.op("pe", lambda e, R=R: e.matmul(kB[:, :], lhsT=masks[:, m_L, :], rhs=R[:, :], start=True, stop=True),
                         reads=[masks.p(), R.p()], writes=[kB.p()])
                    yield 0
                    P.op("act", lambda e, E=E: e.activation(out=E[:, :], in_=kB[:, :], func=AF.Exp),
                         reads=[kB.p()], writes=[E.p()])
                    P.op("pe", lambda e, tsl=tsl: e.matmul(kA[:, 128:384], lhsT=CT[:, tsl], rhs=h16[:, :], start=True, stop=True),
                         reads=[CT.p(), h16.p()], writes=[kA.p()])
                    yield 0
                    P.op("dve", lambda e, E=E, MT=MT, cb=cb: e.tensor_tensor(
                        out=MT[:, :].rearrange("p (r l) -> p r l", l=128),
                        in0=E[:, :].rearrange("p (r l) -> p r l", l=128),
                        in1=cb[:, :].unsqueeze(1).to_broadcast([128, 4, 128]), op=ALU.mult),
                        reads=[E.p(), cb.p()], writes=[MT.p()])
                    P.op("dve", lambda e, tmp=tmp, i=i: e.tensor_tensor(
                        out=tmp[:, :].rearrange("p (r c) -> p r c", c=64),
                        in0=kA[:, 128:384].rearrange("p (r c) -> p r c", c=64),
                        in1=dtsv(q_incl)[:, i, d, hs].unsqueeze(2).to_broadcast([128, 4, 64]), op=ALU.mult),
                        reads=[kA.p(), dts.p()], writes=[tmp.p()])
                    yield 0
                    for r in range(4):
                        P.op("pe", lambda e, MT=MT, r=r, xd=xd: e.matmul(
                            kA[:, r * 64:(r + 1) * 64], lhsT=MT[:, r * 128:(r + 1) * 128],
                            rhs=xd[:, r * 64:(r + 1) * 64], start=True, stop=True),
                            reads=[MT.p(), xd.p()], writes=[kA.p()])
                    yield 0
                    yo = cbf.youtb[par]
                    P.op("dve", lambda e, tmp=tmp, yo=yo: e.tensor_tensor(
                        out=yo[:, :], in0=kA[:, 0:256], in1=tmp[:, :], op=ALU.add),
                        reads=[kA.p(), tmp.p()], writes=[yo.p()])
                    if d == 1:
                        yield 0
                        P.op("pool", lambda e, tmp=tmp, i=i: e.tensor_tensor(
                            out=tmp[:, :].rearrange("p (r c) -> p r c", c=64),
                            in0=xs_tok[:, i, :].rearrange("p (r c) -> p r c", c=64),
                            in1=rp[:, RP_DSKIP + 4 * g:RP_DSKIP + 4 * g + 4].unsqueeze(2).to_broadcast([128, 4, 64]),
                            op=ALU.mult),
                            reads=[xs_tok.p(), rp.p()], writes=[tmp.p()])
                        P.op("pool", lambda e, yo=yo, tmp=tmp: e.tensor_tensor(
                            out=yo[:, :], in0=yo[:, :], in1=tmp[:, :], op=ALU.add),
                            reads=[tmp.p(), yo.p()], writes=[yo.p()])
                    P.dma("sp", cbf.ds_yout[par], lambda e, yo=yo, i=i: e.dma_start(
                        out=ydst[:, i - 2, 256 * g:256 * g + 256], in_=yo[:, :]), reads=[yo.p()])
                    yield 0
                if not last:
                    P.op("pe", lambda e, i=i, xdd=xdd: e.matmul(
                        kB[:, 0:256], lhsT=B_tok[:, i, :], rhs=xdd[:, :], start=True, stop=True),
                        reads=[B_tok.p(), xdd.p()], writes=[kB.p()])
                    P.op("dve", lambda e, i=i: e.tensor_tensor(
                        out=h32[:, :].rearrange("p (r c) -> p r c", c=64),
                        in0=h32[:, :].rearrange("p (r c) -> p r c", c=64),
                        in1=dtsv(6)[:, i, d, hs].unsqueeze(2).to_broadcast([128, 4, 64]), op=ALU.mult),
                        reads=[h32.p(), dts.p()], writes=[h32.p()])
                    yield 0
                    P.op("dve", lambda e: e.tensor_tensor(out=h32[:, :], in0=h32[:, :], in1=kB[:, 0:256], op=ALU.add),
                         reads=[h32.p(), kB.p()], writes=[h32.p()])
                    P.op("act", lambda e: e.copy(out=h16[:, :], in_=h32[:, :]), reads=[h32.p()], writes=[h16.p()])
                yield 1

        for rnd in range(2):
            chains = []
            for k in range(2):
                g = 2 * rnd + k
                gb = gbufs[k]
                P.dma("sp", gb.ds, lambda e, gb=gb, g=g: e.dma_start(out=gb.BT[:, :], in_=bc_s[g, :, :]), writes=[gb.BT.p()])
                P.dma("sp", gb.ds, lambda e, gb=gb, g=g: e.dma_start(out=gb.CT[:, :], in_=bc_s[4 + g, :, :]), writes=[gb.CT.p()])
                P.dma("sp", gb.ds, lambda e, gb=gb, g=g: e.dma_start(out=gb.xs_tok[:, :, :], in_=xs_v[:, :, 256 * g:256 * g + 256]),
                      writes=[gb.xs_tok.p()])
                P.dma("sp", gb.ds, lambda e, gb=gb, g=g: e.dma_start(out=gb.B_tok[:, :, :], in_=bt_v[:, :, 128 * g:128 * g + 128]),
                      writes=[gb.B_tok.p()])
                for d in range(2):
                    chains.append(ssd_dir_chain(gb, cbufs[2 * k + d], g, d))
            interleave(chains)
        phD.__exit__()
        phX.__exit__()
        if "ssd" in dbg:
            ds_dbg3 = P.dsem("ds_dbg3")
            P.dma("sp", ds_dbg3, lambda e: e.dma_start(out=dbg_d["y"][:, :], in_=y_s[:, :]))
            P.dma("sp", ds_dbg3, lambda e: e.dma_start(out=dbg_d["yb"][:, :], in_=yb_s[:, :]))
            P.barrier()

        BS = 512
        NB = 48
        I32 = mybir.dt.int32
        gates = sb("gates", [128, 2, NT])
        sloti = sb("sloti", [128, 2, NT], I32)
        widx = sb("widx", [128, 2, NB], I32)
        hc = sb("hc", [128, 128])
        masksE = sb("masksE", [128, 2, 128])
        ds_c2 = P.dsem("ds_c2")
        P.dma("sp", ds_c2, lambda e: e.dma_start(out=hc[:, :], in_=hc_d[:, :]), writes=[hc.p()])
        P.dma("sp", ds_c2, lambda e: e.dma_start(out=masksE[:, :, :], in_=masks_d[:, 3:5, :]), writes=[masksE.p()])
        phE = Phase().__enter__()
        lgall = sb("lgall", [128, NT, 36])
        phE1 = Phase().__enter__()
        wz = sb("wz", [128, KC, 1024], BF16)
        wout = sb("wout", [128, 12, 1024], BF16)
        four_sb = sb("four_sb", [128, 4, T], BF16)
        wr = sb("wr", [128, KC, 36])
        junk2 = sb("junk2", [128, D])
        ds_e = P.dsem("ds_e")
        ds_e2 = P.dsem("ds_e2")
        P.dma("pool", ds_e2, lambda e: e.dma_start(out=wz[:, :, :], in_=w_in_v[:, :, 0:1024]), writes=[wz.p()])
        P.dma("pool", ds_e2, lambda e: e.dma_start(out=wout[:, :, :], in_=w_out_d.rearrange("(k p) d -> p k d", p=128)),
              writes=[wout.p()])
        P.dma("sp", ds_e, lambda e: e.dma_start(out=four_sb[:, :, :], in_=four_s.rearrange("g p t -> p g t")),
              writes=[four_sb.p()])
        P.dma("sp", ds_e, lambda e: e.dma_start(out=wr[:, :, :], in_=wr_d[:, :, :]), writes=[wr.p()])

        ds_z = P.dsem("ds_z", barrier=False)
        t_hs0 = Tile("hs_zero")
        zf_pos = [0]

        def zero_fill_issue():
            if zf_pos[0] < 12:
                b_ = zf_pos[0]
                zf_pos[0] += 1
                P.dma("pool", ds_z, lambda e, b_=b_: e.dma_start(out=hs_sorted[b_ * 2048:(b_ + 1) * 2048, :], in_=zeros_d[:, :]),
                      writes=[t_hs0])

        def e_chain(ch):
            kb = banks[4 * ch:4 * ch + 4]
            xb = sb("xt2_%d" % ch, [128, D])
            yb = sb("yt2_%d" % ch, [128, D])
            yb3 = sb("yt3_%d" % ch, [128, D])
            ds_y3 = P.dsem("ds_y3_%d" % ch)
            ds_x2 = P.dsem("ds_x2_%d" % ch)
            ds_y2 = P.dsem("ds_y2_%d" % ch)
            xn2 = sb("xn2_%d" % ch, [128, D], BF16)
            utmp2 = sb("utmp2_%d" % ch, [128, KC, 128])
            uTt = sb("uTt%d" % ch, [128, KC, 128], BF16)
            sz = sb("sz%d" % ch, [128, D])
            yz = sb("yz%d" % ch, [128, D])
            yzb = sb("yzb%d" % ch, [128, D], BF16)
            catT = sb("catT%d" % ch, [128, KC, 128], BF16)
            x1b = sb("x1t%d" % ch, [128, D])
            ds_x1 = P.dsem("ds_x1_%d" % ch)
            hn = sb("hn%d" % ch, [128, D])
            hT32 = sb("hT32_%d" % ch, [128, KC, 128])
            hb2 = sb("hTb%d" % ch, [128, D], BF16)
            ds_hT = P.dsem("ds_hT%d" % ch)
            sse = sb("sse%d" % ch, [128, 4])
            yield 1
            for i in range(ch, NT, 2):
                rows = slice(i * 128, (i + 1) * 128)
                if ch == 0:
                    zero_fill_issue()
                P.dma("sp", ds_x2, lambda e, rows=rows: e.dma_start(out=xb[:, :], in_=x_d[rows, :]), writes=[xb.p()])
                P.dma("sp", ds_y2, lambda e, rows=rows: e.dma_start(out=yb[:, :], in_=y_s[rows, :]), writes=[yb.p()])
                P.dma("sp", ds_y3, lambda e, rows=rows: e.dma_start(out=yb3[:, :], in_=yb_s[rows, :]), writes=[yb3.p()])
                P.op("pool", lambda e: e.tensor_tensor(out=yb[:, :], in0=yb[:, :], in1=yb3[:, :], op=ALU.add),
                     reads=[yb.p(), yb3.p()], writes=[yb.p()])
                P.op("act", lambda e, i=i: e.activation(out=xn2[:, :], in_=xb[:, :], func=AF.Copy, scale=rs[:, i + 2:i + 3]),
                     reads=[xb.p(), rs.p()], writes=[xn2.p()])
                yield 0
                pt = kb[0]
                ptb = pt.t[:, :].bitcast(BF16)
                for kc in range(KC):
                    P.op("pe", lambda e, ptb=ptb, kc=kc: e.transpose(
                        ptb[:, kc * 128:(kc + 1) * 128], xn2[:, kc * 128:(kc + 1) * 128], ident_b[:, :]),
                        reads=[xn2.p(), ident_b.p()], writes=[pt.p()])
                yield 0
                P.op("dve", lambda e, ptb=ptb: e.tensor_tensor(
                    out=utmp2[:, :, :], in0=ptb.rearrange("p (k t) -> p k t", k=KC),
                    in1=scA[:, :, 0:1].to_broadcast([128, KC, 128]), op=ALU.mult),
                    reads=[pt.p(), scA.p()], writes=[utmp2.p()])
                yield 0
                P.op("pool", lambda e: e.tensor_tensor(
                    out=uTt[:, :, :], in0=utmp2[:, :, :], in1=modT[:, 0:8, 0:1].to_broadcast([128, KC, 128]), op=ALU.add),
                    reads=[utmp2.p(), modT.p()], writes=[uTt.p()])
                yield 0
                for nb in range(2):
                    zb = kb[1 + nb]
                    for kc in range(KC):
                        P.op("pe", lambda e, zb=zb, kc=kc, nb=nb: e.matmul(
                            zb[:, :], lhsT=uTt[:, kc, :], rhs=wz[:, kc, nb * 512:(nb + 1) * 512],
                            start=(kc == 0), stop=(kc == KC - 1)), reads=[uTt.p(), wz.p()], writes=[zb.p()])
                    yield 0
                    P.op("act", lambda e, zb=zb, nb=nb: e.activation(out=sz[:, nb * 512:(nb + 1) * 512], in_=zb[:, :], func=AF.Silu),
                         reads=[zb.p()], writes=[sz.p()])
                yield 0
                P.op("dve", lambda e: e.tensor_tensor(out=yz[:, :], in0=yb[:, :], in1=sz[:, :], op=ALU.mult),
                     reads=[yb.p(), sz.p()], writes=[yz.p()])
                yield 0
                P.op("act", lambda e: e.activation(out=junk2[:, :], in_=yz[:, :], func=AF.Square, accum_out=sse[:, 0:1]),
                     reads=[yz.p()], writes=[junk2.p(), sse.p()])
                P.op("act", lambda e: e.activation(out=sse[:, 1:2], in_=sse[:, 0:1], func=AF.Sqrt, scale=1.0 / D, bias=epsb[:, 0:1]),
                     reads=[sse.p(), epsb.p()], writes=[sse.p()])
                yield 0
                P.op("dve", lambda e: e.reciprocal(out=sse[:, 1:2], in_=sse[:, 1:2]), reads=[sse.p()], writes=[sse.p()])
                yield 0
                P.op("act", lambda e: e.activation(out=yzb[:, :], in_=yz[:, :], func=AF.Copy, scale=sse[:, 1:2]),
                     reads=[yz.p(), sse.p()], writes=[yzb.p()])
                yield 0
                for kc in range(KC):
                    P.op("pe", lambda e, ptb=ptb, kc=kc: e.transpose(
                        ptb[:, kc * 128:(kc + 1) * 128], yzb[:, kc * 128:(kc + 1) * 128], ident_b[:, :]),
                        reads=[yzb.p(), ident_b.p()], writes=[pt.p()])
                yield 0
                P.op("dve", lambda e, ptb=ptb: e.tensor_tensor(
                    out=catT[:, :, :], in0=ptb.rearrange("p (k t) -> p k t", k=KC),
                    in1=pp[:, 64:72].unsqueeze(2).to_broadcast([128, KC, 128]), op=ALU.mult),
                    reads=[pt.p(), pp.p()], writes=[catT.p()])
                yield 0
                for nb in range(2):
                    mb = kb[1 + nb]
                    for k in range(12):
                        lh = (lambda k=k: catT[:, k, :]) if k < 8 else (lambda k=k, i=i: four_sb[:, k - 8, i * 128:(i + 1) * 128])
                        P.op("pe", lambda e, mb=mb, k=k, nb=nb, lh=lh: e.matmul(
                            mb[:, :], lhsT=lh(), rhs=wout[:, k, nb * 512:(nb + 1) * 512], start=(k == 0), stop=(k == 11)),
                            reads=[catT.p(), four_sb.p(), wout.p()], writes=[mb.p()])
                    yield 0
                    P.op("dve", lambda e, mb=mb, nb=nb: e.tensor_tensor(
                        out=x1b[:, nb * 512:(nb + 1) * 512], in0=mb[:, :], in1=g12[:, 0, nb * 512:(nb + 1) * 512], op=ALU.mult),
                        reads=[mb.p(), g12.p()], writes=[x1b.p()])
                yield 0
                P.op("pool", lambda e: e.tensor_tensor(out=x1b[:, :], in0=x1b[:, :], in1=xb[:, :], op=ALU.add),
                     reads=[x1b.p(), xb.p()], writes=[x1b.p()])
                yield 0
                P.dma("sp", ds_x1, lambda e, rows=rows: e.dma_start(out=x1_s[rows, :], in_=x1b[:, :]), reads=[x1b.p()])
                P.op("act", lambda e: e.activation(out=junk2[:, :], in_=x1b[:, :], func=AF.Square, accum_out=sse[:, 2:3]),
                     reads=[x1b.p()], writes=[junk2.p(), sse.p()])
                P.op("act", lambda e: e.activation(out=sse[:, 3:4], in_=sse[:, 2:3], func=AF.Sqrt, scale=1.0 / D, bias=epsb[:, 0:1]),
                     reads=[sse.p(), epsb.p()], writes=[sse.p()])
                yield 0
                P.op("dve", lambda e: e.reciprocal(out=sse[:, 3:4], in_=sse[:, 3:4]), reads=[sse.p()], writes=[sse.p()])
                yield 0
                P.op("act", lambda e: e.activation(out=hn[:, :], in_=x1b[:, :], func=AF.Copy, scale=sse[:, 3:4]),
                     reads=[x1b.p(), sse.p()], writes=[hn.p()])
                yield 0
                P.op("dve", lambda e: e.tensor_tensor(out=hn[:, :], in0=hn[:, :], in1=g12[:, 3, :], op=ALU.mult),
                     reads=[hn.p(), g12.p()], writes=[hn.p()])
                yield 0
                P.op("pool", lambda e: e.tensor_tensor(out=hn[:, :], in0=hn[:, :], in1=g12[:, 2, :], op=ALU.add),
                     reads=[hn.p(), g12.p()], writes=[hn.p()])
                yield 0
                P.op("act", lambda e: e.copy(out=hb2[:, :], in_=hn[:, :]), reads=[hn.p()], writes=[hb2.p()])
                P.dma("sp", ds_hT, lambda e, rows=rows: e.dma_start(out=h_s[rows, :], in_=hb2[:, :]), reads=[hb2.p()])
                for h2 in range(2):
                    hb_ = kb[1 + h2]
                    for k4 in range(4):
                        kc = 4 * h2 + k4
                        P.op("pe", lambda e, hb_=hb_, k4=k4, kc=kc: e.transpose(
                            hb_[:, k4 * 128:(k4 + 1) * 128], hn[:, kc * 128:(kc + 1) * 128], ident_f[:, :]),
                            reads=[hn.p(), ident_f.p()], writes=[hb_.p()])
                    yield 0
                    if h2 == 0:
                        P.op("act", lambda e, hb_=hb_, h2=h2: e.copy(
                            out=hT32[:, 4 * h2:4 * h2 + 4, :], in_=hb_[:, :].rearrange("p (k t) -> p k t", k=4)),
                            reads=[hb_.p()], writes=[hT32.p()])
                    else:
                        P.op("dve", lambda e, hb_=hb_, h2=h2: e.tensor_copy(
                            out=hT32[:, 4 * h2:4 * h2 + 4, :], in_=hb_[:, :].rearrange("p (k t) -> p k t", k=4)),
                            reads=[hb_.p()], writes=[hT32.p()])
                yield 0
                lb = kb[3]
                for kc in range(KC):
                    P.op("pe", lambda e, kc=kc, lb=lb: e.matmul(lb[:, 0:36], lhsT=hT32[:, kc, :], rhs=wr[:, kc, :],
                                                              start=(kc == 0), stop=(kc == KC - 1)),
                         reads=[hT32.p(), wr.p()], writes=[lb.p()])
                yield 0
                P.op("dve", lambda e, lb=lb, i=i: e.tensor_copy(out=lgall[:, i, :], in_=lb[:, 0:36]), reads=[lb.p()], writes=[lgall.p()])
                yield 1

        interleave([e_chain(0), e_chain(1)])
        phE1.__exit__()
        if "noroute" not in dbg:
            r_lg = sb("r_lg", [128, NT, 4]); r_mx = sb("r_mx", [128, NT]); r_eg = sb("r_eg", [128, NT, 4])
            r_sg = sb("r_sg", [128, NT]); r_oh = sb("r_oh", [128, NT, 4]); r_le = sb("r_le", [128, NT, 4, 8])
            r_sel = sb("r_sel", [128, NT, 8]); r_m1 = sb("r_m1", [128, NT]); r_o1 = sb("r_o1", [128, NT, 8])
            r_s2 = sb("r_s2", [128, NT, 8]); r_m2 = sb("r_m2", [128, NT]); r_o2 = sb("r_o2", [128, NT, 8])
            r_e2 = sb("r_e2", [128, NT])
            OH1 = sb("OH1", [128, NT, 32]); OH2 = sb("OH2", [128, NT, 32]); Asum = sb("Asum", [128, NT * 32])
            rank = sb("rank", [128, NT, 32]); TTb = sb("TTb", [128, NT, 32]); PTb = sb("PTb", [128, NT, 32])
            cnt = sb("cnt", [128, 32]); cmpj = sb("cmpj", [128, 32, 16]); nblk = sb("nblk", [128, 32])
            sblk = sb("sblk", [128, 32]); eblk = sb("eblk", [128, 32]); cmpb = sb("cmpb", [128, NB, 32])
            ebf = sb("ebf", [128, NB]); tmpr = sb("tmpr", [128, NT, 32]); slotf = sb("slotf", [128, 2, NT])
            RT = [lgall, r_lg, r_mx, r_eg, r_sg, r_oh, r_le, r_sel, r_m1, r_o1, r_s2, r_m2, r_o2, r_e2, rp,
                  OH1, OH2, Asum, rank, TTb, PTb, cnt, cmpj, nblk, sblk, eblk, cmpb, ebf, tmpr, slotf, gates, sloti, widx, hc]
            rt = [b.p() for b in RT]

            rmax = [int(x[5:]) for x in dbg if x.startswith("rmax:")]
            rmax = rmax[0] if rmax else 10 ** 9
            rcnt = [0]

            def V(fn):
                rcnt[0] += 1
                if rcnt[0] <= rmax:
                    P.op("dve", fn, reads=rt, writes=rt)

            def A(fn):
                rcnt[0] += 1
                if rcnt[0] <= rmax:
                    P.op("act", fn, reads=rt, writes=rt)
            bc3 = lambda ap, n: ap.unsqueeze(2).to_broadcast([128, NT, n])
            V(lambda e: e.tensor_tensor(out=r_lg[:, :, :], in0=lgall[:, :, 0:4],
                                        in1=rp[:, 80:84].unsqueeze(1).to_broadcast([128, NT, 4]), op=ALU.add))
            V(lambda e: e.tensor_reduce(out=r_mx[:, :], in_=r_lg[:, :, :], axis=AX.X, op=ALU.max))
            V(lambda e: e.tensor_tensor(out=r_eg[:, :, :], in0=r_lg[:, :, :], in1=bc3(r_mx[:, :], 4), op=ALU.subtract))
            V(lambda e: e.tensor_tensor(out=r_oh[:, :, :], in0=r_lg[:, :, :], in1=bc3(r_mx[:, :], 4), op=ALU.is_equal))
            A(lambda e: e.activation(out=r_eg[:, :, :], in_=r_eg[:, :, :], func=AF.Exp))
            V(lambda e: e.tensor_reduce(out=r_sg[:, :], in_=r_eg[:, :, :], axis=AX.X, op=ALU.add))
            V(lambda e: e.reciprocal(out=r_sg[:, :], in_=r_sg[:, :]))
            V(lambda e: e.tensor_tensor(out=r_le[:, :, :, :], in0=lgall[:, :, 4:36].rearrange("p t (g x) -> p t g x", x=8),
                                        in1=rp[:, 84:116].rearrange("p (g x) -> p g x", x=8).unsqueeze(1).to_broadcast([128, NT, 4, 8]),
                                        op=ALU.add))
            V(lambda e: e.tensor_tensor(out=r_le[:, :, :, :], in0=r_le[:, :, :, :],
                                        in1=r_oh[:, :, :].unsqueeze(3).to_broadcast([128, NT, 4, 8]), op=ALU.mult))
            V(lambda e: e.tensor_reduce(out=r_sel[:, :, :], in_=r_le[:, :, :, :].rearrange("p t g x -> p t x g"), axis=AX.X, op=ALU.add))
            V(lambda e: e.tensor_reduce(out=r_m1[:, :], in_=r_sel[:, :, :], axis=AX.X, op=ALU.max))
            V(lambda e: e.tensor_tensor(out=r_o1[:, :, :], in0=r_sel[:, :, :], in1=bc3(r_m1[:, :], 8), op=ALU.is_equal))
            V(lambda e: e.scalar_tensor_tensor(out=r_s2[:, :, :], in0=r_o1[:, :, :], scalar=-1.0e30, in1=r_sel[:, :, :],
                                               op0=ALU.mult, op1=ALU.add))
            V(lambda e: e.tensor_reduce(out=r_m2[:, :], in_=r_s2[:, :, :], axis=AX.X, op=ALU.max))
            V(lambda e: e.tensor_tensor(out=r_o2[:, :, :], in0=r_s2[:, :, :], in1=bc3(r_m2[:, :], 8), op=ALU.is_equal))
            V(lambda e: e.tensor_tensor(out=r_e2[:, :], in0=r_m2[:, :], in1=r_m1[:, :], op=ALU.subtract))
            A(lambda e: e.activation(out=r_e2[:, :], in_=r_e2[:, :], func=AF.Exp))
            V(lambda e: e.tensor_scalar(out=gates[:, 0, :], in0=r_e2[:, :], scalar1=1.0, scalar2=None, op0=ALU.add))
            V(lambda e: e.reciprocal(out=gates[:, 0, :], in_=gates[:, 0, :]))
            V(lambda e: e.tensor_tensor(out=gates[:, 0, :], in0=gates[:, 0, :], in1=r_sg[:, :], op=ALU.mult))
            V(lambda e: e.tensor_tensor(out=gates[:, 1, :], in0=gates[:, 0, :], in1=r_e2[:, :], op=ALU.mult))
            V(lambda e: e.tensor_tensor(out=OH1[:, :, :].rearrange("p t (g x) -> p t g x", x=8),
                                        in0=r_o1[:, :, :].unsqueeze(2).to_broadcast([128, NT, 4, 8]),
                                        in1=r_oh[:, :, :].unsqueeze(3).to_broadcast([128, NT, 4, 8]), op=ALU.mult))
            V(lambda e: e.tensor_tensor(out=OH2[:, :, :].rearrange("p t (g x) -> p t g x", x=8),
                                        in0=r_o2[:, :, :].unsqueeze(2).to_broadcast([128, NT, 4, 8]),
                                        in1=r_oh[:, :, :].unsqueeze(3).to_broadcast([128, NT, 4, 8]), op=ALU.mult))
            V(lambda e: e.tensor_tensor(out=Asum[:, :], in0=OH1[:, :, :].rearrange("p t e -> p (t e)"),
                                        in1=OH2[:, :, :].rearrange("p t e -> p (t e)"), op=ALU.add))
            for hf in range(2):
                P.op("pe", lambda e, hf=hf: e.matmul(banks[hf][:, :], lhsT=masksE[:, 0, :], rhs=Asum[:, hf * 512:(hf + 1) * 512],
                                                     start=True, stop=True), reads=rt + [masksE.p()], writes=[banks[hf].p()])
                P.op("pe", lambda e, hf=hf: e.matmul(banks[2 + hf][:, :], lhsT=masksE[:, 1, :], rhs=Asum[:, hf * 512:(hf + 1) * 512],
                                                     start=True, stop=True), reads=rt + [masksE.p()], writes=[banks[2 + hf].p()])
                P.op("dve", lambda e, hf=hf: e.tensor_copy(out=rank[:, hf * 16:(hf + 1) * 16, :],
                                                           in_=banks[hf][:, :].rearrange("p (t e) -> p t e", e=32)),
                     reads=[banks[hf].p()] + rt, writes=rt)
                P.op("dve", lambda e, hf=hf: e.tensor_copy(out=TTb[:, hf * 16:(hf + 1) * 16, :],
                                                           in_=banks[2 + hf][:, :].rearrange("p (t e) -> p t e", e=32)),
                     reads=[banks[2 + hf].p()] + rt, writes=rt)
            V(lambda e: e.memset(PTb[:, 0, :], 0.0))
            for ti in range(1, NT):
                V(lambda e, ti=ti: e.tensor_tensor(out=PTb[:, ti, :], in0=PTb[:, ti - 1, :], in1=TTb[:, ti - 1, :], op=ALU.add))
            V(lambda e: e.tensor_tensor(out=cnt[:, :], in0=PTb[:, NT - 1, :], in1=TTb[:, NT - 1, :], op=ALU.add))
            V(lambda e: e.tensor_tensor(out=rank[:, :, :], in0=rank[:, :, :], in1=PTb[:, :, :], op=ALU.add))
            V(lambda e: e.tensor_tensor(out=cmpj[:, :, :], in0=cnt[:, :].unsqueeze(2).to_broadcast([128, 32, 16]),
                                        in1=hc[:, 0:16].unsqueeze(1).to_broadcast([128, 32, 16]), op=ALU.is_gt))
            V(lambda e: e.tensor_reduce(out=nblk[:, :], in_=cmpj[:, :, :], axis=AX.X, op=ALU.add))
            V(lambda e: e.memset(sblk[:, 0:1], 0.0))
            for ex in range(1, 32):
                V(lambda e, ex=ex: e.tensor_tensor(out=sblk[:, ex:ex + 1], in0=sblk[:, ex - 1:ex], in1=nblk[:, ex - 1:ex], op=ALU.add))
            V(lambda e: e.tensor_tensor(out=eblk[:, :], in0=sblk[:, :], in1=nblk[:, :], op=ALU.add))
            V(lambda e: e.tensor_tensor(out=cmpb[:, :, :], in0=eblk[:, :].unsqueeze(1).to_broadcast([128, NB, 32]),
                                        in1=hc[:, 16:16 + NB].unsqueeze(2).to_broadcast([128, NB, 32]), op=ALU.is_le))
            V(lambda e: e.tensor_reduce(out=ebf[:, :], in_=cmpb[:, :, :], axis=AX.X, op=ALU.add))
            V(lambda e: e.tensor_scalar(out=ebf[:, :], in0=ebf[:, :], scalar1=31.0, scalar2=128.0, op0=ALU.min, op1=ALU.mult))
            V(lambda e: e.tensor_tensor(out=ebf[:, :], in0=ebf[:, :], in1=hc[:, 80:81].to_broadcast([128, NB]), op=ALU.add))
            V(lambda e: e.tensor_copy(out=widx[:, 0, :], in_=ebf[:, :]))
            V(lambda e: e.tensor_scalar(out=ebf[:, :], in0=ebf[:, :], scalar1=1.0, scalar2=None, op0=ALU.add))
            V(lambda e: e.tensor_copy(out=widx[:, 1, :], in_=ebf[:, :]))
            V(lambda e: e.tensor_scalar(out=sblk[:, :], in0=sblk[:, :], scalar1=float(BS), scalar2=None, op0=ALU.mult))
            V(lambda e: e.tensor_tensor(out=rank[:, :, :], in0=rank[:, :, :],
                                        in1=sblk[:, :].unsqueeze(1).to_broadcast([128, NT, 32]), op=ALU.add))
            for j, OH in enumerate((OH1, OH2)):
                V(lambda e, OH=OH: e.tensor_tensor(out=tmpr[:, :, :], in0=rank[:, :, :], in1=OH[:, :, :], op=ALU.mult))
                V(lambda e, j=j: e.tensor_reduce(out=slotf[:, j, :], in_=tmpr[:, :, :], axis=AX.X, op=ALU.add))
            V(lambda e: e.tensor_copy(out=sloti[:, 0, :], in_=slotf[:, 0, :]))
            V(lambda e: e.tensor_copy(out=sloti[:, 1, :], in_=slotf[:, 1, :]))
        if "x1" in dbg:
            P.barrier()
            ds_dbg5 = P.dsem("ds_dbg5")
            P.dma("sp", ds_dbg5, lambda e: e.dma_start(out=dbg_d["x1"][:, :], in_=x1_s[:, :]))
            P.dma("sp", ds_dbg5, lambda e: e.dma_start(out=dbg_d["lg"][:, :, :], in_=lgall[:, :, :]), reads=[lgall.p()])
            P.dma("sp", ds_dbg5, lambda e: e.dma_start(out=dbg_d["slot"][:, :, :], in_=sloti[:, :, :]), reads=[sloti.p()])
            P.dma("sp", ds_dbg5, lambda e: e.dma_start(out=dbg_d["widx"][:, :, :], in_=widx[:, :, :]), reads=[widx.p()])
            P.dma("sp", ds_dbg5, lambda e: e.dma_start(out=dbg_d["gates"][:, :, :], in_=gates[:, :, :]), reads=[gates.p()])
            if "noroute" not in dbg:
                P.dma("sp", ds_dbg5, lambda e: e.dma_start(out=dbg_d["slotf"][:, :, :], in_=slotf[:, :, :]), reads=[slotf.p()])
        phE.__exit__()

        if "nomoe" not in dbg:
            phF1 = Phase().__enter__()
            NH = 4
            hsb = [sb("hsb%d" % i, [128, D], BF16) for i in range(NH)]
            ds_hsb = [P.dsem("ds_hsb%d" % i) for i in range(NH)]
            ds_sc = [P.dsem("ds_sc%d" % i) for i in range(NH)]

            def f1_load(i):
                hb_ = hsb[i % NH]
                rows = slice(i * 128, (i + 1) * 128)
                P.dma("sp", ds_hsb[i % NH], lambda e, hb_=hb_, rows=rows: e.dma_start(out=hb_[:, :], in_=h_s[rows, :]),
                      writes=[hb_.p()])
            for i in range(min(NH - 1, NT)):
                f1_load(i)
            for i in range(NT):
                if i + NH - 1 < NT:
                    f1_load(i + NH - 1)
                hb_ = hsb[i % NH]
                for j in range(2):
                    P.dma("pool", ds_sc[i % NH], lambda e, hb_=hb_, i=i, j=j: e.indirect_dma_start(
                        out=hs_sorted[:, :], out_offset=bass.IndirectOffsetOnAxis(ap=sloti[:, j, i:i + 1], axis=0),
                        in_=hb_[:, :], in_offset=None), reads=[hb_.p(), sloti.p(), t_hs0])
            phF1.__exit__()
            phF2 = Phase().__enter__()
            weg_v, weu_v, wed_v = wgs, wus, wds
            hs_v = hs_sorted.rearrange("(b s p) d -> b p s d", s=4, p=128)
            ys_v = y_sorted.rearrange("(b s p) d -> b p s d", s=4, p=128)

            def f2_chain(ch):
                kb = banks[4 * ch:4 * ch + 4]
                Wg = [sb("Wg%d_%d" % (ch, i), [128, KC * 512], BF16) for i in range(2)]
                Wu = [sb("Wu%d_%d" % (ch, i), [128, KC * 512], BF16) for i in range(2)]
                Wd = [sb("Wd%d_%d" % (ch, i), [128, 4 * D], BF16) for i in range(2)]
                ds_w = [P.dsem("ds_w%d_%d" % (ch, i)) for i in range(2)]
                hsblk = [sb("hsblk%d_%d" % (ch, i), [128, 4, D], BF16) for i in range(2)]
                ds_hb = [P.dsem("ds_hblk%d_%d" % (ch, i)) for i in range(2)]
                hTblk = sb("hTblk%d" % ch, [128, KC, BS], BF16)
                actT = sb("actT%d" % ch, [128, 4, BS], BF16, nparts=4)
                sgb = [sb("sgb%d_%d" % (ch, i), [128, 512]) for i in range(2)]
                yblk = [sb("yblk%d_%d" % (ch, i), [128, D]) for i in range(2)]
                ds_yb = [P.dsem("ds_yb%d_%d" % (ch, i)) for i in range(2)]
                blocks = list(range(ch, NB, 2))

                def fetch(n):
                    b = blocks[n]
                    par = n % 2
                    for (wt, src) in ((Wg[par], weg_v), (Wu[par], weu_v), (Wd[par], wed_v)):
                        P.dma("pool", ds_w[par], lambda e, wt=wt, src=src, b=b: e.indirect_dma_start(
                            out=wt[:, :], out_offset=None, in_=src[:, :],
                            in_offset=bass.IndirectOffsetOnAxis(ap=widx[:, 0, b:b + 1], axis=0)),
                            reads=[widx.p(), t_wcast], writes=[wt.p()])
                    hb_ = hsblk[par]
                    P.dma("sp", ds_hb[par], lambda e, hb_=hb_, b=b: e.dma_start(out=hb_[:, :, :], in_=hs_v[b]), writes=[hb_.p()])
                fetch(0)
                yield 1
                for n, b in enumerate(blocks):
                    par = n % 2
                    if n + 1 < len(blocks):
                        fetch(n + 1)
                    wg, wu, wd, hb_ = Wg[par], Wu[par], Wd[par], hsblk[par]
                    for j2 in range(4):
                        pt = kb[j2]
                        ptb = pt.t[:, :].bitcast(BF16)
                        for kk in range(2):
                            kc = 2 * j2 + kk
                            for s_ in range(4):
                                P.op("pe", lambda e, ptb=ptb, hb_=hb_, kk=kk, kc=kc, s_=s_: e.transpose(
                                    ptb[:, kk * 512 + s_ * 128:kk * 512 + (s_ + 1) * 128], hb_[:, s_, kc * 128:(kc + 1) * 128],
                                    ident_b[:, :]), reads=[hb_.p(), ident_b.p()], writes=[pt.p()])
                        if j2 % 2 == 0:
                            P.op("act", lambda e, ptb=ptb, j2=j2: e.copy(
                                out=hTblk[:, 2 * j2:2 * j2 + 2, :], in_=ptb.rearrange("p (k t) -> p k t", k=2)),
                                reads=[pt.p()], writes=[hTblk.p()])
                        else:
                            P.op("dve", lambda e, ptb=ptb, j2=j2: e.tensor_copy(
                                out=hTblk[:, 2 * j2:2 * j2 + 2, :], in_=ptb.rearrange("p (k t) -> p k t", k=2)),
                                reads=[pt.p()], writes=[hTblk.p()])
                        yield 0
                    for fc in range(4):
                        pg, pu = kb[fc % 2], kb[2 + fc % 2]
                        sg_ = sgb[fc % 2]
                        for kc in range(KC):
                            P.op("pe", lambda e, pg=pg, wg=wg, kc=kc, fc=fc: e.matmul(
                                pg[:, :], lhsT=wg[:, kc * 512 + fc * 128:kc * 512 + (fc + 1) * 128], rhs=hTblk[:, kc, :],
                                start=(kc == 0), stop=(kc == KC - 1)), reads=[wg.p(), hTblk.p()], writes=[pg.p()])
                        yield 0
                        for kc in range(KC):
                            P.op("pe", lambda e, pu=pu, wu=wu, kc=kc, fc=fc: e.matmul(
                                pu[:, :], lhsT=wu[:, kc * 512 + fc * 128:kc * 512 + (fc + 1) * 128], rhs=hTblk[:, kc, :],
                                start=(kc == 0), stop=(kc == KC - 1)), reads=[wu.p(), hTblk.p()], writes=[pu.p()])
                        P.op("act", lambda e, pg=pg, sg_=sg_: e.activation(out=sg_[:, :], in_=pg[:, :], func=AF.Silu),
                             reads=[pg.p()], writes=[sg_.p()])
                        yield 0
                        P.op("dve", lambda e, pu=pu, sg_=sg_, fc=fc: e.tensor_tensor(
                            out=actT[:, fc, :], in0=pu[:, :], in1=sg_[:, :], op=ALU.mult),
                            reads=[pu.p(), sg_.p()], writes=[actT.p(fc)])
                    for s_ in range(4):
                        for nb in range(2):
                            pd = kb[(2 * s_ + nb) % 4]
                            for fc in range(4):
                                P.op("pe", lambda e, pd=pd, fc=fc, s_=s_, nb=nb, wd=wd: e.matmul(
                                    pd[:, :], lhsT=actT[:, fc, s_ * 128:(s_ + 1) * 128],
                                    rhs=wd[:, fc * D + nb * 512:fc * D + (nb + 1) * 512],
                                    start=(fc == 0), stop=(fc == 3)), reads=[actT.p(fc), wd.p()], writes=[pd.p()])
                            yb_ = yblk[s_ % 2]
                            if nb == 0:
                                P.op("act", lambda e, pd=pd, yb_=yb_, nb=nb: e.copy(
                                    out=yb_[:, nb * 512:(nb + 1) * 512], in_=pd[:, :]), reads=[pd.p()], writes=[yb_.p()])
                            else:
                                P.op("dve", lambda e, pd=pd, yb_=yb_, nb=nb: e.tensor_copy(
                                    out=yb_[:, nb * 512:(nb + 1) * 512], in_=pd[:, :]), reads=[pd.p()], writes=[yb_.p()])
                            yield 0
                        P.dma("sp", ds_yb[s_ % 2], lambda e, b=b, s_=s_, yb_=yb_: e.dma_start(
                            out=ys_v[b][:, s_, :], in_=yb_[:, :]), reads=[yb_.p()])
                    yield 1

            interleave([f2_chain(0), f2_chain(1)])
            phF2.__exit__()
            phF = Phase().__enter__()
            NF = 3
            ya = [sb("ya%d" % i, [128, D]) for i in range(NF)]
            yb2 = [sb("yb2_%d" % i, [128, D]) for i in range(NF)]
            x1f = [sb("x1f%d" % i, [128, D]) for i in range(NF)]
            ds_ya = [P.dsem("ds_ya%d" % i) for i in range(NF)]
            ds_x1f = [P.dsem("ds_x1f%d" % i) for i in range(NF)]
            of = [sb("of%d" % i, [128, D]) for i in range(2)]
            of2 = [sb("of2_%d" % i, [128, D]) for i in range(2)]
            ds_out = [P.dsem("ds_out%d" % i) for i in range(2)]
            fnb = sb("fnb", [128, D])
            ssf = sb("ssf", [128, 4])
            ds_f = P.dsem("ds_f")
            P.dma("sp", ds_f, lambda e: e.dma_start(out=fnb[:, :], in_=rbig_d[:, 2, :]), writes=[fnb.p()])

            def f3_load(i):
                q = i % NF
                rows = slice(i * 128, (i + 1) * 128)
                P.dma("sp", ds_x1f[q], lambda e, rows=rows, q=q: e.dma_start(out=x1f[q][:, :], in_=x1_s[rows, :]),
                      writes=[x1f[q].p()])
                for j, yt_ in enumerate((ya[q], yb2[q])):
                    P.dma("pool", ds_ya[q], lambda e, yt_=yt_, i=i, j=j: e.indirect_dma_start(
                        out=yt_[:, :], out_offset=None, in_=y_sorted[:, :],
                        in_offset=bass.IndirectOffsetOnAxis(ap=sloti[:, j, i:i + 1], axis=0)),
                        reads=[sloti.p()], writes=[yt_.p()])
            for i in range(NF - 1):
                f3_load(i)
            for i in range(NT):
                if i + NF - 1 < NT:
                    f3_load(i + NF - 1)
                q = i % NF
                par = i % 2
                rows = slice(i * 128, (i + 1) * 128)
                o1, o2 = of[par], of2[par]
                P.op("dve", lambda e, q=q, i=i, o1=o1: e.tensor_scalar(
                    out=o1[:, :], in0=ya[q][:, :], scalar1=gates[:, 0, i:i + 1], scalar2=None, op0=ALU.mult),
                    reads=[ya[q].p(), gates.p()], writes=[o1.p()])
                P.op("dve", lambda e, q=q, i=i, o1=o1: e.scalar_tensor_tensor(
                    out=o1[:, :], in0=yb2[q][:, :], scalar=gates[:, 1, i:i + 1], in1=o1[:, :], op0=ALU.mult, op1=ALU.add),
                    reads=[yb2[q].p(), gates.p(), o1.p()], writes=[o1.p()])
                P.op("pool", lambda e, o1=o1: e.tensor_tensor(out=o1[:, :], in0=o1[:, :], in1=g12[:, 1, :], op=ALU.mult),
                     reads=[o1.p(), g12.p()], writes=[o1.p()])
                P.op("pool", lambda e, q=q, o1=o1: e.tensor_tensor(out=o1[:, :], in0=o1[:, :], in1=x1f[q][:, :], op=ALU.add),
                     reads=[o1.p(), x1f[q].p()], writes=[o1.p()])
                P.op("act", lambda e, o1=o1, o2=o2, par=par: e.activation(out=o2[:, :], in_=o1[:, :], func=AF.Square,
                                                                          accum_out=ssf[:, 2 * par:2 * par + 1]),
                     reads=[o1.p()], writes=[o2.p(), ssf.p()])
                P.op("act", lambda e, par=par: e.activation(out=ssf[:, 2 * par + 1:2 * par + 2], in_=ssf[:, 2 * par:2 * par + 1],
                                                            func=AF.Sqrt, scale=1.0 / D, bias=epsb[:, 0:1]),
                     reads=[ssf.p(), epsb.p()], writes=[ssf.p()])
                P.op("dve", lambda e, par=par: e.reciprocal(out=ssf[:, 2 * par + 1:2 * par + 2], in_=ssf[:, 2 * par + 1:2 * par + 2]),
                     reads=[ssf.p()], writes=[ssf.p()])
                P.op("dve", lambda e, o1=o1, o2=o2, par=par: e.scalar_tensor_tensor(
                    out=o2[:, :], in0=o1[:, :], scalar=ssf[:, 2 * par + 1:2 * par + 2], in1=fnb[:, :],
                    op0=ALU.mult, op1=ALU.mult), reads=[o1.p(), ssf.p(), fnb.p()], writes=[o2.p()])
                P.dma("sp", ds_out[par], lambda e, rows=rows, o2=o2: e.dma_start(out=out_d[rows, :], in_=o2[:, :]), reads=[o2.p()])
            phF.__exit__()
        P.barrier(final=True)
        P.emit(block)
    return nc


def _prep_inputs(inputs, b):
    g = lambda n: np.asarray(inputs[n], dtype=np.float32)
    c = _get_consts()
    m = {}
    m["x"] = np.ascontiguousarray(g("x")[b])
    m["ctx"] = np.ascontiguousarray(g("ctx")[b])
    cv = np.stack([g("c")[b].reshape(KC, 128).T, g("c_ctx").reshape(KC, 128).T], axis=-1)
    m["cvec"] = np.ascontiguousarray(cv)
    m["w_mod"] = np.ascontiguousarray(g("w_mod")[0])
    pp = np.zeros((128, 128), np.float32)
    pp[:, 0:8] = g("norm1")[0].reshape(KC, 128).T
    pp[:, 8:56] = g("b_mod")[0].reshape(48, 128).T
    pp[:, 56:64] = g("norm2")[0].reshape(KC, 128).T
    pp[:, 64:72] = g("ssd_norm")[0].reshape(KC, 128).T
    m["pp"] = pp
    m["w_in"] = np.ascontiguousarray(g("w_in")[0])
    cw = g("conv_w")[0]
    cb = g("conv_b")[0]
    cpar = np.zeros((128, 16, 6), np.float32)
    cpar[:, :, 0:5] = cw.reshape(5, 16, 128).transpose(2, 1, 0)
    cpar[:, :, 5] = cb.reshape(16, 128).T
    m["cpar"] = cpar
    rp = np.zeros((128, 128), np.float32)
    rp[:, 0:32] = g("dt_bias")[0].reshape(1, 32)
    rp[:, 32:64] = g("a_log")[0].reshape(1, 32)
    rp[:, 64:80] = g("d_skip")[0].reshape(1, 16)
    rp[:, 80:84] = g("b_rg")[0].reshape(1, 4)
    rp[:, 84:116] = g("b_re")[0].reshape(1, 32)
    rbig = np.zeros((128, 6, D), np.float32)
    rbig[:, 0, :] = g("b_mod")[0][2 * D:3 * D][None, :]
    rbig[:, 1, :] = g("b_mod")[0][5 * D:6 * D][None, :]
    rbig[:, 2, :] = g("final_norm")[None, :]
    rbig[:, 3, :] = g("b_mod")[0][3 * D:4 * D][None, :]
    rbig[:, 4, :] = g("b_mod")[0][4 * D:5 * D][None, :]
    rbig[:, 5, :] = g("norm2")[0][None, :]
    m["rbig"] = rbig
    m["w_four"] = np.ascontiguousarray(g("w_four")[0])
    m["w_out"] = np.ascontiguousarray(g("w_out")[0])
    wr = np.concatenate([g("w_rg")[0], g("w_re")[0].transpose(1, 0, 2).reshape(D, 32)], axis=1)
    m["wr"] = np.ascontiguousarray(wr.reshape(KC, 128, 36).transpose(1, 0, 2))
    m["w_eg"] = np.ascontiguousarray(g("w_eg")[0].reshape(32, KC, 128, 512).transpose(0, 2, 1, 3))
    m["w_eu"] = np.ascontiguousarray(g("w_eu")[0].reshape(32, KC, 128, 512).transpose(0, 2, 1, 3))
    m["w_ed"] = np.ascontiguousarray(g("w_ed")[0].reshape(32, 4, 128, D).transpose(0, 2, 1, 3))
    hc = np.zeros((128, 128), np.float32)
    hc[:, 0:16] = (512.0 * np.arange(16))[None, :]
    hc[:, 16:64] = np.arange(48, dtype=np.float32)[None, :]
    hc[:, 80] = np.arange(128, dtype=np.float32)
    m["hc"] = hc
    m["zeros"] = np.zeros((2048, D), dtype=ml_dtypes.bfloat16)
    m["cs"] = c["cs"]
    m["dft_tab"] = c["dft_tab"]
    m["rp"] = rp
    m["masks"] = np.ascontiguousarray(np.stack([c["m_le"], c["m_gt"], c["m_ge"], c["m_lt"], c["ones"]], axis=1))
    m["ident_f"] = c["ident_f"]
    m["ident_b"] = c["ident_b"]
    return m


def kernel(**inputs):
    nc = build()
    in_maps = [_prep_inputs(inputs, b) for b in range(8)]
    res = run_bass_kernel_spmd(nc, in_maps, core_ids=list(range(8)))
    return np.stack([r["out"] for r in res.results], axis=0)
```

```python
import contextlib
import numpy as np
import ml_dtypes
import concourse.bass as bass
import concourse.mybir as mybir
from concourse.bass_utils import run_bass_kernel_spmd

F32 = mybir.dt.float32
BF16 = mybir.dt.bfloat16
ALU = mybir.AluOpType
AF = mybir.ActivationFunctionType
AX = mybir.AxisListType

D = 1024
KC = 8
T = 4096
TC = 256
TA = T + TC
NT = T // 128
NTA = TA // 128
DPROJ = 3616
EPS = 1e-6
ENGS = ("pe", "act", "dve", "pool", "sp")


class Tile:
    __slots__ = ("name", "w", "r")

    def __init__(self, name):
        self.name = name
        self.w = None
        self.r = {}


class DmaSem:
    def __init__(self, sem):
        self.sem = sem
        self.count = 0


class Ins:
    __slots__ = ("fn", "signal", "rank", "eng")

    def __init__(self, fn, eng):
        self.fn = fn
        self.eng = eng
        self.signal = False
        self.rank = None


class Prog:
    def __init__(self, nc):
        self.nc = nc
        self.ops = {e: [] for e in ENGS}
        self.known = {e: {} for e in ENGS}
        self.esem = {}
        self.stack = contextlib.ExitStack()
        for e in ENGS:
            self.esem[e] = self.stack.enter_context(nc.semaphore("s_" + e))
        self.dma_sems = []
        self.bg_sems = []
        self.last_ins = {e: None for e in ENGS}
        self.nins = {e: 0 for e in ENGS}

    def dsem(self, name, barrier=True):
        s = DmaSem(self.stack.enter_context(self.nc.semaphore(name)))
        if barrier:
            self.dma_sems.append(s)
        else:
            self.bg_sems.append(s)
        return s

    def _need(self, eng, tok):
        if tok is None:
            return
        if tok[0] == "e":
            ins = tok[1]
            if ins.eng == eng and eng == "pe":
                return
            key = ("e", ins.eng)
            idx = ins.rank
            if self.known[eng].get(key, -1) >= idx:
                return
            self.known[eng][key] = idx
            ins.signal = True
            self.ops[eng].append(("wait_e", ins))
        else:
            _, ds, val = tok
            val = max(val, ds.count)
            key = ("d", id(ds))
            if self.known[eng].get(key, -1) >= val:
                return
            self.known[eng][key] = val
            self.ops[eng].append(("wait_d", ds, val))

    def _deps(self, eng, reads, writes):
        for t in reads:
            self._need(eng, t.w)
        for t in writes:
            self._need(eng, t.w)
            for r in t.r.values():
                self._need(eng, r)

    def _commit(self, tok, reads, writes):
        key = ("e", tok[1].eng) if tok[0] == "e" else ("d", id(tok[1]))
        for t in reads:
            t.r[key] = tok
        for t in writes:
            t.w = tok
            t.r = {}

    def op(self, eng, fn, reads=(), writes=()):
        self._deps(eng, reads, writes)
        ins = Ins(fn, eng)
        ins.rank = self.nins[eng]
        self.nins[eng] += 1
        self.ops[eng].append(("ins", ins))
        self.last_ins[eng] = ins
        self._commit(("e", ins), reads, writes)
        return ins

    def dma(self, eng, ds, fn, reads=(), writes=()):
        self._deps(eng, reads, writes)
        ds.count += 16
        self.ops[eng].append(("dma", fn, ds))
        self._commit(("d", ds, ds.count), reads, writes)

    def barrier(self, final=False):
        toks = []
        if final:
            for ds in self.bg_sems:
                if ds.count:
                    toks.append(("d", ds, ds.count))
        for e in ENGS:
            if self.last_ins[e] is not None:
                toks.append(("e", self.last_ins[e]))
        for ds in self.dma_sems:
            if ds.count:
                toks.append(("d", ds, ds.count))
        for e in ENGS:
            for tk in toks:
                if tk[0] == "e" and tk[1].eng == e and e in ("pe", "sp"):
                    continue
                self._need(e, tk)

    def emit(self, block):
        sigcount = {}
        for e in ENGS:
            c = 0
            for o in self.ops[e]:
                if o[0] == "ins":
                    if o[1].signal:
                        c += 1
                        sigcount[id(o[1])] = c
        engs = {"pe": block.tensor, "act": block.scalar, "dve": block.vector,
                "pool": block.gpsimd, "sp": block.sync}
        esem = self.esem

        def make(e):
            ops = self.ops[e]

            def body(eng):
                for o in ops:
                    if o[0] == "ins":
                        r = o[1].fn(eng)
                        if o[1].signal:
                            r.then_inc(esem[e], 1)
                    elif o[0] == "dma":
                        o[1](eng).then_inc(o[2].sem, 16)
                    elif o[0] == "wait_e":
                        eng.wait_ge(esem[o[1].eng], sigcount[id(o[1])])
                    else:
                        eng.wait_ge(o[1].sem, o[2])
            return body

        for e in ENGS:
            engs[e](make(e))


class Buf:
    def __init__(self, t, name, nparts=1):
        self.t = t
        self.tiles = [Tile("%s.%d" % (name, i)) for i in range(nparts)]

    def __getitem__(self, k):
        return self.t[k]

    def p(self, i=0):
        return self.tiles[i]

    def all(self):
        return list(self.tiles)


def _consts():
    k = np.arange(128)
    c = {}
    c["ident_f"] = np.eye(128, dtype=np.float32)
    c["ident_b"] = np.eye(128, dtype=np.float32).astype(ml_dtypes.bfloat16)
    c["m_le"] = (k[:, None] <= k[None, :]).astype(np.float32)
    c["m_gt"] = (k[:, None] > k[None, :]).astype(np.float32)
    c["m_ge"] = (k[:, None] >= k[None, :]).astype(np.float32)
    c["m_lt"] = (k[:, None] < k[None, :]).astype(np.float32)
    c["ones"] = np.ones((128, 128), np.float32)
    cc = (k[:, None] * k[None, :]) % 128
    ang = 2.0 * np.pi * cc / 128.0
    c["cs"] = np.concatenate([np.cos(ang), -np.sin(ang)], axis=1).astype(np.float32).astype(ml_dtypes.bfloat16)
    n = (128 * np.arange(32)[None, :] + k[:, None]).astype(np.int64)
    kk = (512 * np.arange(8)[:, None] + np.arange(512)[None, :]).astype(np.int64)
    m = (n[None, :, :, None] * kk[:, None, None, :]) % 4096
    a = (2.0 * np.pi / 4096.0) * m.astype(np.float64)
    tab = np.stack([np.cos(a), np.sin(a)], axis=3)
    c["dft_tab"] = np.ascontiguousarray(tab.astype(np.float32).astype(ml_dtypes.bfloat16))
    return c


_CONSTS = None


def _get_consts():
    global _CONSTS
    if _CONSTS is None:
        _CONSTS = _consts()
    return _CONSTS


def build(dbg=()):
    nc = bass.Bass("TRN2", target_bir_lowering=False)
    dbg = set(dbg)

    def din(name, shape, dt=F32):
        return nc.dram_tensor(name, list(shape), dt, kind="ExternalInput").ap()

    def dout(name, shape, dt=F32):
        return nc.dram_tensor(name, list(shape), dt, kind="ExternalOutput").ap()

    x_d = din("x", [T, D])
    ctx_d = din("ctx", [TC, D])
    cvec_d = din("cvec", [128, KC, 2])
    w_mod_d = din("w_mod", [D, 6 * D])
    pp_d = din("pp", [128, 128])
    ident_f_d = din("ident_f", [128, 128])
    ident_b_d = din("ident_b", [128, 128], BF16)
    w_in_d = din("w_in", [D, DPROJ])
    cpar_d = din("cpar", [128, 16, 6])
    rbig_d = din("rbig", [128, 6, D])
    rp_d = din("rp", [128, 128])
    masks_d = din("masks", [128, 5, 128])
    w_four_d = din("w_four", [4, 128, 128])
    w_out_d = din("w_out", [1536, D])
    wr_d = din("wr", [128, KC, 36])
    w_eg_d = din("w_eg", [32, 128, KC, 512])
    w_eu_d = din("w_eu", [32, 128, KC, 512])
    w_ed_d = din("w_ed", [32, 128, 4, D])
    cs_d = din("cs", [128, 256], BF16)
    dft_d = din("dft_tab", [8, 128, 32, 2, 512], BF16)
    out_d = dout("out", [T, D])
    f_s = nc.dram_tensor("f_s", [4, 128, T], BF16).ap()
    four_s = nc.dram_tensor("four_s", [4, 128, T], BF16).ap()
    x1_s = nc.dram_tensor("x1_s", [T, D], F32).ap()
    h_s = nc.dram_tensor("h_s", [T, D], BF16).ap()
    hs_sorted = nc.dram_tensor("hs_sorted", [48 * 512, D], BF16).ap()
    y_sorted = nc.dram_tensor("y_sorted", [48 * 512, D], F32).ap()
    hc_d = din("hc", [128, 128])
    zeros_d = din("zeros", [2048, D], BF16)
    wgs = nc.dram_tensor("wgs", [32 * 128, 4096], BF16).ap()
    wus = nc.dram_tensor("wus", [32 * 128, 4096], BF16).ap()
    wds = nc.dram_tensor("wds", [32 * 128, 4096], BF16).ap()
    bc_s = nc.dram_tensor("bc_s", [8, 128, TA], BF16).ap()
    xs_s = nc.dram_tensor("xs_s", [TA, 1024], BF16).ap()
    bt_s = nc.dram_tensor("bt_s", [TA, 512], BF16).ap()
    y_s = nc.dram_tensor("y_s", [T, 1024], F32).ap()
    yb_s = nc.dram_tensor("yb_s", [T, 1024], F32).ap()
    dbg_d = {}
    if "uT" in dbg:
        dbg_d["uT"] = dout("dbg_uT", [128, KC, TA], BF16)
    if "ssd" in dbg:
        dbg_d["y"] = dout("dbg_y", [T, 1024])
        dbg_d["yb"] = dout("dbg_yb", [T, 1024])
    if "four" in dbg:
        dbg_d["four"] = dout("dbg_four", [4, 128, T], BF16)
    if "x1" in dbg:
        dbg_d["x1"] = dout("dbg_x1", [T, D])
        dbg_d["lg"] = dout("dbg_lg", [128, NT, 36])
        dbg_d["slot"] = dout("dbg_slot", [128, 2, NT], mybir.dt.int32)
        dbg_d["widx"] = dout("dbg_widx", [128, 2, 48], mybir.dt.int32)
        dbg_d["gates"] = dout("dbg_gates", [128, 2, NT])
        dbg_d["slotf"] = dout("dbg_slotf", [128, 2, NT])
    if "conv" in dbg:
        dbg_d["bc"] = dout("dbg_bc", [8, 128, TA], BF16)
        dbg_d["xs"] = dout("dbg_xs", [TA, 1024], BF16)
        dbg_d["bt"] = dout("dbg_bt", [TA, 512], BF16)
        dbg_d["dts"] = dout("dbg_dts", [128, 7, NTA * 32])

    P = Prog(nc)
    st = contextlib.ExitStack()

    cur = [st]

    def sb(name, shape, dt=F32, nparts=1):
        return Buf(cur[0].enter_context(nc.sbuf_tensor("sb_" + name, list(shape), dt)), name, nparts)

    def ps(name, shape, dt=F32, nparts=1):
        return Buf(st.enter_context(nc.psum_tensor("ps_" + name, list(shape), dt)), name, nparts)

    class Phase:
        def __enter__(self):
            self.prev = cur[0]
            self.stk = contextlib.ExitStack()
            cur[0] = self.stk
            return self

        def __exit__(self, *a):
            P.barrier()
            cur[0] = self.prev
            self.stk.close()
            return False

    stp = contextlib.ExitStack()

    def sbp(name, shape, dt=F32, nparts=1):
        return Buf(stp.enter_context(nc.sbuf_tensor("sb_" + name, list(shape), dt)), name, nparts)

    with P.stack, stp, st, nc.Block() as block:
        ident_f = sb("ident_f", [128, 128])
        ident_b = sb("ident_b", [128, 128], BF16)
        pp = sb("pp", [128, 128])
        cvec = sb("cvec", [128, KC, 2])
        svec = sb("svec", [128, KC, 2])
        ds_c = P.dsem("ds_c")
        P.dma("sp", ds_c, lambda e: e.dma_start(out=ident_f[:, :], in_=ident_f_d[:, :]), writes=[ident_f.p()])
        P.dma("sp", ds_c, lambda e: e.dma_start(out=ident_b[:, :], in_=ident_b_d[:, :]), writes=[ident_b.p()])
        P.dma("sp", ds_c, lambda e: e.dma_start(out=pp[:, :], in_=pp_d[:, :]), writes=[pp.p()])
        P.dma("sp", ds_c, lambda e: e.dma_start(out=cvec[:, :, :], in_=cvec_d[:, :, :]), writes=[cvec.p()])
        P.op("act", lambda e: e.activation(out=svec[:, :, :], in_=cvec[:, :, :], func=AF.Silu),
             reads=[cvec.p()], writes=[svec.p()])

        PP_N1, PP_BMOD = 0, 8

        banks = [ps("bank%d" % i, [128, 512]) for i in range(8)]
        psm = banks[0]
        pst = banks[1:3]
        psA = banks[3:7]
        modT = sb("modT", [128, 48, 2])
        scA = sb("scA", [128, KC, 2])
        epsb = sb("epsb", [128, 1])
        oneb = sb("oneb", [128, 1])
        P.op("pool", lambda e: e.memset(oneb[:, :], 1.0), writes=[oneb.p()])
        P.op("pool", lambda e: e.memset(epsb[:, :], EPS), writes=[epsb.p()])
        rp = sb("rp", [128, 128])
        g12 = sb("g12", [128, 4, D])
        rs = sb("rs", [128, NTA])
        scB = sb("scB", [128, KC])
        phA0 = Phase().__enter__()
        wm = [sb("wm%d" % i, [128, KC, 512], BF16) for i in range(3)]
        ds_wm = [P.dsem("ds_wm%d" % i) for i in range(3)]
        svecb = sb("svecb", [128, KC, 2], BF16)
        P.op("dve", lambda e: e.tensor_copy(out=svecb[:, :, :], in_=svec[:, :, :]), reads=[svec.p()], writes=[svecb.p()])
        w_mod_v = w_mod_d.rearrange("(kc p) n -> p kc n", p=128)
        for blk in range(12):
            b = wm[blk % 3]
            P.dma("pool", ds_wm[blk % 3],
                  lambda e, b=b, blk=blk: e.dma_start(out=b[:, :, :], in_=w_mod_v[:, :, blk * 512:(blk + 1) * 512]),
                  writes=[b.p()])
            for j in range(4):
                cj = blk * 4 + j
                for kc in range(KC):
                    P.op("pe", lambda e, b=b, j=j, kc=kc, cj=cj: e.matmul(
                        psm[:, cj * 2:cj * 2 + 2], lhsT=b[:, kc, j * 128:(j + 1) * 128], rhs=svecb[:, kc, :],
                        start=(kc == 0), stop=(kc == KC - 1)),
                        reads=[b.p(), svecb.p()], writes=[psm.p()])
        P.op("dve", lambda e: e.tensor_tensor(
            out=modT[:, :, :], in0=psm[:, 0:96].rearrange("p (c two) -> p c two", two=2),
            in1=pp[:, PP_BMOD:PP_BMOD + 48].unsqueeze(2).to_broadcast([128, 48, 2]), op=ALU.add),
            reads=[psm.p(), pp.p()], writes=[modT.p()])
        P.op("dve", lambda e: e.scalar_tensor_tensor(
            out=scA[:, :, :], in0=modT[:, 8:16, :], scalar=1.0,
            in1=pp[:, PP_N1:PP_N1 + 8].unsqueeze(2).to_broadcast([128, KC, 2]),
            op0=ALU.add, op1=ALU.mult),
            reads=[modT.p(), pp.p()], writes=[scA.p()])
        P.op("dve", lambda e: e.scalar_tensor_tensor(
            out=scB[:, :], in0=modT[:, 32:40, 0], scalar=1.0, in1=pp[:, 56:64], op0=ALU.add, op1=ALU.mult),
            reads=[modT.p(), pp.p()], writes=[scB.p()])
        bcl = [sb("bcl%d" % i, [128, 128]) for i in range(4)]
        srcs = [lambda c: modT[:, 16 + c, 0:1], lambda c: modT[:, 40 + c, 0:1], lambda c: modT[:, 24 + c, 0:1],
                lambda c: scB[:, c:c + 1]]
        nb_ = 0
        for gi in range(4):
            for hb in range(2):
                pgb = banks[1 + (nb_ % 2)]
                nb_ += 1
                for c4 in range(4):
                    c = hb * 4 + c4
                    bt_ = bcl[(gi * 8 + c) % 4]
                    P.op("dve", lambda e, bt_=bt_, gi=gi, c=c: e.tensor_copy(out=bt_[:, :], in_=srcs[gi](c).to_broadcast([128, 128])),
                         reads=[modT.p(), scB.p()], writes=[bt_.p()])
                    P.op("pe", lambda e, pgb=pgb, bt_=bt_, c4=c4: e.matmul(
                        pgb[:, c4 * 128:(c4 + 1) * 128], lhsT=bt_[:, :], rhs=ident_f[:, :], start=True, stop=True),
                        reads=[bt_.p(), ident_f.p()], writes=[pgb.p()])
                P.op("act", lambda e, pgb=pgb, gi=gi, hb=hb: e.copy(out=g12[:, gi, hb * 512:(hb + 1) * 512], in_=pgb[:, :]),
                     reads=[pgb.p()], writes=[g12.p()])
        phA0.__exit__()
        phX = Phase().__enter__()
        NDT = NTA * 32
        dts = sb("dts", [128, 7, NDT])
        masks = sb("masks", [128, 5, 128])
        phU = Phase().__enter__()
        uT = sb("uT", [128, KC, TA], BF16, nparts=NTA)
        phA1 = Phase().__enter__()
        xt = [sb("xt%d" % i, [128, D]) for i in range(3)]
        ds_x = [P.dsem("ds_x%d" % i) for i in range(3)]
        xn = [sb("xn%d" % i, [128, D], BF16) for i in range(2)]
        junk = sb("junk", [128, D])
        ss = sb("ss", [128, NTA])
        utmp = [sb("utmp%d" % i, [128, KC, 128]) for i in range(2)]
        def xsrc(i):
            return ctx_d[i * 128:(i + 1) * 128, :] if i < 2 else x_d[(i - 2) * 128:(i - 1) * 128, :]
        for i in range(NTA):
            xb = xt[i % 3]
            P.dma("sp", ds_x[i % 3], lambda e, xb=xb, src=xsrc(i): e.dma_start(out=xb[:, :], in_=src), writes=[xb.p()])
            P.op("act", lambda e, xb=xb, i=i: e.activation(out=junk[:, :], in_=xb[:, :], func=AF.Square,
                                                           accum_out=ss[:, i:i + 1]),
                 reads=[xb.p()], writes=[junk.p(), ss.p()])
        P.op("act", lambda e: e.activation(out=rs[:, :], in_=ss[:, :], func=AF.Sqrt, scale=1.0 / D, bias=epsb[:, 0:1]),
             reads=[ss.p(), epsb.p()], writes=[rs.p()])
        P.op("dve", lambda e: e.reciprocal(out=rs[:, :], in_=rs[:, :]), reads=[rs.p()], writes=[rs.p()])
        for i in range(NTA):
            xb = xt[i % 3]
            which = 1 if i < 2 else 0
            P.dma("sp", ds_x[i % 3], lambda e, xb=xb, src=xsrc(i): e.dma_start(out=xb[:, :], in_=src), writes=[xb.p()])
            xnb = xn[i % 2]
            P.op("act", lambda e, xb=xb, xnb=xnb, i=i: e.activation(out=xnb[:, :], in_=xb[:, :], func=AF.Copy,
                                                                    scale=rs[:, i:i + 1]),
                 reads=[xb.p(), rs.p()], writes=[xnb.p()])
            pt = pst[i % 2]
            ptb = pt.t[:, :].bitcast(BF16)
            for kc in range(KC):
                P.op("pe", lambda e, ptb=ptb, xnb=xnb, kc=kc: e.transpose(
                    ptb[:, kc * 128:(kc + 1) * 128], xnb[:, kc * 128:(kc + 1) * 128], ident_b[:, :]),
                    reads=[xnb.p(), ident_b.p()], writes=[pt.p()])
            ut = utmp[i % 2]
            P.op("dve", lambda e, ptb=ptb, ut=ut, which=which: e.tensor_tensor(
                out=ut[:, :, :], in0=ptb.rearrange("p (k t) -> p k t", k=KC),
                in1=scA[:, :, which:which + 1].to_broadcast([128, KC, 128]), op=ALU.mult),
                reads=[pt.p(), scA.p()], writes=[ut.p()])
            P.op("pool", lambda e, ut=ut, i=i, which=which: e.tensor_tensor(
                out=uT[:, :, i * 128:(i + 1) * 128], in0=ut[:, :, :],
                in1=modT[:, 0:8, which:which + 1].to_broadcast([128, KC, 128]), op=ALU.add),
                reads=[ut.p(), modT.p()], writes=[uT.p(i)])

        if "uT" in dbg:
            ds_dbg = P.dsem("ds_dbg")
            P.dma("sp", ds_dbg, lambda e: e.dma_start(out=dbg_d["uT"][:, :, :], in_=uT[:, :, :]),
                  reads=uT.all())

        phA1.__exit__()
        phB = Phase().__enter__()
        RP_DTB, RP_ALOG, RP_DSKIP = 0, 32, 64
        cpar = sb("cpar", [128, 16, 6])
        ds_c3 = P.dsem("ds_c3")
        P.dma("sp", ds_c3, lambda e: e.dma_start(out=cpar[:, :, :], in_=cpar_d[:, :, :]), writes=[cpar.p()])
        P.dma("sp", ds_c3, lambda e: e.dma_start(out=rp[:, :], in_=rp_d[:, :]), writes=[rp.p()])
        P.dma("sp", ds_c3, lambda e: e.dma_start(out=masks[:, :, :], in_=masks_d[:, :, :]), writes=[masks.p()])
        w_in_v = w_in_d.rearrange("(kc p) n -> p kc n", p=128)
        wcb = [sb("wcb%d" % i, [128, KC, 512], BF16) for i in range(2)]
        ds_pc = [P.dsem("ds_pc%d" % i, barrier=False) for i in range(4)]
        t_pc = [Tile("pc%d" % i) for i in range(4)]
        t_wcast = Tile("wcast")
        pc_list = []
        for (src_, dst_) in ((w_eg_d, wgs), (w_eu_d, wus), (w_ed_d, wds)):
            sv_ = src_.rearrange("e p k f -> e p (k f)")
            for ex in range(32):
                pc_list.append((sv_, dst_, ex))
        pc_pos = [0]

        def precast_issue(n):
            for _ in range(n):
                if pc_pos[0] >= len(pc_list):
                    return
                sv_, dst_, ex = pc_list[pc_pos[0]]
                k = pc_pos[0] % 4
                pc_pos[0] += 1
                P.dma("pool", ds_pc[k], lambda e, sv_=sv_, dst_=dst_, ex=ex: e.dma_start(
                    out=dst_[ex * 128:(ex + 1) * 128, :], in_=sv_[ex]), writes=[t_pc[k], t_wcast])
        ds_wc = [P.dsem("ds_wc%d" % i) for i in range(2)]
        pre = [sb("pre%d" % i, [128, TA + 8], BF16) for i in range(2)]
        acc = [sb("acc%d" % i, [128, TA + 4]) for i in range(1)] * 2
        post = [sb("post%d" % i, [128, TA], BF16) for i in range(2)]
        tokb = [sb("tokb%d" % i, [128, NTA, 128], BF16) for i in range(1)] * 2
        ds_post = [P.dsem("ds_post%d" % i) for i in range(2)]
        ds_tokb = [P.dsem("ds_tokb%d" % i) for i in range(2)]
        for pb in pre:
            P.op("pool", lambda e, pb=pb: e.memset(pb[:, :], 0.0), writes=[pb.p()])
        xs_v = xs_s.rearrange("(i p) c -> p i c", p=128)
        bt_v = bt_s.rearrange("(i p) c -> p i c", p=128)
        CL = TA + 4
        for cc in range(16):
            if cc % 4 == 0:
                wb = wcb[(cc // 4) % 2]
                c0 = 1024 + 128 * cc
                P.dma("pool", ds_wc[(cc // 4) % 2],
                      lambda e, wb=wb, c0=c0: e.dma_start(out=wb[:, :, :], in_=w_in_v[:, :, c0:c0 + 512]),
                      writes=[wb.p()])
            j = cc % 4
            pb, ab, qb = pre[cc % 2], acc[cc % 2], post[cc % 2]
            for tb in range(9):
                n = 256 if tb == 0 else 512
                tok0 = 0 if tb == 0 else 256 + 512 * (tb - 1)
                off = 2 if tb == 0 else 262 + 512 * (tb - 1)
                pa = psA[tb % 4]
                for kc in range(KC):
                    P.op("pe", lambda e, pa=pa, wb=wb, j=j, kc=kc, tok0=tok0, n=n: e.matmul(
                        pa[:, 0:n], lhsT=wb[:, kc, j * 128:(j + 1) * 128], rhs=uT[:, kc, tok0:tok0 + n],
                        start=(kc == 0), stop=(kc == KC - 1)),
                        reads=[wb.p()] + uT.all()[tok0 // 128:(tok0 + n) // 128], writes=[pa.p()])
                P.op("act", lambda e, pa=pa, pb=pb, off=off, n=n: e.copy(out=pb[:, off:off + n], in_=pa[:, 0:n]),
                     reads=[pa.p()], writes=[pb.p()])
            ceng = "dve"
            precast_issue(4)
            P.op(ceng, lambda e, ab=ab, pb=pb, cc=cc: e.tensor_scalar(
                out=ab[:, :], in0=pb[:, 0:CL], scalar1=cpar[:, cc, 0:1], scalar2=None, op0=ALU.mult),
                reads=[pb.p(), cpar.p()], writes=[ab.p()])
            for tap in range(1, 5):
                P.op(ceng, lambda e, ab=ab, pb=pb, cc=cc, tap=tap: e.scalar_tensor_tensor(
                    out=ab[:, :], in0=pb[:, tap:tap + CL], scalar=cpar[:, cc, tap:tap + 1], in1=ab[:, :],
                    op0=ALU.mult, op1=ALU.add),
                    reads=[pb.p(), cpar.p(), ab.p()], writes=[ab.p()])
            P.op("act", lambda e, ab=ab, qb=qb, cc=cc: e.activation(
                out=qb[:, 0:256], in_=ab[:, 0:256], func=AF.Silu, bias=cpar[:, cc, 5:6]),
                reads=[ab.p(), cpar.p()], writes=[qb.p()])
            P.op("act", lambda e, ab=ab, qb=qb, cc=cc: e.activation(
                out=qb[:, 256:TA], in_=ab[:, 260:260 + T], func=AF.Silu, bias=cpar[:, cc, 5:6]),
                reads=[ab.p(), cpar.p()], writes=[qb.p()])
            if cc >= 8:
                P.dma("sp", ds_post[cc % 2], lambda e, qb=qb, cc=cc: e.dma_start(out=bc_s[cc - 8, :, :], in_=qb[:, :]),
                      reads=[qb.p()])
            if cc < 12:
                tk = tokb[cc % 2]
                for i0 in range(0, NTA, 8):
                    ni = min(8, NTA - i0)
                    pt = pst[(i0 // 8) % 2]
                    ptb = pt.t[:, :].bitcast(BF16)
                    for ii in range(ni):
                        i = i0 + ii
                        P.op("pe", lambda e, ptb=ptb, qb=qb, i=i, ii=ii: e.transpose(
                            ptb[:, ii * 128:(ii + 1) * 128], qb[:, i * 128:(i + 1) * 128], ident_b[:, :]),
                            reads=[qb.p(), ident_b.p()], writes=[pt.p()])
                    eng2 = "pool" if cc % 2 == 0 else "dve"
                    eng2 = "dve"
                    P.op(eng2, lambda e, ptb=ptb, tk=tk, i0=i0, ni=ni: e.tensor_copy(
                        out=tk[:, i0:i0 + ni, :], in_=ptb[:, 0:ni * 128].rearrange("p (i c) -> p i c", c=128)),
                        reads=[pt.p()], writes=[tk.p()])
                if cc < 8:
                    dst = xs_v[:, :, cc * 128:(cc + 1) * 128]
                else:
                    dst = bt_v[:, :, (cc - 8) * 128:(cc - 7) * 128]
                P.dma("sp", ds_tokb[cc % 2], lambda e, tk=tk, dst=dst: e.dma_start(out=dst, in_=tk[:, :, :]),
                      reads=[tk.p()])

        wdt = sb("wdt", [128, KC, 32], BF16)
        ds_wdt = P.dsem("ds_wdt")
        P.dma("pool", ds_wdt, lambda e: e.dma_start(out=wdt[:, :, :], in_=w_in_v[:, :, 3072:3104]), writes=[wdt.p()])
        abc = sb("abc", [128, 32])
        P.op("act", lambda e: e.activation(out=abc[:, :], in_=rp[:, RP_ALOG:RP_ALOG + 32], func=AF.Exp),
             reads=[rp.p()], writes=[abc.p()])
        P.op("dve", lambda e: e.tensor_scalar(out=abc[:, :], in0=abc[:, :], scalar1=-1.0, scalar2=None, op0=ALU.mult),
             reads=[abc.p()], writes=[abc.p()])
        for c3 in range(3):
            i0 = c3 * 16
            ni = min(16, NTA - i0)
            pa = psA[c3]
            for ii in range(ni):
                i = i0 + ii
                for kc in range(KC):
                    P.op("pe", lambda e, pa=pa, ii=ii, i=i, kc=kc: e.matmul(
                        pa[:, ii * 32:(ii + 1) * 32], lhsT=uT[:, kc, i * 128:(i + 1) * 128], rhs=wdt[:, kc, :],
                        start=(kc == 0), stop=(kc == KC - 1)),
                        reads=[uT.p(i), wdt.p()], writes=[pa.p()])
            P.op("dve", lambda e, pa=pa, i0=i0, ni=ni: e.tensor_tensor(
                out=dts[:, 0, i0 * 32:(i0 + ni) * 32].rearrange("p (i c) -> p i c", c=32),
                in0=pa[:, 0:ni * 32].rearrange("p (i c) -> p i c", c=32),
                in1=rp[:, RP_DTB:RP_DTB + 32].unsqueeze(1).to_broadcast([128, ni, 32]), op=ALU.add),
                reads=[pa.p(), rp.p()], writes=[dts.p()])
        P.op("act", lambda e: e.activation(out=dts[:, 0, :], in_=dts[:, 0, :], func=AF.Exp),
             reads=[dts.p()], writes=[dts.p()])
        P.op("act", lambda e: e.activation(out=dts[:, 0, :], in_=dts[:, 0, :], func=AF.Ln, bias=oneb[:, 0:1]),
             reads=[dts.p(), oneb.p()], writes=[dts.p()])
        P.op("dve", lambda e: e.tensor_tensor(
            out=dts[:, 1, :].rearrange("p (i c) -> p i c", c=32),
            in0=dts[:, 0, :].rearrange("p (i c) -> p i c", c=32),
            in1=abc[:, :].unsqueeze(1).to_broadcast([128, NTA, 32]), op=ALU.mult),
            reads=[dts.p(), abc.p()], writes=[dts.p()])
        for q, mi in enumerate([0, 1, 2, 3, 4]):
            for c3 in range(3):
                c0 = c3 * 512
                n = min(512, NDT - c0)
                pa = psA[(q * 3 + c3) % 4]
                P.op("pe", lambda e, pa=pa, mi=mi, c0=c0, n=n: e.matmul(
                    pa[:, 0:n], lhsT=masks[:, mi, :], rhs=dts[:, 1, c0:c0 + n], start=True, stop=True),
                    reads=[masks.p(), dts.p()], writes=[pa.p()])
                P.op("act", lambda e, pa=pa, q=q, c0=c0, n=n: e.activation(
                    out=dts[:, 2 + q, c0:c0 + n], in_=pa[:, 0:n], func=AF.Exp),
                    reads=[pa.p()], writes=[dts.p()])
        if "conv" in dbg:
            P.barrier()
            ds_dbg2 = P.dsem("ds_dbg2")
            P.dma("sp", ds_dbg2, lambda e: e.dma_start(out=dbg_d["bc"][:, :, :], in_=bc_s[:, :, :]))
            P.dma("sp", ds_dbg2, lambda e: e.dma_start(out=dbg_d["xs"][:, :], in_=xs_s[:, :]))
            P.dma("sp", ds_dbg2, lambda e: e.dma_start(out=dbg_d["bt"][:, :], in_=bt_s[:, :]))
            P.dma("sp", ds_dbg2, lambda e: e.dma_start(out=dbg_d["dts"][:, :, :], in_=dts[:, :, :]), reads=[dts.p()])
        phB.__exit__()

        phC1 = Phase().__enter__()
        wf = sb("wf", [128, KC, 512], BF16)
        ds_wf = P.dsem("ds_wf")
        P.dma("pool", ds_wf, lambda e: e.dma_start(out=wf[:, :, :], in_=w_in_v[:, :, 3104:3616]), writes=[wf.p()])
        fblk = [sb("fblk%d" % i, [128, T], BF16) for i in range(2)]
        ds_fb = [P.dsem("ds_fb%d" % i) for i in range(2)]
        for g in range(4):
            fb = fblk[g % 2]
            for tb in range(8):
                pa = psA[tb % 4]
                tok0 = 256 + 512 * tb
                for kc in range(KC):
                    P.op("pe", lambda e, pa=pa, g=g, kc=kc, tok0=tok0: e.matmul(
                        pa[:, :], lhsT=wf[:, kc, g * 128:(g + 1) * 128], rhs=uT[:, kc, tok0:tok0 + 512],
                        start=(kc == 0), stop=(kc == KC - 1)),
                        reads=[wf.p()] + uT.all()[tok0 // 128:tok0 // 128 + 4], writes=[pa.p()])
                ev = "act" if tb % 2 == 0 else "dve"
                if ev == "act":
                    P.op("act", lambda e, pa=pa, fb=fb, tb=tb: e.copy(out=fb[:, tb * 512:(tb + 1) * 512], in_=pa[:, :]),
                         reads=[pa.p()], writes=[fb.p()])
                else:
                    P.op("dve", lambda e, pa=pa, fb=fb, tb=tb: e.tensor_copy(out=fb[:, tb * 512:(tb + 1) * 512], in_=pa[:, :]),
                         reads=[pa.p()], writes=[fb.p()])
            P.dma("sp", ds_fb[g % 2], lambda e, fb=fb, g=g: e.dma_start(out=f_s[g, :, :], in_=fb[:, :]), reads=[fb.p()])
        phC1.__exit__()

        phU.__exit__()

        phC = Phase().__enter__()
        fT = sb("fT", [128, 4, T], BF16)
        Yb = sb("Yb", [128, NT, 4, 256], BF16, nparts=NT)
        cs = sb("cs", [128, 256], BF16)
        wfour = sb("wfour", [128, 4, 128], BF16)
        tabs = [sb("tabs%d" % i, [128, 8, 2, 512], BF16) for i in range(2)]
        ds_tab = [P.dsem("ds_tab%d" % i) for i in range(2)]
        specT = [sb("specT%d" % i, [128, 4, 512], BF16) for i in range(2)]
        fourb = [sb("fourb%d" % i, [128, 4, 512], BF16) for i in range(2)]
        ds_four = [P.dsem("ds_four%d" % i) for i in range(2)]
        ds_cc = P.dsem("ds_cc")
        P.dma("sp", ds_cc, lambda e: e.dma_start(out=fT[:, :, :], in_=f_s.rearrange("g p t -> p g t")), writes=[fT.p()])
        P.dma("sp", ds_cc, lambda e: e.dma_start(out=cs[:, :], in_=cs_d[:, :]), writes=[cs.p()])
        ds_cc2 = P.dsem("ds_cc2")
        P.dma("pool", ds_cc2, lambda e: e.dma_start(out=wfour[:, :, :], in_=w_four_d.rearrange("g c d -> c g d")),
              writes=[wfour.p()])
        for i in range(NT):
            for h2 in range(2):
                pa = psA[(2 * i + h2) % 4]
                for gg in range(2):
                    g = 2 * h2 + gg
                    P.op("pe", lambda e, pa=pa, gg=gg, g=g, i=i: e.matmul(
                        pa[:, gg * 256:(gg + 1) * 256], lhsT=fT[:, g, i * 128:(i + 1) * 128], rhs=cs[:, :],
                        start=True, stop=True), reads=[fT.p(), cs.p()], writes=[pa.p()])
                if h2 == 0:
                    P.op("act", lambda e, pa=pa, i=i, h2=h2: e.copy(
                        out=Yb[:, i, 2 * h2:2 * h2 + 2, :], in_=pa[:, :].rearrange("p (g c) -> p g c", c=256)),
                        reads=[pa.p()], writes=[Yb.p(i)])
                else:
                    P.op("dve", lambda e, pa=pa, i=i, h2=h2: e.tensor_copy(
                        out=Yb[:, i, 2 * h2:2 * h2 + 2, :], in_=pa[:, :].rearrange("p (g c) -> p g c", c=256)),
                        reads=[pa.p()], writes=[Yb.p(i)])
        ORTHO = 1.0 / float(np.sqrt(4096.0 * 128.0))
        piece = 0
        for kb in range(8):
            for q in range(4):
                tb_ = tabs[piece % 2]
                P.dma("sp", ds_tab[piece % 2], lambda e, tb_=tb_, kb=kb, q=q: e.dma_start(
                    out=tb_[:, :, :, :], in_=dft_d[kb, :, q * 8:(q + 1) * 8, :, :]), writes=[tb_.p()])
                piece += 1
                for ii in range(8):
                    i = q * 8 + ii
                    for g in range(4):
                        P.op("pe", lambda e, g=g, i=i, ii=ii, tb_=tb_: e.matmul(
                            banks[g][:, :], lhsT=Yb[:, i, g, 0:128], rhs=tb_[:, ii, 0, :], start=(i == 0), stop=False),
                            reads=[Yb.p(i), tb_.p()], writes=[banks[g].p()])
                        P.op("pe", lambda e, g=g, i=i, ii=ii, tb_=tb_: e.matmul(
                            banks[g][:, :], lhsT=Yb[:, i, g, 128:256], rhs=tb_[:, ii, 1, :], start=False, stop=(i == NT - 1)),
                            reads=[Yb.p(i), tb_.p()], writes=[banks[g].p()])
            precast_issue(4)
            sp_ = specT[kb % 2]
            fo_ = fourb[kb % 2]
            for g in range(4):
                if g % 2 == 0:
                    P.op("act", lambda e, g=g, sp_=sp_: e.activation(out=sp_[:, g, :], in_=banks[g][:, :], func=AF.Copy,
                                                                    scale=ORTHO), reads=[banks[g].p()], writes=[sp_.p()])
                else:
                    P.op("dve", lambda e, g=g, sp_=sp_: e.tensor_scalar(out=sp_[:, g, :], in0=banks[g][:, :], scalar1=ORTHO,
                                                                       scalar2=None, op0=ALU.mult),
                         reads=[banks[g].p()], writes=[sp_.p()])
            for g in range(4):
                pb_ = banks[4 + g]
                P.op("pe", lambda e, g=g, sp_=sp_, pb_=pb_: e.matmul(
                    pb_[:, :], lhsT=wfour[:, g, :], rhs=sp_[:, g, :], start=True, stop=True),
                    reads=[wfour.p(), sp_.p()], writes=[pb_.p()])
                if g % 2 == 0:
                    P.op("act", lambda e, g=g, fo_=fo_, pb_=pb_: e.copy(out=fo_[:, g, :], in_=pb_[:, :]),
                         reads=[pb_.p()], writes=[fo_.p()])
                else:
                    P.op("dve", lambda e, g=g, fo_=fo_, pb_=pb_: e.tensor_copy(out=fo_[:, g, :], in_=pb_[:, :]),
                         reads=[pb_.p()], writes=[fo_.p()])
            P.dma("sp", ds_four[kb % 2], lambda e, fo_=fo_, kb=kb: e.dma_start(
                out=four_s.rearrange("g p t -> p g t")[:, :, kb * 512:(kb + 1) * 512], in_=fo_[:, :, :]), reads=[fo_.p()])
        precast_issue(1000)
        phC.__exit__()
        if "four" in dbg:
            ds_dbg4 = P.dsem("ds_dbg4")
            P.dma("sp", ds_dbg4, lambda e: e.dma_start(out=dbg_d["four"][:, :, :], in_=four_s[:, :, :]))
            P.barrier()
        phD = Phase().__enter__()
        y_v = y_s.rearrange("(i p) c -> p i c", p=128)
        dtsv = lambda q: dts[:, q, :].rearrange("p (i d h) -> p i d h", d=2, h=16)
        ys_tiles = [[Tile("ys%d_%d" % (g, i)) for i in range(NT)] for g in range(4)]

        def interleave(gens, level=0):
            gens = list(gens)
            while gens:
                for gn in list(gens):
                    try:
                        while next(gn) < level:
                            pass
                    except StopIteration:
                        gens.remove(gn)

        yb_v = yb_s.rearrange("(i p) c -> p i c", p=128)

        class GBuf:
            def __init__(self, k):
                self.BT = sb("BT%d" % k, [128, TA], BF16)
                self.CT = sb("CT%d" % k, [128, TA], BF16)
                self.xs_tok = sb("xs_tok%d" % k, [128, NTA, 256], BF16)
                self.B_tok = sb("B_tok%d" % k, [128, NTA, 128], BF16)
                self.ds = P.dsem("ds_g%d" % k)

        class CBuf:
            def __init__(self, c):
                self.kb = [banks[2 * c], banks[2 * c + 1]]
                self.cbm = [sb("cbm%d_%d" % (c, i), [128, 128], BF16) for i in range(2)]
                self.Rb = [sb("Rb%d_%d" % (c, i), [128, 512]) for i in range(2)]
                self.Eb = [sb("Eb%d_%d" % (c, i), [128, 512]) for i in range(1)] * 2
                self.MTb = [sb("MTb%d_%d" % (c, i), [128, 512], BF16) for i in range(2)]
                self.tmpb = [sb("tmpb%d_%d" % (c, i), [128, 256]) for i in range(2)]
                self.youtb = [sb("yout%d_%d" % (c, i), [128, 256]) for i in range(2)]
                self.xdb = [sb("xdb%d_%d" % (c, i), [128, 256], BF16) for i in range(2)]
                self.xddb = [sb("xddb%d_%d" % (c, i), [128, 256], BF16) for i in range(2)]
                self.ds_yout = [P.dsem("ds_yout%d_%d" % (c, i)) for i in range(2)]
                self.h32 = sb("h32_%d" % c, [128, 256])
                self.h16 = sb("h16_%d" % c, [128, 256], BF16)

        gbufs = [GBuf(0), GBuf(1)]
        cbufs = [CBuf(c) for c in range(4)]

        def ssd_dir_chain(gb, cbf, g, d):
            BT, CT, xs_tok, B_tok = gb.BT, gb.CT, gb.xs_tok, gb.B_tok
            kA, kB = cbf.kb
            h32, h16 = cbf.h32, cbf.h16
            hs = slice(4 * g, 4 * g + 4)
            qd = 3 if d == 0 else 5
            P.op("pool", lambda e: e.memset(h32[:, :], 0.0), writes=[h32.p()])
            P.op("pool", lambda e: e.memset(h16[:, :], 0.0), writes=[h16.p()])
            order = list(range(NTA)) if d == 0 else [1, 0] + list(range(NTA - 1, 1, -1))
            m_cb = 0 if d == 0 else 2
            m_R = 0 if d == 0 else 2
            m_L = 1 if d == 0 else 3
            q_incl = 2 if d == 0 else 4
            ydst = y_v if d == 0 else yb_v
            yield 1
            for idx, i in enumerate(order):
                last = idx == len(order) - 1
                tsl = slice(i * 128, (i + 1) * 128)
                par = idx % 2
                xd, xdd = cbf.xdb[par], cbf.xddb[par]
                P.op("pool", lambda e, xd=xd, i=i: e.tensor_tensor(
                    out=xd[:, :].rearrange("p (r c) -> p r c", c=64),
                    in0=xs_tok[:, i, :].rearrange("p (r c) -> p r c", c=64),
                    in1=dtsv(0)[:, i, d, hs].unsqueeze(2).to_broadcast([128, 4, 64]), op=ALU.mult),
                    reads=[xs_tok.p(), dts.p()], writes=[xd.p()])
                if not last:
                    P.op("pool", lambda e, xd=xd, xdd=xdd, i=i: e.tensor_tensor(
                        out=xdd[:, :].rearrange("p (r c) -> p r c", c=64),
                        in0=xd[:, :].rearrange("p (r c) -> p r c", c=64),
                        in1=dtsv(qd)[:, i, d, hs].unsqueeze(2).to_broadcast([128, 4, 64]), op=ALU.mult),
                        reads=[xd.p(), dts.p()], writes=[xdd.p()])
                yield 0
                if i >= 2:
                    cb, R, E, MT, tmp = cbf.cbm[par], cbf.Rb[par], cbf.Eb[par], cbf.MTb[par], cbf.tmpb[par]
                    P.op("pe", lambda e, tsl=tsl: e.matmul(kA[:, 0:128], lhsT=BT[:, tsl], rhs=CT[:, tsl], start=True, stop=True),
                         reads=[BT.p(), CT.p()], writes=[kA.p()])
                    if idx % 2 == 0:
                        P.op("pool", lambda e, R=R, i=i: e.tensor_tensor(
                            out=R[:, :].rearrange("p (r l) -> p r l", l=128),
                            in0=masks[:, m_R, :].unsqueeze(1).to_broadcast([128, 4, 128]),
                            in1=dtsv(1)[:, i, d, hs].unsqueeze(2).to_broadcast([128, 4, 128]), op=ALU.mult),
                            reads=[masks.p(), dts.p()], writes=[R.p()])
                    else:
                        for r in range(4):
                            P.op("act", lambda e, R=R, r=r, i=i: e.activation(
                                out=R[:, r * 128:(r + 1) * 128], in_=masks[:, m_R, :], func=AF.Copy,
                                scale=dtsv(1)[:, i, d, 4 * g + r:4 * g + r + 1]),
                                reads=[masks.p(), dts.p()], writes=[R.p()])
                    yield 0
                    P.op("dve", lambda e, cb=cb: e.tensor_tensor(
                        out=cb[:, :], in0=kA[:, 0:128], in1=masks[:, m_cb, :], op=ALU.mult),
                        reads=[kA.p(), masks.p()], writes=[cb.p()])
                    P.op("pe", lambda e, R=R: e.matmul(kB[:, :], lhsT=masks[:, m_L, :], rhs=R[:, :], start=True, stop=True),
                         reads=[masks.p(), R.p()], writes=[kB.p()])
                    yield 0
                    P.op("act", lambda e, E=E: e.activation(out=E[:, :], in_=kB[:, :], func=AF.Exp),
                         reads=[kB.p()], writes=[E.p()])
                    P.op("pe", lambda e, tsl=tsl: e.matmul(kA[:, 128:384], lhsT=CT[:, tsl], rhs=h16[:, :], start=True, stop=True),
                         reads=[CT.p(), h16.p()], writes=[kA.p()])
                    yield 0
                    P.op("dve", lambda e, E=E, MT=MT, cb=cb: e.tensor_tensor(
                        out=MT[:, :].rearrange("p (r l) -> p r l", l=128),
                        in0=E[:, :].rearrange("p (r l) -> p r l", l=128),
                        in1=cb[:, :].unsqueeze(1).to_broadcast([128, 4, 128]), op=ALU.mult),
                        reads=[E.p(), cb.p()], writes=[MT.p()])
                    P.op("dve", lambda e, tmp=tmp, i=i: e.tensor_tensor(
                        out=tmp[:, :].rearrange("p (r c) -> p r c", c=64),
                        in0=kA[:, 128:384].rearrange("p (r c) -> p r c", c=64),
                        in1=dtsv(q_incl)[:, i, d, hs].unsqueeze(2).to_broadcast([128, 4, 64]), op=ALU.mult),
                        reads=[kA.p(), dts.p()], writes=[tmp.p()])
                    yield 0
                    for r in range(4):
                        P.op("pe", lambda e, MT=MT, r=r, xd=xd: e.matmul(
                            kA[:, r * 64:(r + 1) * 64], lhsT=MT[:, r * 128:(r + 1) * 128],
                            rhs=xd[:, r * 64:(r + 1) * 64], start=True, stop=True),
                            reads=[MT.p(), xd.p()], writes=[kA.p()])
                    yield 0
                    yo = cbf.youtb[par]
                    P.op("dve", lambda e, tmp=tmp, yo=yo: e.tensor_tensor(
                        out=yo[:, :], in0=kA[:, 0:256], in1=tmp[:, :], op=ALU.add),
                        reads=[kA.p(), tmp.p()], writes=[yo.p()])
                    if d == 1:
                        yield 0
                        P.op("pool", lambda e, tmp=tmp, i=i: e.tensor_tensor(
                            out=tmp[:, :].rearrange("p (r c) -> p r c", c=64),
                            in0=xs_tok[:, i, :].rearrange("p (r c) -> p r c", c=64),
                            in1=rp[:, RP_DSKIP + 4 * g:RP_DSKIP + 4 * g + 4].unsqueeze(2).to_broadcast([128, 4, 64]),
                            op=ALU.mult),
                            reads=[xs_tok.p(), rp.p()], writes=[tmp.p()])
                        P.op("pool", lambda e, yo=yo, tmp=tmp: e.tensor_tensor(
                            out=yo[:, :], in0=yo[:, :], in1=tmp[:, :], op=ALU.add),
                            reads=[tmp.p(), yo.p()], writes=[yo.p()])
                    P.dma("sp", cbf.ds_yout[par], lambda e, yo=yo, i=i: e.dma_start(
                        out=ydst[:, i - 2, 256 * g:256 * g + 256], in_=yo[:, :]), reads=[yo.p()])
                    yield 0
                if not last:
                    P.op("pe", lambda e, i=i, xdd=xdd: e.matmul(
                        kB[:, 0:256], lhsT=B_tok[:, i, :], rhs=xdd[:, :], start=True, stop=True),
                        reads=[B_tok.p(), xdd.p()], writes=[kB.p()])
                    P.op("dve", lambda e, i=i: e.tensor_tensor(
                        out=h32[:, :].rearrange("p (r c) -> p r c", c=64),
                        in0=h32[:, :].rearrange("p (r c) -> p r c", c=64),
                        in1=dtsv(6)[:, i, d, hs].unsqueeze(2).to_broadcast([128, 4, 64]), op=ALU.mult),
                        reads=[h32.p(), dts.p()], writes=[h32.p()])
                    yield 0
                    P.op("dve", lambda e: e.tensor_tensor(out=h32[:, :], in0=h32[:, :], in1=kB[:, 0:256], op=ALU.add),
                         reads=[h32.p(), kB.p()], writes=[h32.p()])
                    P.op("act", lambda e: e.copy(out=h16[:, :], in_=h32[:, :]), reads=[h32.p()], writes=[h16.p()])
                yield 1

        for rnd in range(2):
            chains = []
            for k in range(2):
                g = 2 * rnd + k
                gb = gbufs[k]
                P.dma("sp", gb.ds, lambda e, gb=gb, g=g: e.dma_start(out=gb.BT[:, :], in_=bc_s[g, :, :]), writes=[gb.BT.p()])
                P.dma("sp", gb.ds, lambda e, gb=gb, g=g: e.dma_start(out=gb.CT[:, :], in_=bc_s[4 + g, :, :]), writes=[gb.CT.p()])
                P.dma("sp", gb.ds, lambda e, gb=gb, g=g: e.dma_start(out=gb.xs_tok[:, :, :], in_=xs_v[:, :, 256 * g:256 * g + 256]),
                      writes=[gb.xs_tok.p()])
                P.dma("sp", gb.ds, lambda e, gb=gb, g=g: e.dma_start(out=gb.B_tok[:, :, :], in_=bt_v[:, :, 128 * g:128 * g + 128]),
                      writes=[gb.B_tok.p()])
                for d in range(2):
                    chains.append(ssd_dir_chain(gb, cbufs[2 * k + d], g, d))
            interleave(chains)
        phD.__exit__()
        phX.__exit__()
        if "ssd" in dbg:
            ds_dbg3 = P.dsem("ds_dbg3")
            P.dma("sp", ds_dbg3, lambda e: e.dma_start(out=dbg_d["y"][:, :], in_=y_s[:, :]))
            P.dma("sp", ds_dbg3, lambda e: e.dma_start(out=dbg_d["yb"][:, :], in_=yb_s[:, :]))
            P.barrier()

        BS = 512
        NB = 48
        I32 = mybir.dt.int32
        gates = sb("gates", [128, 2, NT])
        sloti = sb("sloti", [128, 2, NT], I32)
        widx = sb("widx", [128, 2, NB], I32)
        hc = sb("hc", [128, 128])
        masksE = sb("masksE", [128, 2, 128])
        ds_c2 = P.dsem("ds_c2")
        P.dma("sp", ds_c2, lambda e: e.dma_start(out=hc[:, :], in_=hc_d[:, :]), writes=[hc.p()])
        P.dma("sp", ds_c2, lambda e: e.dma_start(out=masksE[:, :, :], in_=masks_d[:, 3:5, :]), writes=[masksE.p()])
        phE = Phase().__enter__()
        lgall = sb("lgall", [128, NT, 36])
        phE1 = Phase().__enter__()
        wz = sb("wz", [128, KC, 1024], BF16)
        wout = sb("wout", [128, 12, 1024], BF16)
        four_sb = sb("four_sb", [128, 4, T], BF16)
        wr = sb("wr", [128, KC, 36])
        junk2 = sb("junk2", [128, D])
        ds_e = P.dsem("ds_e")
        ds_e2 = P.dsem("ds_e2")
        P.dma("pool", ds_e2, lambda e: e.dma_start(out=wz[:, :, :], in_=w_in_v[:, :, 0:1024]), writes=[wz.p()])
        P.dma("pool", ds_e2, lambda e: e.dma_start(out=wout[:, :, :], in_=w_out_d.rearrange("(k p) d -> p k d", p=128)),
              writes=[wout.p()])
        P.dma("sp", ds_e, lambda e: e.dma_start(out=four_sb[:, :, :], in_=four_s.rearrange("g p t -> p g t")),
              writes=[four_sb.p()])
        P.dma("sp", ds_e, lambda e: e.dma_start(out=wr[:, :, :], in_=wr_d[:, :, :]), writes=[wr.p()])

        ds_z = P.dsem("ds_z", barrier=False)
        t_hs0 = Tile("hs_zero")
        zf_pos = [0]

        def zero_fill_issue():
            if zf_pos[0] < 12:
                b_ = zf_pos[0]
                zf_pos[0] += 1
                P.dma("pool", ds_z, lambda e, b_=b_: e.dma_start(out=hs_sorted[b_ * 2048:(b_ + 1) * 2048, :], in_=zeros_d[:, :]),
                      writes=[t_hs0])

        NCH = 3

        def e_chain(ch):
            kA, kB_ = banks[2 * ch], banks[2 * ch + 1]
            kC = banks[6 + ch] if ch < 2 else kB_
            kb = [kA, kB_, kC, kA]
            xb = sb("xt2_%d" % ch, [128, D])
            yb = sb("yt2_%d" % ch, [128, D])
            ds_y3 = P.dsem("ds_y3_%d" % ch)
            ds_x2 = P.dsem("ds_x2_%d" % ch)
            ds_y2 = P.dsem("ds_y2_%d" % ch)
            xn2 = sb("xn2_%d" % ch, [128, D], BF16)
            utmp2 = sb("utmp2_%d" % ch, [128, KC, 128])
            uTt = sb("uTt%d" % ch, [128, KC, 128], BF16)
            sz = sb("sz%d" % ch, [128, D])
            yb3 = sz
            yz = sb("yz%d" % ch, [128, D])
            yzb = xn2
            catT = uTt
            x1b = sb("x1t%d" % ch, [128, D])
            ds_x1 = P.dsem("ds_x1_%d" % ch)
            hn = yz
            hT32 = utmp2
            hb2 = sb("hTb%d" % ch, [128, D], BF16)
            ds_hT = P.dsem("ds_hT%d" % ch)
            sse = sb("sse%d" % ch, [128, 4])
            yield 1
            for i in range(ch, NT, NCH):
                rows = slice(i * 128, (i + 1) * 128)
                if ch == 0:
                    zero_fill_issue()
                    zero_fill_issue()
                P.dma("sp", ds_x2, lambda e, rows=rows: e.dma_start(out=xb[:, :], in_=x_d[rows, :]), writes=[xb.p()])
                P.dma("sp", ds_y2, lambda e, rows=rows: e.dma_start(out=yb[:, :], in_=y_s[rows, :]), writes=[yb.p()])
                P.dma("sp", ds_y3, lambda e, rows=rows: e.dma_start(out=yb3[:, :], in_=yb_s[rows, :]), writes=[yb3.p()])
                P.op("pool", lambda e: e.tensor_tensor(out=yb[:, :], in0=yb[:, :], in1=yb3[:, :], op=ALU.add),
                     reads=[yb.p(), yb3.p()], writes=[yb.p()])
                P.op("act", lambda e, i=i: e.activation(out=xn2[:, :], in_=xb[:, :], func=AF.Copy, scale=rs[:, i + 2:i + 3]),
                     reads=[xb.p(), rs.p()], writes=[xn2.p()])
                yield 0
                pt = kb[0]
                ptb = pt.t[:, :].bitcast(BF16)
                for kc in range(KC):
                    P.op("pe", lambda e, ptb=ptb, kc=kc: e.transpose(
                        ptb[:, kc * 128:(kc + 1) * 128], xn2[:, kc * 128:(kc + 1) * 128], ident_b[:, :]),
                        reads=[xn2.p(), ident_b.p()], writes=[pt.p()])
                yield 0
                P.op("dve", lambda e, ptb=ptb: e.tensor_tensor(
                    out=utmp2[:, :, :], in0=ptb.rearrange("p (k t) -> p k t", k=KC),
                    in1=scA[:, :, 0:1].to_broadcast([128, KC, 128]), op=ALU.mult),
                    reads=[pt.p(), scA.p()], writes=[utmp2.p()])
                yield 0
                P.op("pool", lambda e: e.tensor_tensor(
                    out=uTt[:, :, :], in0=utmp2[:, :, :], in1=modT[:, 0:8, 0:1].to_broadcast([128, KC, 128]), op=ALU.add),
                    reads=[utmp2.p(), modT.p()], writes=[uTt.p()])
                yield 0
                for nb in range(2):
                    zb = kb[1 + nb]
                    for kc in range(KC):
                        P.op("pe", lambda e, zb=zb, kc=kc, nb=nb: e.matmul(
                            zb[:, :], lhsT=uTt[:, kc, :], rhs=wz[:, kc, nb * 512:(nb + 1) * 512],
                            start=(kc == 0), stop=(kc == KC - 1)), reads=[uTt.p(), wz.p()], writes=[zb.p()])
                    yield 0
                    P.op("act", lambda e, zb=zb, nb=nb: e.activation(out=sz[:, nb * 512:(nb + 1) * 512], in_=zb[:, :], func=AF.Silu),
                         reads=[zb.p()], writes=[sz.p()])
                yield 0
                P.op("dve", lambda e: e.tensor_tensor(out=yz[:, :], in0=yb[:, :], in1=sz[:, :], op=ALU.mult),
                     reads=[yb.p(), sz.p()], writes=[yz.p()])
                yield 0
                P.op("act", lambda e: e.activation(out=junk2[:, :], in_=yz[:, :], func=AF.Square, accum_out=sse[:, 0:1]),
                     reads=[yz.p()], writes=[junk2.p(), sse.p()])
                P.op("act", lambda e: e.activation(out=sse[:, 1:2], in_=sse[:, 0:1], func=AF.Sqrt, scale=1.0 / D, bias=epsb[:, 0:1]),
                     reads=[sse.p(), epsb.p()], writes=[sse.p()])
                yield 0
                P.op("dve", lambda e: e.reciprocal(out=sse[:, 1:2], in_=sse[:, 1:2]), reads=[sse.p()], writes=[sse.p()])
                yield 0
                P.op("act", lambda e: e.activation(out=yzb[:, :], in_=yz[:, :], func=AF.Copy, scale=sse[:, 1:2]),
                     reads=[yz.p(), sse.p()], writes=[yzb.p()])
                yield 0
                for kc in range(KC):
                    P.op("pe", lambda e, ptb=ptb, kc=kc: e.transpose(
                        ptb[:, kc * 128:(kc + 1) * 128], yzb[:, kc * 128:(kc + 1) * 128], ident_b[:, :]),
                        reads=[yzb.p(), ident_b.p()], writes=[pt.p()])
                yield 0
                P.op("dve", lambda e, ptb=ptb: e.tensor_tensor(
                    out=catT[:, :, :], in0=ptb.rearrange("p (k t) -> p k t", k=KC),
                    in1=pp[:, 64:72].unsqueeze(2).to_broadcast([128, KC, 128]), op=ALU.mult),
                    reads=[pt.p(), pp.p()], writes=[catT.p()])
                yield 0
                for nb in range(2):
                    mb = kb[1 + nb]
                    for k in range(12):
                        lh = (lambda k=k: catT[:, k, :]) if k < 8 else (lambda k=k, i=i: four_sb[:, k - 8, i * 128:(i + 1) * 128])
                        P.op("pe", lambda e, mb=mb, k=k, nb=nb, lh=lh: e.matmul(
                            mb[:, :], lhsT=lh(), rhs=wout[:, k, nb * 512:(nb + 1) * 512], start=(k == 0), stop=(k == 11)),
                            reads=[catT.p(), four_sb.p(), wout.p()], writes=[mb.p()])
                    yield 0
                    P.op("dve", lambda e, mb=mb, nb=nb: e.tensor_tensor(
                        out=x1b[:, nb * 512:(nb + 1) * 512], in0=mb[:, :], in1=g12[:, 0, nb * 512:(nb + 1) * 512], op=ALU.mult),
                        reads=[mb.p(), g12.p()], writes=[x1b.p()])
                yield 0
                P.op("pool", lambda e: e.tensor_tensor(out=x1b[:, :], in0=x1b[:, :], in1=xb[:, :], op=ALU.add),
                     reads=[x1b.p(), xb.p()], writes=[x1b.p()])
                yield 0
                P.dma("sp", ds_x1, lambda e, rows=rows: e.dma_start(out=x1_s[rows, :], in_=x1b[:, :]), reads=[x1b.p()])
                P.op("act", lambda e: e.activation(out=junk2[:, :], in_=x1b[:, :], func=AF.Square, accum_out=sse[:, 2:3]),
                     reads=[x1b.p()], writes=[junk2.p(), sse.p()])
                P.op("act", lambda e: e.activation(out=sse[:, 3:4], in_=sse[:, 2:3], func=AF.Sqrt, scale=1.0 / D, bias=epsb[:, 0:1]),
                     reads=[sse.p(), epsb.p()], writes=[sse.p()])
                yield 0
                P.op("dve", lambda e: e.reciprocal(out=sse[:, 3:4], in_=sse[:, 3:4]), reads=[sse.p()], writes=[sse.p()])
                yield 0
                P.op("act", lambda e: e.activation(out=hn[:, :], in_=x1b[:, :], func=AF.Copy, scale=sse[:, 3:4]),
                     reads=[x1b.p(), sse.p()], writes=[hn.p()])
                yield 0
                P.op("dve", lambda e: e.tensor_tensor(out=hn[:, :], in0=hn[:, :], in1=g12[:, 3, :], op=ALU.mult),
                     reads=[hn.p(), g12.p()], writes=[hn.p()])
                yield 0
                P.op("pool", lambda e: e.tensor_tensor(out=hn[:, :], in0=hn[:, :], in1=g12[:, 2, :], op=ALU.add),
                     reads=[hn.p(), g12.p()], writes=[hn.p()])
                yield 0
                P.op("act", lambda e: e.copy(out=hb2[:, :], in_=hn[:, :]), reads=[hn.p()], writes=[hb2.p()])
                P.dma("sp", ds_hT, lambda e, rows=rows: e.dma_start(out=h_s[rows, :], in_=hb2[:, :]), reads=[hb2.p()])
                for h2 in range(2):
                    hb_ = kb[1 + h2]
                    for k4 in range(4):
                        kc = 4 * h2 + k4
                        P.op("pe", lambda e, hb_=hb_, k4=k4, kc=kc: e.transpose(
                            hb_[:, k4 * 128:(k4 + 1) * 128], hn[:, kc * 128:(kc + 1) * 128], ident_f[:, :]),
                            reads=[hn.p(), ident_f.p()], writes=[hb_.p()])
                    yield 0
                    if h2 == 0:
                        P.op("act", lambda e, hb_=hb_, h2=h2: e.copy(
                            out=hT32[:, 4 * h2:4 * h2 + 4, :], in_=hb_[:, :].rearrange("p (k t) -> p k t", k=4)),
                            reads=[hb_.p()], writes=[hT32.p()])
                    else:
                        P.op("dve", lambda e, hb_=hb_, h2=h2: e.tensor_copy(
                            out=hT32[:, 4 * h2:4 * h2 + 4, :], in_=hb_[:, :].rearrange("p (k t) -> p k t", k=4)),
                            reads=[hb_.p()], writes=[hT32.p()])
                yield 0
                lb = kb[3]
                for kc in range(KC):
                    P.op("pe", lambda e, kc=kc, lb=lb: e.matmul(lb[:, 0:36], lhsT=hT32[:, kc, :], rhs=wr[:, kc, :],
                                                              start=(kc == 0), stop=(kc == KC - 1)),
                         reads=[hT32.p(), wr.p()], writes=[lb.p()])
                yield 0
                P.op("dve", lambda e, lb=lb, i=i: e.tensor_copy(out=lgall[:, i, :], in_=lb[:, 0:36]), reads=[lb.p()], writes=[lgall.p()])
                yield 1

        interleave([e_chain(c) for c in range(NCH)])
        phE1.__exit__()
        if "noroute" not in dbg:
            r_lg = sb("r_lg", [128, NT, 4]); r_mx = sb("r_mx", [128, NT]); r_eg = sb("r_eg", [128, NT, 4])
            r_sg = sb("r_sg", [128, NT]); r_oh = sb("r_oh", [128, NT, 4]); r_le = sb("r_le", [128, NT, 4, 8])
            r_sel = sb("r_sel", [128, NT, 8]); r_m1 = sb("r_m1", [128, NT]); r_o1 = sb("r_o1", [128, NT, 8])
            r_s2 = sb("r_s2", [128, NT, 8]); r_m2 = sb("r_m2", [128, NT]); r_o2 = sb("r_o2", [128, NT, 8])
            r_e2 = sb("r_e2", [128, NT])
            OH1 = sb("OH1", [128, NT, 32]); OH2 = sb("OH2", [128, NT, 32]); Asum = sb("Asum", [128, NT * 32])
            rank = sb("rank", [128, NT, 32]); TTb = sb("TTb", [128, NT, 32]); PTb = sb("PTb", [128, NT, 32])
            cnt = sb("cnt", [128, 32]); cmpj = sb("cmpj", [128, 32, 16]); nblk = sb("nblk", [128, 32])
            sblk = sb("sblk", [128, 32]); eblk = sb("eblk", [128, 32]); cmpb = sb("cmpb", [128, NB, 32])
            ebf = sb("ebf", [128, NB]); tmpr = sb("tmpr", [128, NT, 32]); slotf = sb("slotf", [128, 2, NT])
            RT = [lgall, r_lg, r_mx, r_eg, r_sg, r_oh, r_le, r_sel, r_m1, r_o1, r_s2, r_m2, r_o2, r_e2, rp,
                  OH1, OH2, Asum, rank, TTb, PTb, cnt, cmpj, nblk, sblk, eblk, cmpb, ebf, tmpr, slotf, gates, sloti, widx, hc]
            rt = [b.p() for b in RT]

            rmax = [int(x[5:]) for x in dbg if x.startswith("rmax:")]
            rmax = rmax[0] if rmax else 10 ** 9
            rcnt = [0]

            def V(fn):
                rcnt[0] += 1
                if rcnt[0] <= rmax:
                    P.op("dve", fn, reads=rt, writes=rt)

            def A(fn):
                rcnt[0] += 1
                if rcnt[0] <= rmax:
                    P.op("act", fn, reads=rt, writes=rt)
            bc3 = lambda ap, n: ap.unsqueeze(2).to_broadcast([128, NT, n])
            V(lambda e: e.tensor_tensor(out=r_lg[:, :, :], in0=lgall[:, :, 0:4],
                                        in1=rp[:, 80:84].unsqueeze(1).to_broadcast([128, NT, 4]), op=ALU.add))
            V(lambda e: e.tensor_reduce(out=r_mx[:, :], in_=r_lg[:, :, :], axis=AX.X, op=ALU.max))
            V(lambda e: e.tensor_tensor(out=r_eg[:, :, :], in0=r_lg[:, :, :], in1=bc3(r_mx[:, :], 4), op=ALU.subtract))
            V(lambda e: e.tensor_tensor(out=r_oh[:, :, :], in0=r_lg[:, :, :], in1=bc3(r_mx[:, :], 4), op=ALU.is_equal))
            A(lambda e: e.activation(out=r_eg[:, :, :], in_=r_eg[:, :, :], func=AF.Exp))
            V(lambda e: e.tensor_reduce(out=r_sg[:, :], in_=r_eg[:, :, :], axis=AX.X, op=ALU.add))
            V(lambda e: e.reciprocal(out=r_sg[:, :], in_=r_sg[:, :]))
            V(lambda e: e.tensor_tensor(out=r_le[:, :, :, :], in0=lgall[:, :, 4:36].rearrange("p t (g x) -> p t g x", x=8),
                                        in1=rp[:, 84:116].rearrange("p (g x) -> p g x", x=8).unsqueeze(1).to_broadcast([128, NT, 4, 8]),
                                        op=ALU.add))
            V(lambda e: e.tensor_tensor(out=r_le[:, :, :, :], in0=r_le[:, :, :, :],
                                        in1=r_oh[:, :, :].unsqueeze(3).to_broadcast([128, NT, 4, 8]), op=ALU.mult))
            V(lambda e: e.tensor_reduce(out=r_sel[:, :, :], in_=r_le[:, :, :, :].rearrange("p t g x -> p t x g"), axis=AX.X, op=ALU.add))
            V(lambda e: e.tensor_reduce(out=r_m1[:, :], in_=r_sel[:, :, :], axis=AX.X, op=ALU.max))
            V(lambda e: e.tensor_tensor(out=r_o1[:, :, :], in0=r_sel[:, :, :], in1=bc3(r_m1[:, :], 8), op=ALU.is_equal))
            V(lambda e: e.scalar_tensor_tensor(out=r_s2[:, :, :], in0=r_o1[:, :, :], scalar=-1.0e30, in1=r_sel[:, :, :],
                                               op0=ALU.mult, op1=ALU.add))
            V(lambda e: e.tensor_reduce(out=r_m2[:, :], in_=r_s2[:, :, :], axis=AX.X, op=ALU.max))
            V(lambda e: e.tensor_tensor(out=r_o2[:, :, :], in0=r_s2[:, :, :], in1=bc3(r_m2[:, :], 8), op=ALU.is_equal))
            V(lambda e: e.tensor_tensor(out=r_e2[:, :], in0=r_m2[:, :], in1=r_m1[:, :], op=ALU.subtract))
            A(lambda e: e.activation(out=r_e2[:, :], in_=r_e2[:, :], func=AF.Exp))
            V(lambda e: e.tensor_scalar(out=gates[:, 0, :], in0=r_e2[:, :], scalar1=1.0, scalar2=None, op0=ALU.add))
            V(lambda e: e.reciprocal(out=gates[:, 0, :], in_=gates[:, 0, :]))
            V(lambda e: e.tensor_tensor(out=gates[:, 0, :], in0=gates[:, 0, :], in1=r_sg[:, :], op=ALU.mult))
            V(lambda e: e.tensor_tensor(out=gates[:, 1, :], in0=gates[:, 0, :], in1=r_e2[:, :], op=ALU.mult))
            V(lambda e: e.tensor_tensor(out=OH1[:, :, :].rearrange("p t (g x) -> p t g x", x=8),
                                        in0=r_o1[:, :, :].unsqueeze(2).to_broadcast([128, NT, 4, 8]),
                                        in1=r_oh[:, :, :].unsqueeze(3).to_broadcast([128, NT, 4, 8]), op=ALU.mult))
            V(lambda e: e.tensor_tensor(out=OH2[:, :, :].rearrange("p t (g x) -> p t g x", x=8),
                                        in0=r_o2[:, :, :].unsqueeze(2).to_broadcast([128, NT, 4, 8]),
                                        in1=r_oh[:, :, :].unsqueeze(3).to_broadcast([128, NT, 4, 8]), op=ALU.mult))
            V(lambda e: e.tensor_tensor(out=Asum[:, :], in0=OH1[:, :, :].rearrange("p t e -> p (t e)"),
                                        in1=OH2[:, :, :].rearrange("p t e -> p (t e)"), op=ALU.add))
            for hf in range(2):
                P.op("pe", lambda e, hf=hf: e.matmul(banks[hf][:, :], lhsT=masksE[:, 0, :], rhs=Asum[:, hf * 512:(hf + 1) * 512],
                                                     start=True, stop=True), reads=rt + [masksE.p()], writes=[banks[hf].p()])
                P.op("pe", lambda e, hf=hf: e.matmul(banks[2 + hf][:, :], lhsT=masksE[:, 1, :], rhs=Asum[:, hf * 512:(hf + 1) * 512],
                                                     start=True, stop=True), reads=rt + [masksE.p()], writes=[banks[2 + hf].p()])
                P.op("dve", lambda e, hf=hf: e.tensor_copy(out=rank[:, hf * 16:(hf + 1) * 16, :],
                                                           in_=banks[hf][:, :].rearrange("p (t e) -> p t e", e=32)),
                     reads=[banks[hf].p()] + rt, writes=rt)
                P.op("dve", lambda e, hf=hf: e.tensor_copy(out=TTb[:, hf * 16:(hf + 1) * 16, :],
                                                           in_=banks[2 + hf][:, :].rearrange("p (t e) -> p t e", e=32)),
                     reads=[banks[2 + hf].p()] + rt, writes=rt)
            V(lambda e: e.memset(PTb[:, 0, :], 0.0))
            for ti in range(1, NT):
                V(lambda e, ti=ti: e.tensor_tensor(out=PTb[:, ti, :], in0=PTb[:, ti - 1, :], in1=TTb[:, ti - 1, :], op=ALU.add))
            V(lambda e: e.tensor_tensor(out=cnt[:, :], in0=PTb[:, NT - 1, :], in1=TTb[:, NT - 1, :], op=ALU.add))
            V(lambda e: e.tensor_tensor(out=rank[:, :, :], in0=rank[:, :, :], in1=PTb[:, :, :], op=ALU.add))
            V(lambda e: e.tensor_tensor(out=cmpj[:, :, :], in0=cnt[:, :].unsqueeze(2).to_broadcast([128, 32, 16]),
                                        in1=hc[:, 0:16].unsqueeze(1).to_broadcast([128, 32, 16]), op=ALU.is_gt))
            V(lambda e: e.tensor_reduce(out=nblk[:, :], in_=cmpj[:, :, :], axis=AX.X, op=ALU.add))
            V(lambda e: e.memset(sblk[:, 0:1], 0.0))
            for ex in range(1, 32):
                V(lambda e, ex=ex: e.tensor_tensor(out=sblk[:, ex:ex + 1], in0=sblk[:, ex - 1:ex], in1=nblk[:, ex - 1:ex], op=ALU.add))
            V(lambda e: e.tensor_tensor(out=eblk[:, :], in0=sblk[:, :], in1=nblk[:, :], op=ALU.add))
            V(lambda e: e.tensor_tensor(out=cmpb[:, :, :], in0=eblk[:, :].unsqueeze(1).to_broadcast([128, NB, 32]),
                                        in1=hc[:, 16:16 + NB].unsqueeze(2).to_broadcast([128, NB, 32]), op=ALU.is_le))
            V(lambda e: e.tensor_reduce(out=ebf[:, :], in_=cmpb[:, :, :], axis=AX.X, op=ALU.add))
            V(lambda e: e.tensor_scalar(out=ebf[:, :], in0=ebf[:, :], scalar1=31.0, scalar2=128.0, op0=ALU.min, op1=ALU.mult))
            V(lambda e: e.tensor_tensor(out=ebf[:, :], in0=ebf[:, :], in1=hc[:, 80:81].to_broadcast([128, NB]), op=ALU.add))
            V(lambda e: e.tensor_copy(out=widx[:, 0, :], in_=ebf[:, :]))
            V(lambda e: e.tensor_scalar(out=ebf[:, :], in0=ebf[:, :], scalar1=1.0, scalar2=None, op0=ALU.add))
            V(lambda e: e.tensor_copy(out=widx[:, 1, :], in_=ebf[:, :]))
            V(lambda e: e.tensor_scalar(out=sblk[:, :], in0=sblk[:, :], scalar1=float(BS), scalar2=None, op0=ALU.mult))
            V(lambda e: e.tensor_tensor(out=rank[:, :, :], in0=rank[:, :, :],
                                        in1=sblk[:, :].unsqueeze(1).to_broadcast([128, NT, 32]), op=ALU.add))
            for j, OH in enumerate((OH1, OH2)):
                V(lambda e, OH=OH: e.tensor_tensor(out=tmpr[:, :, :], in0=rank[:, :, :], in1=OH[:, :, :], op=ALU.mult))
                V(lambda e, j=j: e.tensor_reduce(out=slotf[:, j, :], in_=tmpr[:, :, :], axis=AX.X, op=ALU.add))
            V(lambda e: e.tensor_copy(out=sloti[:, 0, :], in_=slotf[:, 0, :]))
            V(lambda e: e.tensor_copy(out=sloti[:, 1, :], in_=slotf[:, 1, :]))
        if "x1" in dbg:
            P.barrier()
            ds_dbg5 = P.dsem("ds_dbg5")
            P.dma("sp", ds_dbg5, lambda e: e.dma_start(out=dbg_d["x1"][:, :], in_=x1_s[:, :]))
            P.dma("sp", ds_dbg5, lambda e: e.dma_start(out=dbg_d["lg"][:, :, :], in_=lgall[:, :, :]), reads=[lgall.p()])
            P.dma("sp", ds_dbg5, lambda e: e.dma_start(out=dbg_d["slot"][:, :, :], in_=sloti[:, :, :]), reads=[sloti.p()])
            P.dma("sp", ds_dbg5, lambda e: e.dma_start(out=dbg_d["widx"][:, :, :], in_=widx[:, :, :]), reads=[widx.p()])
            P.dma("sp", ds_dbg5, lambda e: e.dma_start(out=dbg_d["gates"][:, :, :], in_=gates[:, :, :]), reads=[gates.p()])
            if "noroute" not in dbg:
                P.dma("sp", ds_dbg5, lambda e: e.dma_start(out=dbg_d["slotf"][:, :, :], in_=slotf[:, :, :]), reads=[slotf.p()])
        phE.__exit__()

        if "nomoe" not in dbg:
            phF1 = Phase().__enter__()
            NH = 4
            hsb = [sb("hsb%d" % i, [128, D], BF16) for i in range(NH)]
            ds_hsb = [P.dsem("ds_hsb%d" % i) for i in range(NH)]
            ds_sc = [P.dsem("ds_sc%d" % i) for i in range(NH)]

            def f1_load(i):
                hb_ = hsb[i % NH]
                rows = slice(i * 128, (i + 1) * 128)
                P.dma("sp", ds_hsb[i % NH], lambda e, hb_=hb_, rows=rows: e.dma_start(out=hb_[:, :], in_=h_s[rows, :]),
                      writes=[hb_.p()])
            for i in range(min(NH - 1, NT)):
                f1_load(i)
            for i in range(NT):
                if i + NH - 1 < NT:
                    f1_load(i + NH - 1)
                hb_ = hsb[i % NH]
                for j in range(2):
                    P.dma("pool", ds_sc[i % NH], lambda e, hb_=hb_, i=i, j=j: e.indirect_dma_start(
                        out=hs_sorted[:, :], out_offset=bass.IndirectOffsetOnAxis(ap=sloti[:, j, i:i + 1], axis=0),
                        in_=hb_[:, :], in_offset=None), reads=[hb_.p(), sloti.p(), t_hs0])
            phF1.__exit__()
            phF2 = Phase().__enter__()
            weg_v, weu_v, wed_v = wgs, wus, wds
            hs_v = hs_sorted.rearrange("(b s p) d -> b p s d", s=4, p=128)
            ys_v = y_sorted.rearrange("(b s p) d -> b p s d", s=4, p=128)

            def f2_chain(ch):
                kb = banks[4 * ch:4 * ch + 4]
                Wg = [sb("Wg%d_%d" % (ch, i), [128, KC * 512], BF16) for i in range(2)]
                Wu = [sb("Wu%d_%d" % (ch, i), [128, KC * 512], BF16) for i in range(2)]
                Wd = [sb("Wd%d_%d" % (ch, i), [128, 4 * D], BF16) for i in range(2)]
                ds_w = [P.dsem("ds_w%d_%d" % (ch, i)) for i in range(2)]
                hsblk = [sb("hsblk%d_%d" % (ch, i), [128, 4, D], BF16) for i in range(2)]
                ds_hb = [P.dsem("ds_hblk%d_%d" % (ch, i)) for i in range(2)]
                hTblk = sb("hTblk%d" % ch, [128, KC, BS], BF16)
                actT = sb("actT%d" % ch, [128, 4, BS], BF16, nparts=4)
                sgb = [sb("sgb%d_%d" % (ch, i), [128, 512]) for i in range(2)]
                yblk = [sb("yblk%d_%d" % (ch, i), [128, D]) for i in range(2)]
                ds_yb = [P.dsem("ds_yb%d_%d" % (ch, i)) for i in range(2)]
                blocks = list(range(ch, NB, 2))

                def fetch(n):
                    b = blocks[n]
                    par = n % 2
                    for (wt, src) in ((Wg[par], weg_v), (Wu[par], weu_v), (Wd[par], wed_v)):
                        P.dma("pool", ds_w[par], lambda e, wt=wt, src=src, b=b: e.indirect_dma_start(
                            out=wt[:, :], out_offset=None, in_=src[:, :],
                            in_offset=bass.IndirectOffsetOnAxis(ap=widx[:, 0, b:b + 1], axis=0)),
                            reads=[widx.p(), t_wcast], writes=[wt.p()])
                    hb_ = hsblk[par]
                    P.dma("sp", ds_hb[par], lambda e, hb_=hb_, b=b: e.dma_start(out=hb_[:, :, :], in_=hs_v[b]), writes=[hb_.p()])
                fetch(0)
                yield 1
                for n, b in enumerate(blocks):
                    par = n % 2
                    if n + 1 < len(blocks):
                        fetch(n + 1)
                    wg, wu, wd, hb_ = Wg[par], Wu[par], Wd[par], hsblk[par]
                    for j2 in range(4):
                        pt = kb[j2]
                        ptb = pt.t[:, :].bitcast(BF16)
                        for kk in range(2):
                            kc = 2 * j2 + kk
                            for s_ in range(4):
                                P.op("pe", lambda e, ptb=ptb, hb_=hb_, kk=kk, kc=kc, s_=s_: e.transpose(
                                    ptb[:, kk * 512 + s_ * 128:kk * 512 + (s_ + 1) * 128], hb_[:, s_, kc * 128:(kc + 1) * 128],
                                    ident_b[:, :]), reads=[hb_.p(), ident_b.p()], writes=[pt.p()])
                        if j2 % 2 == 0:
                            P.op("act", lambda e, ptb=ptb, j2=j2: e.copy(
                                out=hTblk[:, 2 * j2:2 * j2 + 2, :], in_=ptb.rearrange("p (k t) -> p k t", k=2)),
                                reads=[pt.p()], writes=[hTblk.p()])
                        else:
                            P.op("dve", lambda e, ptb=ptb, j2=j2: e.tensor_copy(
                                out=hTblk[:, 2 * j2:2 * j2 + 2, :], in_=ptb.rearrange("p (k t) -> p k t", k=2)),
                                reads=[pt.p()], writes=[hTblk.p()])
                        yield 0
                    for fc in range(4):
                        pg, pu = kb[fc % 2], kb[2 + fc % 2]
                        sg_ = sgb[fc % 2]
                        for kc in range(KC):
                            P.op("pe", lambda e, pg=pg, wg=wg, kc=kc, fc=fc: e.matmul(
                                pg[:, :], lhsT=wg[:, kc * 512 + fc * 128:kc * 512 + (fc + 1) * 128], rhs=hTblk[:, kc, :],
                                start=(kc == 0), stop=(kc == KC - 1)), reads=[wg.p(), hTblk.p()], writes=[pg.p()])
                        yield 0
                        for kc in range(KC):
                            P.op("pe", lambda e, pu=pu, wu=wu, kc=kc, fc=fc: e.matmul(
                                pu[:, :], lhsT=wu[:, kc * 512 + fc * 128:kc * 512 + (fc + 1) * 128], rhs=hTblk[:, kc, :],
                                start=(kc == 0), stop=(kc == KC - 1)), reads=[wu.p(), hTblk.p()], writes=[pu.p()])
                        P.op("act", lambda e, pg=pg, sg_=sg_: e.activation(out=sg_[:, :], in_=pg[:, :], func=AF.Silu),
                             reads=[pg.p()], writes=[sg_.p()])
                        yield 0
                        P.op("dve", lambda e, pu=pu, sg_=sg_, fc=fc: e.tensor_tensor(
                            out=actT[:, fc, :], in0=pu[:, :], in1=sg_[:, :], op=ALU.mult),
                            reads=[pu.p(), sg_.p()], writes=[actT.p(fc)])
                    for s_ in range(4):
                        for nb in range(2):
                            pd = kb[(2 * s_ + nb) % 4]
                            for fc in range(4):
                                P.op("pe", lambda e, pd=pd, fc=fc, s_=s_, nb=nb, wd=wd: e.matmul(
                                    pd[:, :], lhsT=actT[:, fc, s_ * 128:(s_ + 1) * 128],
                                    rhs=wd[:, fc * D + nb * 512:fc * D + (nb + 1) * 512],
                                    start=(fc == 0), stop=(fc == 3)), reads=[actT.p(fc), wd.p()], writes=[pd.p()])
                            yb_ = yblk[s_ % 2]
                            if nb == 0:
                                P.op("act", lambda e, pd=pd, yb_=yb_, nb=nb: e.copy(
                                    out=yb_[:, nb * 512:(nb + 1) * 512], in_=pd[:, :]), reads=[pd.p()], writes=[yb_.p()])
                            else:
                                P.op("dve", lambda e, pd=pd, yb_=yb_, nb=nb: e.tensor_copy(
                                    out=yb_[:, nb * 512:(nb + 1) * 512], in_=pd[:, :]), reads=[pd.p()], writes=[yb_.p()])
                            yield 0
                        P.dma("sp", ds_yb[s_ % 2], lambda e, b=b, s_=s_, yb_=yb_: e.dma_start(
                            out=ys_v[b][:, s_, :], in_=yb_[:, :]), reads=[yb_.p()])
                    yield 1

            interleave([f2_chain(0), f2_chain(1)])
            phF2.__exit__()
            phF = Phase().__enter__()
            NF = 3
            ya = [sb("ya%d" % i, [128, D]) for i in range(NF)]
            yb2 = [sb("yb2_%d" % i, [128, D]) for i in range(NF)]
            x1f = [sb("x1f%d" % i, [128, D]) for i in range(NF)]
            ds_ya = [P.dsem("ds_ya%d" % i) for i in range(NF)]
            ds_x1f = [P.dsem("ds_x1f%d" % i) for i in range(NF)]
            of = [sb("of%d" % i, [128, D]) for i in range(2)]
            of2 = [sb("of2_%d" % i, [128, D]) for i in range(2)]
            ds_out = [P.dsem("ds_out%d" % i) for i in range(2)]
            fnb = sb("fnb", [128, D])
            ssf = sb("ssf", [128, 4])
            ds_f = P.dsem("ds_f")
            P.dma("sp", ds_f, lambda e: e.dma_start(out=fnb[:, :], in_=rbig_d[:, 2, :]), writes=[fnb.p()])

            def f3_load(i):
                q = i % NF
                rows = slice(i * 128, (i + 1) * 128)
                P.dma("sp", ds_x1f[q], lambda e, rows=rows, q=q: e.dma_start(out=x1f[q][:, :], in_=x1_s[rows, :]),
                      writes=[x1f[q].p()])
                for j, yt_ in enumerate((ya[q], yb2[q])):
                    P.dma("pool", ds_ya[q], lambda e, yt_=yt_, i=i, j=j: e.indirect_dma_start(
                        out=yt_[:, :], out_offset=None, in_=y_sorted[:, :],
                        in_offset=bass.IndirectOffsetOnAxis(ap=sloti[:, j, i:i + 1], axis=0)),
                        reads=[sloti.p()], writes=[yt_.p()])
            for i in range(NF - 1):
                f3_load(i)
            for i in range(NT):
                if i + NF - 1 < NT:
                    f3_load(i + NF - 1)
                q = i % NF
                par = i % 2
                rows = slice(i * 128, (i + 1) * 128)
                o1, o2 = of[par], of2[par]
                P.op("dve", lambda e, q=q, i=i, o1=o1: e.tensor_scalar(
                    out=o1[:, :], in0=ya[q][:, :], scalar1=gates[:, 0, i:i + 1], scalar2=None, op0=ALU.mult),
                    reads=[ya[q].p(), gates.p()], writes=[o1.p()])
                P.op("dve", lambda e, q=q, i=i, o1=o1: e.scalar_tensor_tensor(
                    out=o1[:, :], in0=yb2[q][:, :], scalar=gates[:, 1, i:i + 1], in1=o1[:, :], op0=ALU.mult, op1=ALU.add),
                    reads=[yb2[q].p(), gates.p(), o1.p()], writes=[o1.p()])
                P.op("dve", lambda e, o1=o1: e.tensor_tensor(out=o1[:, :], in0=o1[:, :], in1=g12[:, 1, :], op=ALU.mult),
                     reads=[o1.p(), g12.p()], writes=[o1.p()])
                P.op("dve", lambda e, q=q, o1=o1: e.tensor_tensor(out=o1[:, :], in0=o1[:, :], in1=x1f[q][:, :], op=ALU.add),
                     reads=[o1.p(), x1f[q].p()], writes=[o1.p()])
                P.op("act", lambda e, o1=o1, o2=o2, par=par: e.activation(out=o2[:, :], in_=o1[:, :], func=AF.Square,
                                                                          accum_out=ssf[:, 2 * par:2 * par + 1]),
                     reads=[o1.p()], writes=[o2.p(), ssf.p()])
                P.op("act", lambda e, par=par: e.activation(out=ssf[:, 2 * par + 1:2 * par + 2], in_=ssf[:, 2 * par:2 * par + 1],
                                                            func=AF.Sqrt, scale=1.0 / D, bias=epsb[:, 0:1]),
                     reads=[ssf.p(), epsb.p()], writes=[ssf.p()])
                P.op("dve", lambda e, par=par: e.reciprocal(out=ssf[:, 2 * par + 1:2 * par + 2], in_=ssf[:, 2 * par + 1:2 * par + 2]),
                     reads=[ssf.p()], writes=[ssf.p()])
                P.op("dve", lambda e, o1=o1, o2=o2, par=par: e.scalar_tensor_tensor(
                    out=o2[:, :], in0=o1[:, :], scalar=ssf[:, 2 * par + 1:2 * par + 2], in1=fnb[:, :],
                    op0=ALU.mult, op1=ALU.mult), reads=[o1.p(), ssf.p(), fnb.p()], writes=[o2.p()])
                P.dma("sp", ds_out[par], lambda e, rows=rows, o2=o2: e.dma_start(out=out_d[rows, :], in_=o2[:, :]), reads=[o2.p()])
            phF.__exit__()
        P.barrier(final=True)
        P.emit(block)
    return nc


def _prep_inputs(inputs, b):
    g = lambda n: np.asarray(inputs[n], dtype=np.float32)
    c = _get_consts()
    m = {}
    m["x"] = np.ascontiguousarray(g("x")[b])
    m["ctx"] = np.ascontiguousarray(g("ctx")[b])
    cv = np.stack([g("c")[b].reshape(KC, 128).T, g("c_ctx").reshape(KC, 128).T], axis=-1)
    m["cvec"] = np.ascontiguousarray(cv)
    m["w_mod"] = np.ascontiguousarray(g("w_mod")[0])
    pp = np.zeros((128, 128), np.float32)
    pp[:, 0:8] = g("norm1")[0].reshape(KC, 128).T
    pp[:, 8:56] = g("b_mod")[0].reshape(48, 128).T
    pp[:, 56:64] = g("norm2")[0].reshape(KC, 128).T
    pp[:, 64:72] = g("ssd_norm")[0].reshape(KC, 128).T
    m["pp"] = pp
    m["w_in"] = np.ascontiguousarray(g("w_in")[0])
    cw = g("conv_w")[0]
    cb = g("conv_b")[0]
    cpar = np.zeros((128, 16, 6), np.float32)
    cpar[:, :, 0:5] = cw.reshape(5, 16, 128).transpose(2, 1, 0)
    cpar[:, :, 5] = cb.reshape(16, 128).T
    m["cpar"] = cpar
    rp = np.zeros((128, 128), np.float32)
    rp[:, 0:32] = g("dt_bias")[0].reshape(1, 32)
    rp[:, 32:64] = g("a_log")[0].reshape(1, 32)
    rp[:, 64:80] = g("d_skip")[0].reshape(1, 16)
    rp[:, 80:84] = g("b_rg")[0].reshape(1, 4)
    rp[:, 84:116] = g("b_re")[0].reshape(1, 32)
    rbig = np.zeros((128, 6, D), np.float32)
    rbig[:, 0, :] = g("b_mod")[0][2 * D:3 * D][None, :]
    rbig[:, 1, :] = g("b_mod")[0][5 * D:6 * D][None, :]
    rbig[:, 2, :] = g("final_norm")[None, :]
    rbig[:, 3, :] = g("b_mod")[0][3 * D:4 * D][None, :]
    rbig[:, 4, :] = g("b_mod")[0][4 * D:5 * D][None, :]
    rbig[:, 5, :] = g("norm2")[0][None, :]
    m["rbig"] = rbig
    m["w_four"] = np.ascontiguousarray(g("w_four")[0])
    m["w_out"] = np.ascontiguousarray(g("w_out")[0])
    wr = np.concatenate([g("w_rg")[0], g("w_re")[0].transpose(1, 0, 2).reshape(D, 32)], axis=1)
    m["wr"] = np.ascontiguousarray(wr.reshape(KC, 128, 36).transpose(1, 0, 2))
    m["w_eg"] = np.ascontiguousarray(g("w_eg")[0].reshape(32, KC, 128, 512).transpose(0, 2, 1, 3))
    m["w_eu"] = np.ascontiguousarray(g("w_eu")[0].reshape(32, KC, 128, 512).transpose(0, 2, 1, 3))
    m["w_ed"] = np.ascontiguousarray(g("w_ed")[0].reshape(32, 4, 128, D).transpose(0, 2, 1, 3))
    hc = np.zeros((128, 128), np.float32)
    hc[:, 0:16] = (512.0 * np.arange(16))[None, :]
    hc[:, 16:64] = np.arange(48, dtype=np.float32)[None, :]
    hc[:, 80] = np.arange(128, dtype=np.float32)
    m["hc"] = hc
    m["zeros"] = np.zeros((2048, D), dtype=ml_dtypes.bfloat16)
    m["cs"] = c["cs"]
    m["dft_tab"] = c["dft_tab"]
    m["rp"] = rp
    m["masks"] = np.ascontiguousarray(np.stack([c["m_le"], c["m_gt"], c["m_ge"], c["m_lt"], c["ones"]], axis=1))
    m["ident_f"] = c["ident_f"]
    m["ident_b"] = c["ident_b"]
    return m


def kernel(**inputs):
    nc = build()
    in_maps = [_prep_inputs(inputs, b) for b in range(8)]
    res = run_bass_kernel_spmd(nc, in_maps, core_ids=list(range(8)))
    return np.stack([r["out"] for r in res.results], axis=0)
```
